# Optimizing a Trainium2 kernel written in Bass

```python
import math
import jax, jax.numpy as jnp
from jax import lax
import numpy as np


D_MODEL = 2048
BATCH = 16
SEQ = 256
DEPTH = 1
DEC_BATCH = 4
DEC_SEQ = 4096
PAST_LEN = 512

GRID_W = 64
N_HEADS = 8
HEAD_DIM = 128
V_HEAD_DIM = 2 * HEAD_DIM
ATTN_W = N_HEADS * V_HEAD_DIM
D_CONV = 1024
CONV_WIDTH = 3
N_EXPERTS = 64
TOP_K = 8
N_GROUPS = 8
TOPK_GROUPS = 4
D_EXPERT = 512
D_SHARED = 512
ROUTED_SCALE = 2.5
ROPE_THETA = 10000.0
Q_BLOCK = 128
EPS = 1e-6
PROJ_SPLITS = (2 * N_HEADS * HEAD_DIM, 2 * N_HEADS * HEAD_DIM, N_HEADS * V_HEAD_DIM,
               D_CONV, D_CONV, D_CONV, D_MODEL, D_MODEL)
D_PROJ = sum(PROJ_SPLITS)

kernel_name = "hybrid_diff_attn_shortconv_moe_step"


def _rmsnorm(x, g):
    xf = x.astype(jnp.float32)
    xf = xf * lax.rsqrt(jnp.mean(xf * xf, axis=-1, keepdims=True) + EPS)
    return xf.astype(x.dtype) * g


def _axial_rope_tables(n_tokens):
    rows = n_tokens // GRID_W
    row = jnp.repeat(jnp.arange(rows, dtype=jnp.float32), GRID_W)
    col = jnp.tile(jnp.arange(GRID_W, dtype=jnp.float32), rows)
    n_freq = HEAD_DIM // 4
    freqs = ROPE_THETA ** (-jnp.arange(n_freq, dtype=jnp.float32) / n_freq)
    ang_r = row[:, None] * freqs
    ang_c = col[:, None] * freqs
    ang = jnp.concatenate([ang_r, ang_r, ang_c, ang_c], axis=-1)
    return jnp.cos(ang), jnp.sin(ang)


def _apply_rope(x, cos, sin):
    xr = x.reshape(x.shape[:-1] + (2, 2, HEAD_DIM // 4))
    rot = jnp.stack([-xr[..., 1, :], xr[..., 0, :]], axis=-2).reshape(x.shape)
    cos = cos[None, :, None, None, :].astype(x.dtype)
    sin = sin[None, :, None, None, :].astype(x.dtype)
    return x * cos + rot * sin


def _diff_attention(q, k, v, lam):
    b, nq = q.shape[:2]
    nb = nq // Q_BLOCK
    qb = jnp.moveaxis(q.reshape(b, nb, Q_BLOCK, 2, N_HEADS, HEAD_DIM), 1, 0)
    scale = HEAD_DIM ** -0.5

    def block(qblk):
        s = jnp.einsum('bqjhd,bkjhd->bjhqk', qblk, k).astype(jnp.float32) * scale
        p = jax.nn.softmax(s, axis=-1)
        a = p[:, 0] - lam * p[:, 1]
        return jnp.einsum('bhqk,bkhe->bqhe', a.astype(v.dtype), v)

    out = lax.map(block, qb)
    return jnp.moveaxis(out, 0, 1).reshape(b, nq, N_HEADS, V_HEAD_DIM)


def _short_conv(u, w):
    pad = CONV_WIDTH // 2
    n = u.shape[1]
    up = jnp.pad(u, ((0, 0), (pad, pad), (0, 0)))
    return sum(up[:, i:i + n] * w[i] for i in range(CONV_WIDTH))


def _moe(h, p):
    b, n, d = h.shape
    t = h.reshape(b * n, d)
    s = jax.nn.sigmoid((t @ p['w_router']).astype(jnp.float32))
    biased = s + p['router_bias'].astype(jnp.float32)
    per_grp = N_EXPERTS // N_GROUPS
    grp_score = lax.top_k(biased.reshape(-1, N_GROUPS, per_grp), 2)[0].sum(-1)
    _, grp_idx = lax.top_k(grp_score, TOPK_GROUPS)
    grp_mask = jax.nn.one_hot(grp_idx, N_GROUPS, dtype=jnp.float32).sum(1)
    expert_mask = jnp.repeat(grp_mask, per_grp, axis=1)
    masked = jnp.where(expert_mask > 0, biased, -jnp.inf)
    _, idx = lax.top_k(masked, TOP_K)
    w = jnp.take_along_axis(s, idx, axis=1)
    w = w / jnp.sum(w, axis=-1, keepdims=True) * ROUTED_SCALE
    combine = jnp.sum(jax.nn.one_hot(idx, N_EXPERTS, dtype=jnp.float32) * w[..., None], axis=1)

    def expert(acc, xs):
        wg, wu, wd, ce = xs
        hid = jax.nn.silu(t @ wg) * (t @ wu)
        return acc + ce[:, None] * (hid @ wd).astype(jnp.float32), None

    routed, _ = lax.scan(expert, jnp.zeros((t.shape[0], d), jnp.float32),
                         (p['w_exp_gate'], p['w_exp_up'], p['w_exp_down'], combine.T))
    shared = (jax.nn.silu(t @ p['w_sh_gate']) * (t @ p['w_sh_up'])) @ p['w_sh_down']
    return (routed.astype(h.dtype) + shared).reshape(b, n, d)


def _layer(x, mod, p, layer_idx, rope=None, ctx_kv=None):
    shift1, scale1, gate1, shift2, scale2, gate2 = jnp.split(mod, 6, axis=-1)
    b, n, _ = x.shape
    h = _rmsnorm(x, p['g_pre_mix']) * (1 + scale1) + shift1
    q, k, v, cb, cc, cx, ga, gc = jnp.split(h @ p['w_in'], np.cumsum(PROJ_SPLITS)[:-1].tolist(), axis=-1)
    q = q.reshape(b, n, 2, N_HEADS, HEAD_DIM)
    k = k.reshape(b, n, 2, N_HEADS, HEAD_DIM)
    v = v.reshape(b, n, N_HEADS, V_HEAD_DIM)
    lam_init = 0.8 - 0.6 * math.exp(-0.3 * layer_idx)
    lam = (jnp.exp(jnp.sum(p['lambda_q1'] * p['lambda_k1']).astype(jnp.float32))
           - jnp.exp(jnp.sum(p['lambda_q2'] * p['lambda_k2']).astype(jnp.float32)) + lam_init)
    if ctx_kv is None:
        keys, vals = k, v
        new_kv = (k, v)
    else:
        cos, sin = rope
        q = _apply_rope(q, cos, sin)
        keys = jnp.concatenate([_apply_rope(k, cos, sin), ctx_kv[0]], axis=1)
        vals = jnp.concatenate([v, ctx_kv[1]], axis=1)
        new_kv = None
    attn = _diff_attention(q, keys, vals, lam)
    attn = (_rmsnorm(attn, p['g_subln']) * (1 - lam_init)).reshape(b, n, ATTN_W)
    conv = cb * _short_conv(cc * cx, p['conv_w'])
    merged = (jax.nn.sigmoid(ga) * (attn @ p['w_attn_out'])
              + jax.nn.sigmoid(gc) * (conv @ p['w_conv_out']))
    x = x + gate1 * _rmsnorm(merged @ p['w_o'], p['g_post_mix'])
    h2 = _rmsnorm(x, p['g_pre_ffn']) * (1 + scale2) + shift2
    x = x + gate2 * _rmsnorm(_moe(h2, p), p['g_post_ffn'])
    return x, new_kv


def setup_inputs(seed: int = 0) -> dict:
    key = jax.random.key(seed)
    ks = jax.random.split(key, 32)

    def nrm(i, shape, scale):
        return jax.random.normal(ks[i], shape, jnp.float32) * scale

    L, D = DEPTH, D_MODEL
    return {
        'x_prompt': nrm(0, (BATCH, SEQ, D), 1.0),
        'x_sample': nrm(1, (DEC_BATCH, DEC_SEQ, D), 1.0),
        'cache_k': nrm(2, (DEC_BATCH, DEPTH, PAST_LEN, 2, N_HEADS, HEAD_DIM), 1.0),
        'cache_v': nrm(3, (DEC_BATCH, DEPTH, PAST_LEN, N_HEADS, V_HEAD_DIM), 1.0),
        'c': nrm(4, (DEC_BATCH, D), 1.0),
        'c_ctx': nrm(5, (D,), 1.0),
        'w_ada': nrm(6, (L, D, 6 * D), 0.5 * D ** -0.5),
        'b_ada': nrm(7, (L, 6 * D), 0.01),
        'g_pre_mix': 1.0 + nrm(8, (L, D), 0.02),
        'g_post_mix': 1.0 + nrm(9, (L, D), 0.02),
        'w_in': nrm(10, (L, D, D_PROJ), D ** -0.5),
        'lambda_q1': nrm(11, (L, HEAD_DIM), 0.1),
        'lambda_k1': nrm(12, (L, HEAD_DIM), 0.1),
        'lambda_q2': nrm(13, (L, HEAD_DIM), 0.1),
        'lambda_k2': nrm(14, (L, HEAD_DIM), 0.1),
        'g_subln': 1.0 + nrm(15, (L, V_HEAD_DIM), 0.02),
        'conv_w': nrm(16, (L, CONV_WIDTH, D_CONV), CONV_WIDTH ** -0.5),
        'w_attn_out': nrm(17, (L, ATTN_W, D), ATTN_W ** -0.5),
        'w_conv_out': nrm(18, (L, D_CONV, D), D_CONV ** -0.5),
        'w_o': nrm(19, (L, D, D), D ** -0.5),
        'g_pre_ffn': 1.0 + nrm(20, (L, D), 0.02),
        'g_post_ffn': 1.0 + nrm(21, (L, D), 0.02),
        'w_router': nrm(22, (L, D, N_EXPERTS), D ** -0.5),
        'router_bias': nrm(23, (L, N_EXPERTS), 0.01),
        'w_exp_gate': nrm(24, (L, N_EXPERTS, D, D_EXPERT), D ** -0.5),
        'w_exp_up': nrm(25, (L, N_EXPERTS, D, D_EXPERT), D ** -0.5),
        'w_exp_down': nrm(26, (L, N_EXPERTS, D_EXPERT, D), D_EXPERT ** -0.5),
        'w_sh_gate': nrm(27, (L, D, D_SHARED), D ** -0.5),
        'w_sh_up': nrm(28, (L, D, D_SHARED), D ** -0.5),
        'w_sh_down': nrm(29, (L, D_SHARED, D), D_SHARED ** -0.5),
    }


def reference(x_prompt, x_sample, cache_k, cache_v, c, c_ctx, w_ada, b_ada, g_pre_mix, g_post_mix,
              w_in, lambda_q1, lambda_k1, lambda_q2, lambda_k2, g_subln, conv_w, w_attn_out,
              w_conv_out, w_o, g_pre_ffn, g_post_ffn, w_router, router_bias, w_exp_gate, w_exp_up,
              w_exp_down, w_sh_gate, w_sh_up, w_sh_down):
    def params(l):
        return {
            'g_pre_mix': g_pre_mix[l], 'g_post_mix': g_post_mix[l], 'w_in': w_in[l],
            'lambda_q1': lambda_q1[l], 'lambda_k1': lambda_k1[l],
            'lambda_q2': lambda_q2[l], 'lambda_k2': lambda_k2[l],
            'g_subln': g_subln[l], 'conv_w': conv_w[l], 'w_attn_out': w_attn_out[l],
            'w_conv_out': w_conv_out[l], 'w_o': w_o[l], 'g_pre_ffn': g_pre_ffn[l],
            'g_post_ffn': g_post_ffn[l], 'w_router': w_router[l], 'router_bias': router_bias[l],
            'w_exp_gate': w_exp_gate[l], 'w_exp_up': w_exp_up[l], 'w_exp_down': w_exp_down[l],
            'w_sh_gate': w_sh_gate[l], 'w_sh_up': w_sh_up[l], 'w_sh_down': w_sh_down[l],
        }

    xp = x_prompt
    ks, vs = [], []
    for l in range(DEPTH):
        mod_ctx = (jax.nn.silu(c_ctx)[None, :] @ w_ada[l] + b_ada[l])[:, None, :]
        xp, (k_ctx, v_ctx) = _layer(xp, mod_ctx, params(l), l)
        ks.append(k_ctx)
        vs.append(v_ctx)
    new_cache_k = jnp.stack(ks, axis=1)
    new_cache_v = jnp.stack(vs, axis=1)

    rope = _axial_rope_tables(x_sample.shape[1])
    xs = x_sample
    for l in range(DEPTH):
        mod_lat = (jax.nn.silu(c) @ w_ada[l] + b_ada[l])[:, None, :]
        xs, _ = _layer(xs, mod_lat, params(l), l, rope=rope,
                       ctx_kv=(cache_k[:, l], cache_v[:, l]))

    return (xp, xs, new_cache_k, new_cache_v)
```

```python
import math
import numpy as np
from contextlib import ExitStack
import concourse.bass as bass
import concourse.mybir as mybir
from concourse.bass_utils import run_bass_kernel_spmd

F32 = mybir.dt.float32
BF16 = mybir.dt.bfloat16
AF = mybir.ActivationFunctionType
ALU = mybir.AluOpType

D = 2048
NPROJ = 13312
NH = 8
HD = 128
VD = 256
DCONV = 1024
NE = 64
DEXP = 512
EPS = 1e-6
LAM_INIT = 0.8 - 0.6 * math.exp(-0.3 * 0)
ROUTED_SCALE = 2.5
ROPE_THETA = 10000.0
NOWN = 2560
PAST = 512


class Sched:
    def __init__(self, nc, es):
        self.nc = nc
        self.eng = {'pe': nc.tensor, 'act': nc.scalar, 'dve': nc.vector, 'pool': nc.gpsimd, 'sp': nc.sync}
        self.es = es
        self.esem = {e: es.enter_context(nc.semaphore("sem_" + e)) for e in self.eng}
        self.eseq = {e: 0 for e in self.eng}
        self.waited = {e: {} for e in self.eng}
        self.lastw = {}
        self.readers = {}
        self.dsem = {}
        self.bar = es.enter_context(nc.semaphore("sem_bar"))
        self.barc = 0
        self.nsem = 6

    def _deps(self, e, reads, writes):
        deps = {}

        def add(tok):
            if tok is None:
                return
            s, v = tok
            k = id(s)
            if k not in deps or deps[k][1] < v:
                deps[k] = (s, v)
        for r in reads:
            add(self.lastw.get(r))
            if isinstance(r, tuple) and r[0] == 'ps':
                for tok in self.readers.get(r, {}).values():
                    if tok[0] is not self.esem.get(e):
                        add(tok)
        for w in writes:
            add(self.lastw.get(w))
            for tok in self.readers.get(w, {}).values():
                add(tok)
        for k, (s, v) in deps.items():
            if e == 'pe' and s is self.esem['pe']:
                continue
            if self.waited[e].get(k, 0) >= v:
                continue
            self.eng[e].wait_ge(s, v)
            self.waited[e][k] = v

    def _record(self, tok, reads, writes):
        for r in reads:
            self.readers.setdefault(r, {})[id(tok[0])] = tok
        for w in writes:
            self.lastw[w] = tok
            self.readers[w] = {}

    def op(self, e, fn, reads=(), writes=()):
        self._deps(e, reads, writes)
        ins = fn(self.eng[e])
        self.eseq[e] += 1
        ins.then_inc(self.esem[e], 1)
        self._record((self.esem[e], self.eseq[e]), reads, writes)
        return ins

    def dma(self, q, pairs, reads=(), writes=(), semkey=None, **kw):
        self._deps(q, reads, writes)
        if semkey not in self.dsem:
            self.dsem[semkey] = [self.es.enter_context(self.nc.semaphore("dsem%d" % self.nsem)), 0]
            self.nsem += 1
        ent = self.dsem[semkey]
        for (o, i) in pairs:
            self.eng[q].dma_start(out=o, in_=i, **kw).then_inc(ent[0], 16)
            ent[1] += 16
        self._record((ent[0], ent[1]), reads, writes)

    def barrier(self):
        sp = self.eng['sp']
        for e in ('pe', 'act', 'dve', 'pool'):
            if self.eseq[e] > 0:
                sp.wait_ge(self.esem[e], self.eseq[e])
        for k, ent in self.dsem.items():
            if ent[1] > 0:
                sp.wait_ge(ent[0], ent[1])
        self.barc += 1
        sp.nop().then_inc(self.bar, 1)
        for e in ('pe', 'act', 'dve', 'pool'):
            self.eng[e].wait_ge(self.bar, self.barc)
        self.lastw = {}
        self.readers = {}


def mm_group(pe, out, pairs):
    n = len(pairs)
    last = None
    for i, (l, r) in enumerate(pairs):
        last = pe.matmul(out, lhsT=l, rhs=r, start=(i == 0), stop=(i == n - 1))
    return last


def build(phases=99, dbg=False, only_tiles=None, cut=None, nexp_dbg=None):
    nc = bass.Bass("TRN2", target_bir_lowering=False)

    def din(name, shape, dt=F32):
        return nc.dram_tensor(name, list(shape), dt, kind="ExternalInput").ap()

    def dout(name, shape, dt=F32):
        return nc.dram_tensor(name, list(shape), dt, kind="ExternalOutput").ap()

    def dscr(name, shape, dt):
        return nc.dram_tensor(name, list(shape), dt, kind="ExternalOutput" if dbg else "Internal").ap()

    xp = din("xp", [512, D])
    xs = din("xs", [4096, D])
    xh = din("xh", [2, D])
    pos = din("pos", [2, 4096])
    hmask = din("hmask", [1, 4])
    csel = din("csel", [2, D])
    ck = din("ck", [PAST, 2048])
    cv = din("cv", [PAST, 2048])
    w_ada = din("w_ada", [D, 6 * D])
    b_ada = din("b_ada", [1, 6 * D])
    g_pre_mix = din("g_pre_mix", [1, D])
    g_post_mix = din("g_post_mix", [1, D])
    w_in = din("w_in", [D, NPROJ])
    lq1 = din("lq1", [1, HD])
    lk1 = din("lk1", [1, HD])
    lq2 = din("lq2", [1, HD])
    lk2 = din("lk2", [1, HD])
    g_subln = din("g_subln", [1, VD])
    conv_w = din("conv_w", [3, DCONV])
    w_ao = din("w_ao", [2048, D])
    w_co = din("w_co", [DCONV, D])
    w_o = din("w_o", [D, D])
    g_pre_ffn = din("g_pre_ffn", [1, D])
    g_post_ffn = din("g_post_ffn", [1, D])
    w_router = din("w_router", [D, NE])
    router_bias = din("router_bias", [1, NE])
    if phases >= 4:
        w_eg = din("w_eg", [NE, D, DEXP])
        w_eu = din("w_eu", [NE, D, DEXP])
        w_ed = din("w_ed", [NE, DEXP, D])
    w_sg = din("w_sg", [D, DEXP])
    w_su = din("w_su", [D, DEXP])
    w_sd = din("w_sd", [DEXP, D])
    yp = dout("yp", [512, D])
    ys = dout("ys", [2048, D])
    nk = dout("nk", [512, 2048])
    nv = dout("nv", [512, 2048])
    modrows = dscr("modrows", [12, D], F32)
    qT = dscr("qT", [2048, NOWN], BF16)
    kTp = dscr("kTp", [2048, 512], BF16)
    kTs = dscr("kTs", [2048, 4096], BF16)
    vp = dscr("vp", [512, 2048], BF16)
    vs = dscr("vs", [4096, 2048], BF16)
    sgaT = dscr("sgaT", [2048, NOWN], BF16)
    mcT = dscr("mcT", [2048, NOWN], BF16)
    attnT = dscr("attnT", [2048, NOWN], BF16)
    x1d = dscr("x1d", [NOWN, D], F32)
    NBLK = (NOWN * 8) // 512 + NE
    NSLOT = NBLK * 512
    h2d = dscr("h2d", [NOWN, D], BF16)
    shd = dscr("shd", [NOWN, D], F32)
    xsort = dscr("xsort", [NSLOT, D], BF16)
    ysorth = [dscr("ysort%d" % i, [NSLOT, D // 2], F32) for i in range(2)]
    ropec = dscr("ropec", [128, 4096], F32)
    ropes = dscr("ropes", [128, 4096], F32)

    with ExitStack() as es:
        s = Sched(nc, es)

        def sb(name, shape, dt):
            return es.enter_context(nc.sbuf_tensor(name, list(shape), dt))

        ps = [es.enter_context(nc.psum_tensor("ps%d" % i, [128, 512], F32)) for i in range(8)]
        psc = {}

        def psn(pool):
            k = tuple(pool)
            c = psc.get(k, 0)
            psc[k] = c + 1
            return pool[c % len(pool)]

        ident_f = sb("ident_f", [128, 128], F32)
        ident = sb("ident", [128, 128], BF16)
        pmat = sb("pmat", [128, 128], BF16)
        ones_b = sb("ones_b", [128, 128], BF16)
        st1 = sb("st1", [128, 8], F32)
        negpi = sb("negpi", [128, 1], F32)
        epsb = sb("epsb", [128, 1], F32)
        lam_t = sb("lam_t", [128, 2], F32)
        gsub = sb("gsub", [128, 2], F32)
        cw = sb("cw", [128, 3, 8], F32)
        hm = sb("hm", [128, 4], F32)
        NTB = NOWN // 128
        stc = [0]
        stfc = [0]

        class WS:
            def __init__(self, ring):
                self.ring = ring
                self.NR = len(ring)
                self.items = []
                self.issued = 0
                self.consumed = 0
                self.base = 0

            def add(self, src, k):
                self.items.append((src, k))

            def _view(self, slot, src, k):
                n = src.shape[-1]
                return self.ring[slot][:, 0:k * n].rearrange("p (k n) -> p k n", k=k)

            def _issue(self, i):
                src, k = self.items[i]
                slot = (self.base + i) % self.NR
                s.dma('pool', [(self._view(slot, src, k), src)], writes=[('ring', slot)], semkey=('ring', slot))

            def get(self):
                lim = min(len(self.items), self.consumed + self.NR)
                while self.issued < lim:
                    self._issue(self.issued)
                    self.issued += 1
                slot = (self.base + self.consumed) % self.NR
                src, k = self.items[self.consumed]
                self.consumed += 1
                return slot, self._view(slot, src, k)

            def get_group(self, n):
                lim = min(len(self.items), self.consumed + self.NR)
                while self.issued < lim:
                    self._issue(self.issued)
                    self.issued += 1
                out = []
                for _ in range(n):
                    slot = (self.base + self.consumed) % self.NR
                    src, k = self.items[self.consumed]
                    assert self.consumed < self.issued
                    self.consumed += 1
                    out.append((slot, self._view(slot, src, k)))
                return out

            def finish(self):
                assert self.consumed == len(self.items), (self.consumed, len(self.items))
                self.base = (self.base + len(self.items)) % self.NR
                self.items = []
                self.issued = 0
                self.consumed = 0

        def wsrc(w2d, c0, n):
            return w2d[:, c0:c0 + n].rearrange("(k p) n -> p k n", p=128)

        s.op('pool', lambda g: g.memset(ident_f[:], 0.0), writes=['ident_f'])
        s.op('pool', lambda g: g.affine_select(out=ident_f[:], in_=ident_f[:], pattern=[[-1, 128]],
                                               compare_op=ALU.not_equal, fill=1.0, base=0, channel_multiplier=1),
             reads=['ident_f'], writes=['ident_f'])
        s.op('dve', lambda v: v.tensor_copy(ident[:], ident_f[:]), reads=['ident_f'], writes=['ident'])
        for (a, b) in ((0, 32), (32, 0), (64, 96), (96, 64)):
            s.op('dve', lambda v, a=a, b=b: v.tensor_copy(pmat[:, a:a + 32], ident_f[:, b:b + 32]),
                 reads=['ident_f'], writes=['pmat'])
        s.op('dve', lambda v: v.memset(ones_b[:], 1.0), writes=['ones_b'])
        s.op('dve', lambda v: v.memset(negpi[:], -math.pi), writes=['negpi'])
        s.op('dve', lambda v: v.memset(epsb[:], EPS), writes=['epsb'])

        pes = ExitStack()
        sbp = lambda name, shape, dt: pes.enter_context(nc.sbuf_tensor(name + '_p1', list(shape), dt))
        ring = [sbp("ring%d" % i, [128, 8192], BF16) for i in range(4)]
        ws = WS(ring)
        big = sbp("big1", [128, 12288], F32)
        tmpf = sbp("tmpf1", [128, 2 * HD], F32)
        scT = sbp("scT", [128, 16, 2], F32)
        scTb = sbp("scTb", [128, 16, 2], BF16)
        btl = [sbp("bt%d" % i, [2, 512], F32) for i in range(2)]
        tg = [sbp("tg%d" % i, [2, D], F32) for i in range(2)]
        tr_ = [sbp("tr%d" % i, [2, D], F32) for i in range(2)]
        s.dma('sp', [(scT[:, :, cc_], csel[cc_:cc_ + 1, :].rearrange("o (k p) -> p (o k)", p=128)) for cc_ in range(2)],
              writes=['scT'], semkey='scT', allow_slow_non_contiguous=True)
        s.op('act', lambda a: a.activation(out=scTb[:], in_=scT[:], func=AF.Silu), reads=['scT'], writes=['scTb'])
        modv = big[0:2, 0:6 * D]
        for n in range(24):
            ws.add(wsrc(w_ada, n * 512, 512), 16)
        for n in range(24):
            slot, wv = ws.get()
            b = psn([0, 1])
            bi = n % 2
            s.dma('sp', [(btl[bi][:], b_ada[0:1, n * 512:(n + 1) * 512].partition_broadcast(2))], writes=[('bt', bi)], semkey=('bt', bi))
            s.op('pe', lambda pe, wv=wv, b=b: mm_group(pe, ps[b][0:2, :], [(scTb[:, k, :], wv[:, k, :]) for k in range(16)]),
                 reads=[('ring', slot), 'scTb'], writes=[('ps', b)])
            s.op('dve', lambda v, n=n, b=b, bi=bi: v.tensor_add(modv[:, n * 512:(n + 1) * 512], ps[b][0:2, :], btl[bi][:]),
                 reads=[('ps', b), ('bt', bi)], writes=['modv'])
        ws.finish()
        mv = lambda i: modv[:, i * D:(i + 1) * D]
        mr3 = modrows.rearrange("(c i) d -> c i d", c=2)
        plan = [(0, 1, g_pre_mix, True), (1, 0, None, False), (2, 2, g_post_mix, False),
                (3, 4, g_pre_ffn, True), (4, 3, None, False), (5, 5, g_post_ffn, False)]
        for idx, (row, chunk, gv, plus1) in enumerate(plan):
            ti = idx % 2
            if gv is not None:
                s.dma('sp', [(tg[ti][:], gv.partition_broadcast(2))], writes=[('tg', ti)], semkey=('tg', ti))
                if plus1:
                    s.op('dve', lambda v, ti=ti, chunk=chunk: v.scalar_tensor_tensor(out=tr_[ti][:], in0=mv(chunk), scalar=1.0, in1=tg[ti][:],
                                                                                    op0=ALU.add, op1=ALU.mult),
                         reads=['modv', ('tg', ti)], writes=[('tr', ti)])
                else:
                    s.op('dve', lambda v, ti=ti, chunk=chunk: v.tensor_mul(tr_[ti][:], mv(chunk), tg[ti][:]),
                         reads=['modv', ('tg', ti)], writes=[('tr', ti)])
            else:
                s.op('dve', lambda v, ti=ti, chunk=chunk: v.tensor_copy(tr_[ti][:], mv(chunk)), reads=['modv'], writes=[('tr', ti)])
            s.dma('sp', [(mr3[:, row, :], tr_[ti][:])], reads=[('tr', ti)], semkey=('trs', ti))

        lt = sbp("lt", [128, 4, HD], F32)
        for i, l in enumerate((lq1, lk1, lq2, lk2)):
            s.dma('sp', [(lt[:, i, :], l.partition_broadcast(128))], writes=[('lt', i)], semkey=('lt', i))
        for j in range(2):
            s.op('dve', lambda v, j=j: v.tensor_tensor(out=tmpf[:, j * HD:(j + 1) * HD], in0=lt[:, 2 * j, :], in1=lt[:, 2 * j + 1, :], op=ALU.mult),
                 reads=[('lt', 2 * j), ('lt', 2 * j + 1)], writes=[('tmpf', j)])
            s.op('dve', lambda v, j=j: v.reduce_sum(out=st1[:, j:j + 1], in_=tmpf[:, j * HD:(j + 1) * HD], axis=mybir.AxisListType.X),
                 reads=[('tmpf', j)], writes=[('st1', j)])
        s.op('act', lambda a: a.activation(out=st1[:, 2:4], in_=st1[:, 0:2], func=AF.Exp), reads=[('st1', 0), ('st1', 1)], writes=['st1e'])
        s.op('dve', lambda v: v.tensor_sub(lam_t[:, 0:1], st1[:, 3:4], st1[:, 2:3]), reads=['st1e'], writes=['lam_t'])
        s.op('dve', lambda v: v.tensor_scalar_add(lam_t[:, 0:1], lam_t[:, 0:1], -LAM_INIT), reads=['lam_t'], writes=['lam_t'])
        s.dma('sp', [(gsub[:], g_subln.rearrange("o (h p) -> p (o h)", p=128))], writes=['gsub'], semkey='gsub',
              allow_slow_non_contiguous=True)
        s.op('dve', lambda v: v.tensor_scalar_mul(gsub[:], gsub[:], 1.0 - LAM_INIT), reads=['gsub'], writes=['gsub'])
        s.dma('sp', [(cw[:, i_, :], conv_w[i_:i_ + 1, :].rearrange("o (c p) -> p (o c)", p=128)) for i_ in range(3)],
              writes=['cw'], semkey='cw', allow_slow_non_contiguous=True)

        I32 = mybir.dt.int32
        ang = big[:, 0:4096]
        rt1 = big[:, 4096:8192]
        rtf = big[:, 8192:12288]
        rti = rtf.bitcast(I32)
        pidx = sbp("pidx", [128, 4], F32)
        io_i = sbp("io_i", [128, 128], I32)
        io_f = sbp("io_f", [128, 128], F32)
        s.op('pool', lambda g: g.iota(io_i[:], pattern=[[0, 4], [1, 32]], base=0, channel_multiplier=0), writes=['io_i'])
        s.op('dve', lambda v: v.tensor_copy(io_f[:], io_i[:]), reads=['io_i'], writes=['io_f'])
        s.op('dve', lambda v: v.tensor_mul(io_f[:], io_f[:], ident_f[:]), reads=['io_f', 'ident_f'], writes=['io_f'])
        s.op('dve', lambda v: v.reduce_sum(out=pidx[:, 1:2], in_=io_f[:], axis=mybir.AxisListType.X), reads=['io_f'], writes=['pidx1'])
        s.op('act', lambda a: a.activation(out=pidx[:, 2:3], in_=pidx[:, 1:2], func=AF.Exp, scale=-math.log(ROPE_THETA) / 32.0),
             reads=['pidx1'], writes=['freq'])
        s.op('pool', lambda g: g.iota(io_i[:], pattern=[[0, 2], [1, 2], [0, 32]], base=0, channel_multiplier=0), reads=['io_i'], writes=['io_i'])
        s.op('dve', lambda v: v.tensor_copy(io_f[:], io_i[:]), reads=['io_i', 'io_f'], writes=['io_f'])
        s.op('dve', lambda v: v.tensor_mul(io_f[:], io_f[:], ident_f[:]), reads=['io_f', 'ident_f'], writes=['io_f'])
        s.op('dve', lambda v: v.reduce_sum(out=pidx[:, 3:4], in_=io_f[:], axis=mybir.AxisListType.X), reads=['io_f'], writes=['sgn'])
        s.op('dve', lambda v: v.tensor_scalar(out=pidx[:, 3:4], in0=pidx[:, 3:4], scalar1=2.0, scalar2=-1.0, op0=ALU.mult, op1=ALU.add),
             reads=['sgn'], writes=['sgn'])
        s.dma('sp', [(ang[0:64, :], pos[0:1, :].partition_broadcast(64)), (ang[64:128, :], pos[1:2, :].partition_broadcast(64))],
              reads=['modv'], writes=['ang', 'modv'], semkey='posb')
        s.op('dve', lambda v: v.tensor_scalar_mul(ang, ang, pidx[:, 2:3]), reads=['ang', 'freq'], writes=['ang'])

        def sin_of(shift, dst_dram, signed, key):
            s.op('dve', lambda v: v.tensor_scalar(out=rtf, in0=ang, scalar1=shift, scalar2=1.0 / (2 * math.pi), op0=ALU.add, op1=ALU.mult),
                 reads=['ang'], writes=['rtf'])
            s.op('dve', lambda v: v.tensor_copy(rt1.bitcast(I32), rtf), reads=['rtf'], writes=['rt1'])
            s.op('dve', lambda v: v.tensor_copy(rtf, rt1.bitcast(I32)), reads=['rt1'], writes=['rtf'])
            s.op('dve', lambda v: v.scalar_tensor_tensor(out=rt1, in0=rtf, scalar=-2 * math.pi, in1=ang, op0=ALU.mult, op1=ALU.add),
                 reads=['rtf', 'ang'], writes=['rt1'])
            s.op('dve', lambda v: v.tensor_scalar(out=rt1, in0=rt1, scalar1=-3.1415925 - shift, scalar2=3.1415925 - shift, op0=ALU.max, op1=ALU.min),
                 reads=['rt1'], writes=['rt1'])
            s.op('act', lambda a: a.activation(out=rt1, in_=rt1, func=AF.Sin, bias=sh_t[:, key:key + 1]), reads=['rt1', 'sh_t'], writes=['rt1'])
            if signed:
                s.op('dve', lambda v: v.tensor_scalar_mul(rt1, rt1, pidx[:, 3:4]), reads=['rt1', 'sgn'], writes=['rt1'])
            s.dma('sp', [(dst_dram, rt1)], reads=['rt1'], writes=[('rope', key)], semkey=('rope', key))

        sh_t = sbp("sh_t", [128, 2], F32)
        s.op('dve', lambda v: v.memset(sh_t[:, 0:1], 0.0), writes=['sh_t'])
        s.op('dve', lambda v: v.memset(sh_t[:, 1:2], math.pi / 2), reads=['sh_t'], writes=['sh_t'])
        sin_of(0.0, ropes, True, 0)
        sin_of(math.pi / 2, ropec, False, 1)
        s.dma('sp', [(hm[:], hmask.partition_broadcast(128))], writes=['hm'], semkey='hm')
        s.barrier()
        pes.close()
        if phases <= 1:
            return nc

        def load_bc(i, cond, row, key):
            r = cond * 6 + row
            s.dma('sp', [(bc[i][:], modrows[r:r + 1, :].partition_broadcast(128))], writes=[('bc', i)], semkey=('bc', i))

        def rstd_from_ss(col, dim=D):
            s.op('act', lambda a: a.activation(out=st1[:, col:col + 1], in_=st1[:, col:col + 1], func=AF.Sqrt, bias=epsb[:, 0:1], scale=1.0 / dim),
                 reads=[('ss', col), 'epsb'], writes=[('ss', col)])
            s.op('dve', lambda v: v.reciprocal(st1[:, col:col + 1], st1[:, col:col + 1]), reads=[('ss', col)], writes=[('ss', col)])

        def norm_mod_transpose(src_ap, src_key, npart, dst_fn, a_bc, b_bc, hb_store=None):
            P = npart
            s.op('act', lambda a: a.activation(out=hb[0:P, :], in_=src_ap, func=AF.Square, accum_out=st1[0:P, 4:5]),
                 reads=[src_key], writes=['hb', ('ss', 4)])
            rstd_from_ss(4)
            s.op('dve', lambda v: v.scalar_tensor_tensor(out=src_ap, in0=src_ap, scalar=st1[0:P, 4:5], in1=bc[a_bc][0:P, :],
                                                          op0=ALU.mult, op1=ALU.mult),
                 reads=[('ss', 4), ('bc', a_bc)], writes=[src_key])
            s.op('dve', lambda v: v.tensor_add(hb[0:P, :], src_ap, bc[b_bc][0:P, :]), reads=[src_key, ('bc', b_bc)], writes=['hb'])
            if hb_store is not None:
                s.op('dve', lambda v: v.tensor_copy(hbp[0:P, :].rearrange("p (k q) -> p k q", k=16), hb[0:P, :].rearrange("p (q k) -> p k q", k=16)),
                     reads=['hb'], writes=['hbp'])
                s.dma('sp', [(hb_store, hbp[0:P, :])], reads=['hbp'], semkey='hbst')
            transpose16(hb, 'hb', P, dst_fn)

        def transpose16(src, src_key, P, dst_fn):
            for half in range(2):
                b = psn([6, 7])
                pst = ps[b].bitcast(BF16)

                def tr(pe, half=half, pst=pst):
                    last = None
                    for j in range(8):
                        k = half * 8 + j
                        last = pe.transpose(pst[:, j * 128:j * 128 + P], src[0:P, k * 128:(k + 1) * 128], ident[0:P, 0:P])
                    return last
                s.op('pe', tr, reads=[src_key, 'ident'], writes=[('ps', b)])
                dst, dkeys = dst_fn(half)
                s.op('act', lambda a, pst=pst, dst=dst: a.copy(dst, pst.rearrange("p (j t) -> p j t", j=8)[:, :, 0:P]),
                     reads=[('ps', b)], writes=dkeys)

        tiles2 = [
            dict(name='P', T=512, x=xp, x0=0, cond=0, rope=None, full=True, own0=0, kdst=(kTp, 0), vdst=(vp, 0),
                 segs=[(0, 256), (256, 512)], halo=None, outkv=True),
            dict(name='S0', T=1024, x=xs, x0=0, cond=1, rope=0, full=True, own0=512, kdst=(kTs, 0), vdst=(vs, 0),
                 segs=[(0, 1024)], halo=((xh, 0), (xs, 1024), 0), outkv=False),
            dict(name='S1', T=1024, x=xs, x0=1024, cond=1, rope=1024, full=True, own0=1536, kdst=(kTs, 1024), vdst=(vs, 1024),
                 segs=[(0, 1024)], halo=((xs, 1023), (xh, 1), 2), outkv=False),
            dict(name='O0', T=1024, x=xs, x0=2048, cond=1, rope=2048, full=False, own0=None, kdst=(kTs, 2048), vdst=(vs, 2048),
                 segs=None, halo=None, outkv=False),
            dict(name='O1', T=1024, x=xs, x0=3072, cond=1, rope=3072, full=False, own0=None, kdst=(kTs, 3072), vdst=(vs, 3072),
                 segs=None, halo=None, outkv=False),
        ]
        pes = ExitStack()
        sbp = lambda name, shape, dt: pes.enter_context(nc.sbuf_tensor(name + '_p2', list(shape), dt))
        ring = [sbp("ring%d" % i, [128, 8192], BF16) for i in range(4)]
        ws = WS(ring)
        hT = sbp("hT", [128, 16, 1024], BF16)
        hTh = sbp("hTh", [128, 16, 2], BF16)
        bc = [sbp("bc%d" % i, [128, D], F32) for i in range(2)]
        xt = [sbp("xt%d" % i, [128, D], F32) for i in range(2)]
        hb = sbp("hb", [128, D], BF16)
        stage = [sbp("stage%d" % i, [128, 512], BF16) for i in range(4)]
        stagef = [sbp("stagef%d" % i, [128, 512], F32) for i in range(3)]
        ropeC = sbp("ropeC", [128, 1024], F32)
        ropeS = sbp("ropeS", [128, 1024], F32)
        ccu = sbp("ccu", [128, 4, 1026], F32)
        yv = sbp("yv", [128, 1024], F32)
        convT = sbp("convT", [128, 8, 1024], BF16)
        hbv = sbp("sgcb", [128, 4, 1024], BF16)
        xh2 = sbp("xh2", [2, D], F32)

        def stg():
            i = stc[0] % len(stage)
            stc[0] += 1
            return i

        def stgf():
            i = stfc[0] % len(stagef)
            stfc[0] += 1
            return i
        xcnt = [0]

        for tl in tiles2:
            if only_tiles is not None and tl['name'] not in only_tiles:
                continue
            T = tl['T']
            nb = T // 128
            nm = T // 512
            cond = tl['cond']
            full = tl['full']
            load_bc(0, cond, 0, 'A1')
            load_bc(1, cond, 1, 'B1')
            if tl['rope'] is not None:
                r0 = tl['rope']
                s.dma('sp', [(ropeC[:, 0:T], ropec[:, r0:r0 + T])], writes=['ropeC'], semkey='ropeC')
                s.dma('sp', [(ropeS[:, 0:T], ropes[:, r0:r0 + T])], writes=['ropeS'], semkey='ropeS')
            for i in range(nb):
                xi = xcnt[0] % 2
                xcnt[0] += 1
                r = tl['x0'] + i * 128
                s.dma('sp', [(xt[xi][:], tl['x'][r:r + 128, :])], writes=[('xt', xi)], semkey=('xt', xi))
                norm_mod_transpose(xt[xi][:], ('xt', xi), 128,
                                   lambda half, i=i: (hT[:, half * 8:(half + 1) * 8, i * 128:(i + 1) * 128], [('hT', i, half)]),
                                   0, 1)
            hT_keys = [('hT', i, h) for i in range(nb) for h in range(2)]
            if tl['halo'] is not None:
                (lsrc, lrow), (rsrc, rrow), hmc = tl['halo']
                s.dma('sp', [(xh2[0:1, :], lsrc[lrow:lrow + 1, :]), (xh2[1:2, :], rsrc[rrow:rrow + 1, :])], writes=['xh2'], semkey='xh2')
                norm_mod_transpose(xh2[:], 'xh2', 2,
                                   lambda half: (hTh[:, half * 8:(half + 1) * 8, :], [('hTh', half)]), 0, 1)
            hTh_keys = [('hTh', 0), ('hTh', 1)]

            if full:
                order = [('q', c) for c in range(4)] + [('k', c) for c in range(4, 8)] + [('v', c) for c in range(8, 12)]
                for j in range(2):
                    order += [('cc', 14 + j), ('cx', 16 + j), ('cb', 12 + j)]
                order += [('ga', c) for c in range(18, 22)]
                for c in range(22, 26):
                    order += [('gc', c), ('wco', c - 22)]
            else:
                order = [('k', c) for c in range(4, 8)] + [('v', c) for c in range(8, 12)]
            if cut is not None:
                order = order[:cut]
            for kind, c in order:
                if kind == 'wco':
                    ws.add(wsrc(w_co, c * 512, 512), 8)
                else:
                    ws.add(wsrc(w_in, c * 512, 512), 16)
            sgc_stage = None
            for kind, c in order:
                slot, wv = ws.get()
                wkey = ('ring', slot)
                if kind in ('q', 'k'):
                    for sc in range(4):
                        prow = ((c % 4) * 4 + sc) * 128
                        for m in range(nm):
                            b = psn([0, 1, 2, 3])
                            s.op('pe', lambda pe, b=b, sc=sc, m=m, wv=wv: mm_group(
                                pe, ps[b][:], [(wv[:, k, sc * 128:(sc + 1) * 128], hT[:, k, m * 512:(m + 1) * 512]) for k in range(16)]),
                                reads=[wkey] + hT_keys, writes=[('ps', b)])
                            si = stg()
                            if tl['rope'] is None:
                                s.op('act', lambda a, b=b, si=si: a.copy(stage[si][:], ps[b][:]), reads=[('ps', b)], writes=[('stage', si)])
                            else:
                                sx = stg()
                                s.op('act', lambda a, b=b, sx=sx: a.copy(stage[sx][:], ps[b][:]), reads=[('ps', b)], writes=[('stage', sx)])
                                b2 = psn([4, 5])
                                s.op('pe', lambda pe, b2=b2, sx=sx: pe.matmul(ps[b2][:], lhsT=pmat[:], rhs=stage[sx][:], start=True, stop=True),
                                     reads=[('stage', sx), 'pmat'], writes=[('ps', b2)])
                                f1 = stgf()
                                f2 = stgf()
                                s.op('dve', lambda v, b=b, f1=f1, m=m: v.tensor_mul(stagef[f1][:], ps[b][:], ropeC[:, m * 512:(m + 1) * 512]),
                                     reads=[('ps', b), 'ropeC'], writes=[('stagef', f1)])
                                s.op('dve', lambda v, b2=b2, f2=f2, m=m: v.tensor_mul(stagef[f2][:], ps[b2][:], ropeS[:, m * 512:(m + 1) * 512]),
                                     reads=[('ps', b2), 'ropeS'], writes=[('stagef', f2)])
                                s.op('dve', lambda v, f1=f1, f2=f2, si=si: v.tensor_add(stage[si][:], stagef[f1][:], stagef[f2][:]),
                                     reads=[('stagef', f1), ('stagef', f2)], writes=[('stage', si)])
                            if kind == 'q':
                                c0 = tl['own0'] + m * 512
                                s.dma('sp', [(qT[prow:prow + 128, c0:c0 + 512], stage[si][:])], reads=[('stage', si)], semkey=('stq', si))
                            else:
                                kd, k0 = tl['kdst']
                                c0 = k0 + m * 512
                                s.dma('sp', [(kd[prow:prow + 128, c0:c0 + 512], stage[si][:])], reads=[('stage', si)], semkey=('stq', si))
                    if kind == 'k' and tl['outkv']:
                        for i in range(nb):
                            b = psn([0, 1, 2, 3])
                            s.op('pe', lambda pe, b=b, i=i, wv=wv: mm_group(
                                pe, ps[b][:], [(hT[:, k, i * 128:(i + 1) * 128], wv[:, k, :]) for k in range(16)]),
                                reads=[wkey] + hT_keys, writes=[('ps', b)])
                            f1 = stgf()
                            s.op('act', lambda a, b=b, f1=f1: a.copy(stagef[f1][:], ps[b][:]), reads=[('ps', b)], writes=[('stagef', f1)])
                            cc0 = (c - 4) * 512
                            s.dma('sp', [(nk[i * 128:(i + 1) * 128, cc0:cc0 + 512], stagef[f1][:])], reads=[('stagef', f1)], semkey=('stf', f1))
                elif kind == 'v':
                    vd, v0 = tl['vdst']
                    cc0 = (c - 8) * 512
                    for i in range(nb):
                        b = psn([0, 1, 2, 3])
                        s.op('pe', lambda pe, b=b, i=i, wv=wv: mm_group(
                            pe, ps[b][:], [(hT[:, k, i * 128:(i + 1) * 128], wv[:, k, :]) for k in range(16)]),
                            reads=[wkey] + hT_keys, writes=[('ps', b)])
                        si = stg()
                        r = v0 + i * 128
                        if tl['outkv']:
                            f1 = stgf()
                            s.op('act', lambda a, b=b, f1=f1: a.copy(stagef[f1][:], ps[b][:]), reads=[('ps', b)], writes=[('stagef', f1)])
                            s.dma('sp', [(nv[i * 128:(i + 1) * 128, cc0:cc0 + 512], stagef[f1][:])], reads=[('stagef', f1)], semkey=('stf', f1))
                            s.op('dve', lambda v, si=si, f1=f1: v.tensor_copy(stage[si][:], stagef[f1][:]), reads=[('stagef', f1)], writes=[('stage', si)])
                        else:
                            s.op('act', lambda a, b=b, si=si: a.copy(stage[si][:], ps[b][:]), reads=[('ps', b)], writes=[('stage', si)])
                        s.dma('sp', [(vd[r:r + 128, cc0:cc0 + 512], stage[si][:])], reads=[('stage', si)], semkey=('stq', si))
                elif kind in ('cc', 'cx'):
                    for sc in range(4):
                        for m in range(nm):
                            b = psn([0, 1, 2, 3])
                            s.op('pe', lambda pe, b=b, sc=sc, m=m, wv=wv: mm_group(
                                pe, ps[b][:], [(wv[:, k, sc * 128:(sc + 1) * 128], hT[:, k, m * 512:(m + 1) * 512]) for k in range(16)]),
                                reads=[wkey] + hT_keys, writes=[('ps', b)])
                            dst = ccu[:, sc, 1 + m * 512:1 + (m + 1) * 512]
                            if kind == 'cc':
                                s.op('act', lambda a, b=b, dst=dst: a.copy(dst, ps[b][:]), reads=[('ps', b)], writes=[('ccu', sc, m)])
                            else:
                                s.op('dve', lambda v, b=b, dst=dst: v.tensor_mul(dst, dst, ps[b][:]), reads=[('ps', b), ('ccu', sc, m)],
                                     writes=[('ccu', sc, m)])
                        for hc, col in ((0, 0), (1, T + 1)):
                            dsth = ccu[:, sc, col:col + 1]
                            if tl['halo'] is None:
                                if kind == 'cc':
                                    s.op('dve', lambda v, dsth=dsth: v.memset(dsth, 0.0), writes=[('ccuh', sc, hc)])
                                continue
                            b = psn([0, 1, 2, 3])
                            s.op('pe', lambda pe, b=b, sc=sc, hc=hc, wv=wv: mm_group(
                                pe, ps[b][:, 0:1], [(wv[:, k, sc * 128:(sc + 1) * 128], hTh[:, k, hc:hc + 1]) for k in range(16)]),
                                reads=[wkey] + hTh_keys, writes=[('ps', b)])
                            if kind == 'cc':
                                s.op('act', lambda a, b=b, dsth=dsth: a.copy(dsth, ps[b][:, 0:1]), reads=[('ps', b)], writes=[('ccuh', sc, hc)])
                            else:
                                mcol = tl['halo'][2] + hc
                                s.op('dve', lambda v, b=b, dsth=dsth, mcol=mcol: v.scalar_tensor_tensor(
                                    out=dsth, in0=ps[b][:, 0:1], scalar=hm[:, mcol:mcol + 1], in1=dsth, op0=ALU.mult, op1=ALU.mult),
                                    reads=[('ps', b), ('ccuh', sc, hc), 'hm'], writes=[('ccuh', sc, hc)])
                elif kind == 'cb':
                    j = c - 12
                    for sc in range(4):
                        ch = j * 4 + sc
                        ukeys = [('ccu', sc, m) for m in range(nm)] + [('ccuh', sc, 0), ('ccuh', sc, 1)]
                        u = ccu[:, sc, :]
                        first = True
                        for (a0, b0) in tl['segs']:
                            has_halo = tl['halo'] is not None
                            s.op('dve', lambda v, a0=a0, b0=b0, ch=ch, u=u: v.tensor_scalar(
                                out=yv[:, a0:b0], in0=u[:, 1 + a0:1 + b0], scalar1=cw[:, 1, ch:ch + 1], scalar2=None, op0=ALU.mult),
                                reads=ukeys + ['cw'], writes=['yv'])
                            la = a0 if has_halo else a0 + 1
                            s.op('dve', lambda v, la=la, b0=b0, ch=ch, u=u: v.scalar_tensor_tensor(
                                out=yv[:, la:b0], in0=u[:, la:b0], scalar=cw[:, 0, ch:ch + 1], in1=yv[:, la:b0], op0=ALU.mult, op1=ALU.add),
                                reads=ukeys + ['cw', 'yv'], writes=['yv'])
                            rb = b0 if has_halo else b0 - 1
                            s.op('dve', lambda v, a0=a0, rb=rb, ch=ch, u=u: v.scalar_tensor_tensor(
                                out=yv[:, a0:rb], in0=u[:, a0 + 2:rb + 2], scalar=cw[:, 2, ch:ch + 1], in1=yv[:, a0:rb], op0=ALU.mult, op1=ALU.add),
                                reads=ukeys + ['cw', 'yv'], writes=['yv'])
                        for m in range(nm):
                            b = psn([0, 1, 2, 3])
                            s.op('pe', lambda pe, b=b, sc=sc, m=m, wv=wv: mm_group(
                                pe, ps[b][:], [(wv[:, k, sc * 128:(sc + 1) * 128], hT[:, k, m * 512:(m + 1) * 512]) for k in range(16)]),
                                reads=[wkey] + hT_keys, writes=[('ps', b)])
                            s.op('dve', lambda v, b=b, ch=ch, m=m: v.tensor_mul(convT[:, ch, m * 512:(m + 1) * 512], ps[b][:], yv[:, m * 512:(m + 1) * 512]),
                                 reads=[('ps', b), 'yv'], writes=[('convT', ch, m)])
                elif kind in ('ga', 'gc'):
                    if kind == 'gc':
                        sgc_stage = {}
                    for sc in range(4):
                        drow = ((c - (18 if kind == 'ga' else 22)) * 4 + sc) * 128
                        for m in range(nm):
                            b = psn([0, 1, 2, 3])
                            s.op('pe', lambda pe, b=b, sc=sc, m=m, wv=wv: mm_group(
                                pe, ps[b][:], [(wv[:, k, sc * 128:(sc + 1) * 128], hT[:, k, m * 512:(m + 1) * 512]) for k in range(16)]),
                                reads=[wkey] + hT_keys, writes=[('ps', b)])
                            if kind == 'ga':
                                si = stg()
                                s.op('act', lambda a, b=b, si=si: a.activation(out=stage[si][:], in_=ps[b][:], func=AF.Sigmoid),
                                     reads=[('ps', b)], writes=[('stage', si)])
                                c0 = tl['own0'] + m * 512
                                s.dma('sp', [(sgaT[drow:drow + 128, c0:c0 + 512], stage[si][:])], reads=[('stage', si)], semkey=('stq', si))
                            else:
                                dst = hbv[:, sc, m * 512:(m + 1) * 512]
                                s.op('act', lambda a, b=b, dst=dst: a.activation(out=dst, in_=ps[b][:], func=AF.Sigmoid),
                                     reads=[('ps', b)], writes=[('sgc', sc, m)])
                elif kind == 'wco':
                    cvkeys = [('convT', ch, m) for ch in range(8) for m in range(nm)]
                    for sc in range(4):
                        drow = (c * 4 + sc) * 128
                        for m in range(nm):
                            b = psn([0, 1, 2, 3])
                            s.op('pe', lambda pe, b=b, sc=sc, m=m, wv=wv: mm_group(
                                pe, ps[b][:], [(wv[:, k, sc * 128:(sc + 1) * 128], convT[:, k, m * 512:(m + 1) * 512]) for k in range(8)]),
                                reads=[wkey] + cvkeys, writes=[('ps', b)])
                            si = stg()
                            s.op('dve', lambda v, b=b, si=si, sc=sc, m=m: v.tensor_mul(stage[si][:], ps[b][:], hbv[:, sc, m * 512:(m + 1) * 512]),
                                 reads=[('ps', b), ('sgc', sc, m)], writes=[('stage', si)])
                            c0 = tl['own0'] + m * 512
                            s.dma('sp', [(mcT[drow:drow + 128, c0:c0 + 512], stage[si][:])], reads=[('stage', si)], semkey=('stq', si))
            ws.finish()
        s.barrier()
        pes.close()
        if phases <= 2:
            return nc

        pes = ExitStack()
        sbp = lambda name, shape, dt: pes.enter_context(nc.sbuf_tensor(name + '_p3', list(shape), dt))
        NKMAX = 4096 + PAST
        kTh = [sbp("kTh%d" % i, [128, 2, NKMAX], BF16) for i in range(2)]
        vh = [sbp("vh%d" % i, [128, 32, VD], BF16) for i in range(2)]
        qh = [sbp("qh%d" % i, [128, 2, 2048], BF16) for i in range(2)]
        ckb = sbp("ckb", [128, 4, 2048], BF16)
        cvb = sbp("cvb", [128, 4, 2048], BF16)
        pT = [sbp("pT%d" % i, [128, 512], BF16) for i in range(4)]
        onrm = sbp("onrm", [128, 2, 2, 512], F32)
        rl = sbp("rl", [128, 512], F32)
        av = sbp("av", [128, 2, 512], F32)
        sq = sbp("sq", [128, 2, 512], BF16)
        rs3 = sbp("rs3", [128, 512], F32)
        ost = [sbp("ost%d" % i, [128, 512], BF16) for i in range(2)]
        zt = sbp("zt", [128, 4, D], BF16)
        s.op('dve', lambda v: v.memset(zt[:], 0.0), writes=['zt'])
        xsv = xsort.rearrange("(b i p) d -> b p i d", p=128, i=4)
        for b_ in range(NBLK):
            s.dma('sp', [(xsv[b_], zt[:])], reads=['zt'], semkey='ztst')
        s.dma('pool', [(ckb[:], ck.rearrange("(b p) n -> p b n", p=128))], writes=['ckb'], semkey='ckb')
        s.dma('pool', [(cvb[:], cv.rearrange("(b p) n -> p b n", p=128))], writes=['cvb'], semkey='cvb')
        SCALE = HD ** -0.5
        seqs = [dict(q0=0, nq=256, QT=256, ksrc=kTp, k0=0, vsrc=vp, nkb=2, cache=False),
                dict(q0=256, nq=256, QT=256, ksrc=kTp, k0=256, vsrc=vp, nkb=2, cache=False),
                dict(q0=512, nq=2048, QT=512, ksrc=kTs, k0=0, vsrc=vs, nkb=32, cache=True)]
        jobs = [(sq_, h) for sq_ in seqs for h in range(NH)]
        if only_tiles is not None:
            jobs = [jb for jb in jobs if (('P' in only_tiles and not jb[0]['cache']) or ('S0' in only_tiles and jb[0]['cache']))]
            if cut is not None:
                jobs = jobs[:cut]

        def attn_load(ji):
            sd, h = jobs[ji]
            bi = ji % 2
            nk_ = sd['nkb'] * 128
            s.dma('sp', [(kTh[bi][:, j, 0:nk_], sd['ksrc'][(j * NH + h) * 128:(j * NH + h + 1) * 128, sd['k0']:sd['k0'] + nk_]) for j in range(2)],
                  writes=[('kTh', bi)], semkey=('kTh', bi))
            s.dma('sp', [(vh[bi][:, 0:sd['nkb'], :], sd['vsrc'][sd['k0']:sd['k0'] + nk_, h * VD:(h + 1) * VD].rearrange("(kb p) e -> p kb e", p=128))],
                  writes=[('vh', bi)], semkey=('vh', bi))
            s.dma('sp', [(qh[bi][:, j, 0:sd['nq']], qT[(j * NH + h) * 128:(j * NH + h + 1) * 128, sd['q0']:sd['q0'] + sd['nq']]) for j in range(2)],
                  writes=[('qh', bi)], semkey=('qh', bi))

        pcnt = [0]
        ocnt = [0]
        if jobs:
            attn_load(0)
        for ji, (sd, h) in enumerate(jobs):
            bi = ji % 2
            if ji + 1 < len(jobs):
                attn_load(ji + 1)
            nkb = sd['nkb'] + (4 if sd['cache'] else 0)
            QT = sd['QT']
            if sd['cache']:
                b = psn([6, 7])
                pst = ps[b].bitcast(BF16)

                def trc(pe, pst=pst, h=h):
                    last = None
                    for j in range(2):
                        for blk in range(4):
                            c0 = (j * NH + h) * 128
                            last = pe.transpose(pst[:, (j * 4 + blk) * 128:(j * 4 + blk + 1) * 128], ckb[:, blk, c0:c0 + 128], ident[:])
                    return last
                s.op('pe', trc, reads=['ckb', 'ident'], writes=[('ps', b)])
                s.op('act', lambda a, pst=pst, bi=bi: a.copy(kTh[bi][:, :, 4096:4096 + PAST], pst.rearrange("p (j t) -> p j t", j=2)),
                     reads=[('ps', b), ('kTh', bi)], writes=[('kThc', bi)])
            kkeys = [('kTh', bi), ('kThc', bi)]

            def vblk(kb, half, bi=bi, sd=sd, h=h):
                if kb < sd['nkb']:
                    return vh[bi][:, kb, half * 128:(half + 1) * 128]
                return cvb[:, kb - sd['nkb'], h * VD + half * 128:h * VD + (half + 1) * 128]
            for qt in range(sd['nq'] // QT):
                qsl = slice(qt * QT, (qt + 1) * QT)
                for j in range(2):
                    accb = [0, 1, 2] if j == 0 else [3, 4, 5]

                    def emit_s(kb, j=j, qsl=qsl, bi=bi):
                        sbk = psn([6, 7])
                        s.op('pe', lambda pe: pe.matmul(ps[sbk][:, 0:QT], lhsT=kTh[bi][:, j, kb * 128:(kb + 1) * 128], rhs=qh[bi][:, j, qsl],
                                                        start=True, stop=True),
                             reads=kkeys + [('qh', bi)], writes=[('ps', sbk)])
                        pi = pcnt[0] % 4
                        pcnt[0] += 1
                        s.op('act', lambda a: a.activation(out=pT[pi][:, 0:QT], in_=ps[sbk][:, 0:QT], func=AF.Exp, scale=SCALE),
                             reads=[('ps', sbk)], writes=[('pT', pi)])
                        return pi
                    pis = {0: emit_s(0)}
                    for kb in range(nkb):
                        if kb + 1 < nkb:
                            pis[kb + 1] = emit_s(kb + 1)
                        pi = pis.pop(kb)

                        def pv(pe, kb=kb, pi=pi):
                            st_, sp_ = (kb == 0), (kb == nkb - 1)
                            pe.matmul(ps[accb[0]][:, 0:QT], lhsT=vblk(kb, 0), rhs=pT[pi][:, 0:QT], start=st_, stop=sp_)
                            pe.matmul(ps[accb[1]][:, 0:QT], lhsT=vblk(kb, 1), rhs=pT[pi][:, 0:QT], start=st_, stop=sp_)
                            return pe.matmul(ps[accb[2]][:, 0:QT], lhsT=ones_b[:], rhs=pT[pi][:, 0:QT], start=st_, stop=sp_)
                        s.op('pe', pv, reads=[('pT', pi), ('vh', bi), 'cvb', 'ones_b'], writes=[('ps', accb[0]), ('ps', accb[1]), ('ps', accb[2])])
                    s.op('dve', lambda v: v.reciprocal(rl[:, 0:QT], ps[accb[2]][:, 0:QT]), reads=[('ps', accb[2])], writes=['rl'])
                    for half in range(2):
                        s.op('dve', lambda v, half=half, j=j: v.tensor_mul(onrm[:, j, half, 0:QT], ps[accb[half]][:, 0:QT], rl[:, 0:QT]),
                             reads=[('ps', accb[half]), 'rl'], writes=[('onrm', j, half)])
                for half in range(2):
                    s.op('dve', lambda v, half=half: v.scalar_tensor_tensor(out=av[:, half, 0:QT], in0=onrm[:, 1, half, 0:QT], scalar=lam_t[:, 0:1],
                                                                            in1=onrm[:, 0, half, 0:QT], op0=ALU.mult, op1=ALU.add),
                         reads=[('onrm', 1, half), ('onrm', 0, half), 'lam_t'], writes=[('av', half)])
                    s.op('dve', lambda v, half=half: v.tensor_mul(sq[:, half, 0:QT], av[:, half, 0:QT], av[:, half, 0:QT]),
                         reads=[('av', half)], writes=[('sq', half)])
                sbk = psn([6, 7])
                s.op('pe', lambda pe, sbk=sbk: mm_group(pe, ps[sbk][:, 0:QT], [(ones_b[:], sq[:, hf, 0:QT]) for hf in range(2)]),
                     reads=[('sq', 0), ('sq', 1), 'ones_b'], writes=[('ps', sbk)])
                s.op('act', lambda a, sbk=sbk: a.activation(out=rs3[:, 0:QT], in_=ps[sbk][:, 0:QT], func=AF.Sqrt, bias=epsb[:, 0:1], scale=1.0 / VD),
                     reads=[('ps', sbk), 'epsb'], writes=['rs3'])
                s.op('dve', lambda v: v.reciprocal(rs3[:, 0:QT], rs3[:, 0:QT]), reads=['rs3'], writes=['rs3'])
                for half in range(2):
                    oi = ocnt[0] % 2
                    ocnt[0] += 1
                    s.op('dve', lambda v, half=half, oi=oi: v.scalar_tensor_tensor(out=ost[oi][:, 0:QT], in0=av[:, half, 0:QT], scalar=gsub[:, half:half + 1],
                                                                                   in1=rs3[:, 0:QT], op0=ALU.mult, op1=ALU.mult),
                         reads=[('av', half), 'rs3', 'gsub'], writes=[('ost', oi)])
                    r0 = h * VD + half * 128
                    c0 = sd['q0'] + qt * QT
                    s.dma('sp', [(attnT[r0:r0 + 128, c0:c0 + QT], ost[oi][:, 0:QT])], reads=[('ost', oi)], semkey=('ost', oi))
        s.barrier()
        pes.close()
        if phases <= 3:
            return nc

        combA = sb("combA", [128, NTB, NE], F32)
        selA = sb("selA", [128, NTB, NE], BF16)
        s.op('dve', lambda v: v.memset(combA[:], 0.0), writes=['combA0'])
        s.op('dve', lambda v: v.memset(selA[:], 0.0), writes=['selA0'])
        pes = ExitStack()
        sbp = lambda name, shape, dt: pes.enter_context(nc.sbuf_tensor(name + '_p4', list(shape), dt))
        ring = [sbp("ring%d" % i, [128, 8192], BF16) for i in range(4)]
        ws = WS(ring)
        big = sbp("big", [128, 8, D], F32)
        big_bf = big[:].rearrange("p a d -> p (a d)").bitcast(BF16)
        hT = sbp("hT", [128, 16, 1024], BF16)
        bc = [sbp("bc%d" % i, [128, D], F32) for i in range(2)]
        hb = sbp("hb", [128, D], BF16)
        stagef = [sbp("stagef%d" % i, [128, 512], F32) for i in range(1)]
        wr = sbp("wr", [128, 16, NE], BF16)
        rbias = sbp("rbias", [128, NE], F32)
        rt = sbp("rt", [128, 6, NE], F32)
        m8 = sbp("m8", [128, 8, 8], F32)
        g8 = sbp("g8", [128, 4, 8], F32)
        s.dma('pool', [(wr[:], w_router.rearrange("(k p) n -> p k n", p=128))], writes=['wr'], semkey='wr')
        s.dma('sp', [(rbias[:], router_bias.partition_broadcast(128))], writes=['rbias'], semkey='rbias')
        tiles4 = [dict(name='P', T=512, x=xp, x0=0, cond=0, own0=0, y=yp, y0=0),
                  dict(name='S0', T=1024, x=xs, x0=0, cond=1, own0=512, y=ys, y0=0),
                  dict(name='S1', T=1024, x=xs, x0=1024, cond=1, own0=1536, y=ys, y0=1024)]
        for tl in tiles4:
            if only_tiles is not None and tl['name'] not in only_tiles:
                continue
            T = tl['T']
            nb = T // 128
            nm = T // 512
            own0 = tl['own0']
            cond = tl['cond']
            ls = ExitStack()
            sbl = lambda name, shape, dt: ls.enter_context(nc.sbuf_tensor(name + '_4a' + tl['name'], list(shape), dt))
            gbuf = [sbl("gbuf%d" % i, [128, 1024], BF16) for i in range(2)]
            mbuf = [sbl("mbuf%d" % i, [128, 1024], BF16) for i in range(1)]
            aT = big_bf[:, 0:16 * T].rearrange("p (k t) -> p k t", k=16)
            s.dma('sp', [(aT, attnT[:, own0:own0 + T].rearrange("(k p) t -> p k t", p=128))], writes=['aT'], semkey='aT')
            for c in range(4):
                ws.add(wsrc(w_ao, c * 512, 512), 16)
            for c in range(4):
                ws.add(wsrc(w_o, c * 512, 512), 16)
            gcnt = 0
            for c in range(4):
                slot, wv = ws.get()
                for sc in range(4):
                    drow = (c * 4 + sc) * 128
                    gi = gcnt % 2
                    gcnt += 1
                    s.dma('sp', [(gbuf[gi][:, 0:T], sgaT[drow:drow + 128, own0:own0 + T])], writes=[('gbuf', gi)], semkey=('gbuf', gi))
                    s.dma('sp', [(mbuf[0][:, 0:T], mcT[drow:drow + 128, own0:own0 + T])], writes=[('mbuf', 0)], semkey=('mbuf', 0))
                    for m in range(nm):
                        b = psn([0, 1, 2, 3])
                        s.op('pe', lambda pe, b=b, sc=sc, m=m, wv=wv: mm_group(
                            pe, ps[b][:], [(wv[:, k, sc * 128:(sc + 1) * 128], aT[:, k, m * 512:(m + 1) * 512]) for k in range(16)]),
                            reads=[('ring', slot), 'aT'], writes=[('ps', b)])
                        f1 = stgf()
                        s.op('dve', lambda v, b=b, f1=f1, gi=gi, m=m: v.tensor_mul(stagef[f1][:], ps[b][:], gbuf[gi][:, m * 512:(m + 1) * 512]),
                             reads=[('ps', b), ('gbuf', gi)], writes=[('stagef', f1)])
                        s.op('dve', lambda v, f1=f1, gi=gi, m=m, c=c, sc=sc: v.tensor_add(hT[:, c * 4 + sc, m * 512:(m + 1) * 512], stagef[f1][:],
                                                                                      mbuf[0][:, m * 512:(m + 1) * 512]),
                             reads=[('stagef', f1), ('mbuf', 0)], writes=[('hT', c * 4 + sc, m)])
            s.barrier()
            ls.close()
            ls = ExitStack()
            sbl = lambda name, shape, dt: ls.enter_context(nc.sbuf_tensor(name + '_4c' + tl['name'], list(shape), dt))
            xt4 = [sbl("xt%d" % i, [128, D], F32) for i in range(1)]
            hbp = sbl("hbp", [128, D], BF16)
            for c in range(4):
                slot, wv = ws.get()
                for i in range(nb):
                    b = psn([0, 1, 2, 3])
                    s.op('pe', lambda pe, b=b, i=i, wv=wv: mm_group(
                        pe, ps[b][:], [(hT[:, k, i * 128:(i + 1) * 128], wv[:, k, :]) for k in range(16)]),
                        reads=[('ring', slot)], writes=[('ps', b)])
                    s.op('act', lambda a, b=b, i=i, c=c: a.copy(big[:, i, c * 512:(c + 1) * 512], ps[b][:]), reads=[('ps', b)], writes=[('o1', i)])
            ws.finish()
            s.barrier()
            load_bc(0, cond, 2, 'G1')
            for i in range(nb):
                r = tl['x0'] + i * 128
                s.dma('sp', [(xt4[0][:], tl['x'][r:r + 128, :])], writes=[('xt', 0)], semkey=('xt4', 0))
                s.op('act', lambda a, i=i: a.activation(out=hb[:], in_=big[:, i, :], func=AF.Square, accum_out=st1[:, 5:6]),
                     reads=[('o1', i)], writes=['hb', ('ss', 5)])
                rstd_from_ss(5)
                s.op('dve', lambda v, i=i: v.scalar_tensor_tensor(out=big[:, i, :], in0=big[:, i, :], scalar=st1[:, 5:6], in1=bc[0][:],
                                                                  op0=ALU.mult, op1=ALU.mult),
                     reads=[('ss', 5), ('bc', 0)], writes=[('o1', i)])
                s.op('dve', lambda v, i=i: v.tensor_add(big[:, i, :], big[:, i, :], xt4[0][:]), reads=[('xt', 0)], writes=[('o1', i)])
                s.dma('sp', [(x1d[own0 + i * 128:own0 + (i + 1) * 128, :], big[:, i, :])], reads=[('o1', i)], writes=[('x1d', i)], semkey=('x1s', i))
            load_bc(1, cond, 3, 'A2')
            load_bc(0, cond, 4, 'B2')
            for i in range(nb):
                norm_mod_transpose(big[:, i, :], ('o1', i), 128,
                                   lambda half, i=i: (hT[:, half * 8:(half + 1) * 8, i * 128:(i + 1) * 128], [('h2T', i, half)]), 1, 0,
                                   hb_store=h2d[own0 + i * 128:own0 + (i + 1) * 128, :])
                b = psn([4, 5])
                s.op('pe', lambda pe, b=b, i=i: mm_group(pe, ps[b][:, 0:NE], [(hT[:, k, i * 128:(i + 1) * 128], wr[:, k, :]) for k in range(16)]),
                     reads=['wr', ('h2T', i, 0), ('h2T', i, 1)], writes=[('ps', b)])
                sg_, bi_, mk_, sel_, w_, pen_ = [rt[:, q_, :] for q_ in range(6)]
                s.op('act', lambda a, b=b: a.activation(out=sg_, in_=ps[b][:, 0:NE], func=AF.Sigmoid), reads=[('ps', b)], writes=['rt_sg'])
                s.op('dve', lambda v: v.tensor_add(bi_, sg_, rbias[:]), reads=['rt_sg', 'rbias'], writes=['rt_bi'])
                for g in range(8):
                    s.op('dve', lambda v, g=g: v.max(out=m8[:, g, :], in_=bi_[:, g * 8:(g + 1) * 8]), reads=['rt_bi'], writes=[('m8', g)])
                s.op('dve', lambda v: v.tensor_add(g8[:, 0, :], m8[:, :, 0], m8[:, :, 1]), reads=[('m8', g) for g in range(8)], writes=['g8_0'])
                s.op('dve', lambda v: v.max(out=g8[:, 1, :], in_=g8[:, 0, :]), reads=['g8_0'], writes=['g8_1'])
                s.op('dve', lambda v: v.tensor_scalar(out=g8[:, 2, :], in0=g8[:, 0, :], scalar1=g8[:, 1, 3:4], scalar2=None, op0=ALU.is_ge),
                     reads=['g8_0', 'g8_1'], writes=['g8_2'])
                s.op('dve', lambda v: v.tensor_scalar(out=g8[:, 3, :], in0=g8[:, 2, :], scalar1=-1.0, scalar2=1e30, op0=ALU.add, op1=ALU.mult),
                     reads=['g8_2'], writes=['g8_3'])
                for g in range(8):
                    s.op('dve', lambda v, g=g: v.tensor_scalar(out=mk_[:, g * 8:(g + 1) * 8], in0=bi_[:, g * 8:(g + 1) * 8], scalar1=g8[:, 2, g:g + 1],
                                                               scalar2=g8[:, 3, g:g + 1], op0=ALU.mult, op1=ALU.add),
                         reads=['rt_bi', 'g8_2', 'g8_3'], writes=[('rt_mk', g)])
                s.op('dve', lambda v: v.max(out=g8[:, 1, :], in_=mk_), reads=[('rt_mk', g) for g in range(8)] + ['g8_2'], writes=['g8_1'])
                s.op('dve', lambda v: v.tensor_scalar(out=sel_, in0=mk_, scalar1=g8[:, 1, 7:8], scalar2=None, op0=ALU.is_ge),
                     reads=[('rt_mk', g) for g in range(8)] + ['g8_1'], writes=['rt_sel'])
                s.op('dve', lambda v: v.tensor_mul(w_, sg_, sel_), reads=['rt_sg', 'rt_sel'], writes=['rt_w'])
                s.op('dve', lambda v: v.reduce_sum(out=st1[:, 6:7], in_=w_, axis=mybir.AxisListType.X), reads=['rt_w'], writes=[('ss', 6)])
                s.op('dve', lambda v: v.reciprocal(st1[:, 6:7], st1[:, 6:7]), reads=[('ss', 6)], writes=[('ss', 6)])
                gi_ = own0 // 128 + i
                s.op('dve', lambda v, gi_=gi_: v.tensor_scalar(out=combA[:, gi_, :], in0=w_, scalar1=st1[:, 6:7], scalar2=ROUTED_SCALE, op0=ALU.mult, op1=ALU.mult),
                     reads=['rt_w', ('ss', 6)], writes=[('combA', gi_)])
                s.op('dve', lambda v, gi_=gi_: v.tensor_copy(selA[:, gi_, :], sel_), reads=['rt_sel'], writes=[('selA', gi_)])
            s.barrier()
            ls.close()
            ls = ExitStack()
            sbl = lambda name, shape, dt: ls.enter_context(nc.sbuf_tensor(name + '_4d' + tl['name'], list(shape), dt))
            hid = sbl("hid", [128, 4, 1024], BF16)
            ws.add(wsrc(w_sg, 0, DEXP), 16)
            ws.add(wsrc(w_su, 0, DEXP), 16)
            ws.add(w_sd.rearrange("(k p) n -> p k n", p=128), 4)
            (gslot, wg), (uslot, wu), (dslot, wd) = ws.get_group(3)
            for hc in range(4):
                for m in range(nm):
                    bg = psn([0, 1])
                    bu = psn([2, 3])
                    s.op('pe', lambda pe, bg=bg, hc=hc, m=m, wg=wg: mm_group(
                        pe, ps[bg][:], [(wg[:, k, hc * 128:(hc + 1) * 128], hT[:, k, m * 512:(m + 1) * 512]) for k in range(16)]),
                        reads=[('ring', gslot)], writes=[('ps', bg)])
                    s.op('pe', lambda pe, bu=bu, hc=hc, m=m, wu=wu: mm_group(
                        pe, ps[bu][:], [(wu[:, k, hc * 128:(hc + 1) * 128], hT[:, k, m * 512:(m + 1) * 512]) for k in range(16)]),
                        reads=[('ring', uslot)], writes=[('ps', bu)])
                    f1 = stgf()
                    s.op('act', lambda a, bg=bg, f1=f1: a.activation(out=stagef[f1][:], in_=ps[bg][:], func=AF.Silu),
                         reads=[('ps', bg)], writes=[('stagef', f1)])
                    s.op('dve', lambda v, bu=bu, f1=f1, hc=hc, m=m: v.tensor_mul(hid[:, hc, m * 512:(m + 1) * 512], stagef[f1][:], ps[bu][:]),
                         reads=[('ps', bu), ('stagef', f1)], writes=[('hid', hc, m)])
            hkeys = [('hid', hc, m) for hc in range(4) for m in range(nm)]
            for i in range(nb):
                for cb in range(4):
                    b = psn([4, 5, 6, 7])
                    s.op('pe', lambda pe, b=b, i=i, cb=cb, wd=wd: mm_group(
                        pe, ps[b][:], [(hid[:, hc, i * 128:(i + 1) * 128], wd[:, hc, cb * 512:(cb + 1) * 512]) for hc in range(4)]),
                        reads=[('ring', dslot)] + hkeys, writes=[('ps', b)])
                    s.op('act', lambda a, b=b, i=i, cb=cb: a.copy(big[:, i, cb * 512:(cb + 1) * 512], ps[b][:]), reads=[('ps', b)], writes=[('acc', i)])
                s.dma('sp', [(shd[own0 + i * 128:own0 + (i + 1) * 128, :], big[:, i, :])], reads=[('acc', i)], semkey=('x1s', i))
            ws.finish()
            s.barrier()
            ls.close()
        pes.close()
        if phases <= 4:
            return nc

        I32 = mybir.dt.int32
        pes = ExitStack()
        sbp = lambda name, shape, dt: pes.enter_context(nc.sbuf_tensor(name + '_p5', list(shape), dt))
        selb = selA
        ltri = sbp("ltri", [128, 128], BF16)
        ltf = sbp("ltf", [128, 128], F32)
        rankA = sbp("rankA", [128, NTB, NE], F32)
        cnt = sbp("cnt", [128, 6, NE], F32)
        cnti = sbp("cnti", [128, NE], I32)
        valt = sbp("valt", [128, NE], F32)
        oht = sbp("oht", [128, NE], F32)
        t8 = sbp("t8", [128, 8], F32)
        slotf = sbp("slotf", [128, NTB * 8], F32)
        sloti = sbp("sloti", [128, NTB * 8], I32)
        wk = sbp("wk", [128, NTB * 8], F32)
        ebf = sbp("ebf", [128, NBLK], F32)
        widx = sbp("widx", [128, NBLK], I32)
        ebp = sbp("ebp", [128, NBLK], F32)
        wdf = sbp("wdf", [128, NBLK, 4], F32)
        widxd = sbp("widxd", [128, NBLK * 4], I32)
        pk4 = sbp("pk4", [128, 4], F32)
        pcol_i = sbp("pcol_i", [128, 1], I32)
        pcol = sbp("pcol", [128, 1], F32)
        s.op('pool', lambda g: g.memset(ltf[:], 1.0), writes=['ltf'])
        s.op('pool', lambda g: g.affine_select(out=ltf[:], in_=ltf[:], pattern=[[1, 128]], compare_op=ALU.is_gt, fill=0.0, base=0,
                                               channel_multiplier=-1), reads=['ltf'], writes=['ltf'])
        s.op('dve', lambda v: v.tensor_copy(ltri[:], ltf[:]), reads=['ltf'], writes=['ltri'])
        s.op('pool', lambda g: g.iota(pcol_i[:], pattern=[[0, 1]], base=0, channel_multiplier=1), writes=['pcol_i'])
        s.op('dve', lambda v: v.tensor_copy(pcol[:], pcol_i[:]), reads=['pcol_i'], writes=['pcol'])
        for i in range(NTB):
            b = psn([0, 1, 2, 3])
            s.op('pe', lambda pe, b=b, i=i: mm_group(pe, ps[b][:, 0:NE], [(ltri[:], selb[:, i, :])] + [(ones_b[:], selb[:, i2, :]) for i2 in range(i)]),
                 reads=['selb', 'ltri', 'ones_b'], writes=[('ps', b)])
            s.op('act', lambda a, b=b, i=i: a.copy(rankA[:, i, :], ps[b][:, 0:NE]), reads=[('ps', b)], writes=[('rank', i)])
        b = psn([0, 1, 2, 3])
        s.op('pe', lambda pe, b=b: mm_group(pe, ps[b][:, 0:NE], [(ones_b[:], selb[:, i2, :]) for i2 in range(NTB)]),
             reads=['selb', 'ones_b'], writes=[('ps', b)])
        c_cnt, c_nb, c_a, c_b, c_bs, c_sb = [cnt[:, q_, :] for q_ in range(6)]
        s.op('act', lambda a, b=b: a.copy(c_cnt, ps[b][:, 0:NE]), reads=[('ps', b)], writes=['cnt'])
        s.op('dve', lambda v: v.tensor_scalar(out=c_nb, in0=c_cnt, scalar1=511.0, scalar2=1.0 / 512.0, op0=ALU.add, op1=ALU.mult), reads=['cnt'], writes=['nb'])
        s.op('dve', lambda v: v.tensor_scalar_add(c_nb, c_nb, -0.5 + 2.0 ** -11), reads=['nb'], writes=['nb'])
        s.op('dve', lambda v: v.tensor_copy(cnti[:], c_nb), reads=['nb'], writes=['cnti'])
        s.op('dve', lambda v: v.tensor_copy(c_nb, cnti[:]), reads=['cnti'], writes=['nb'])
        s.op('dve', lambda v: v.tensor_copy(c_a, c_nb), reads=['nb'], writes=['sa'])
        cur, nxt, kc, kn = c_a, c_b, 'sa', 'sb'
        for st_ in (1, 2, 4, 8, 16, 32):
            s.op('dve', lambda v, cur=cur, nxt=nxt, st_=st_: v.tensor_copy(nxt[:, 0:st_], cur[:, 0:st_]), reads=[kc], writes=[kn])
            s.op('dve', lambda v, cur=cur, nxt=nxt, st_=st_: v.tensor_add(nxt[:, st_:NE], cur[:, st_:NE], cur[:, 0:NE - st_]), reads=[kc, kn], writes=[kn])
            cur, nxt, kc, kn = nxt, cur, kn, kc
        s.op('dve', lambda v, cur=cur: v.tensor_sub(c_bs, cur, c_nb), reads=[kc, 'nb'], writes=['bs'])
        s.op('dve', lambda v: v.tensor_scalar(out=c_sb, in0=c_bs, scalar1=512.0, scalar2=1.0, op0=ALU.mult, op1=ALU.add), reads=['bs'], writes=['sbase'])
        for i in range(NTB):
            s.op('dve', lambda v, i=i: v.tensor_add(valt[:], rankA[:, i, :], c_sb), reads=[('rank', i), 'sbase'], writes=['valt'])
            s.op('dve', lambda v, i=i: v.tensor_mul(valt[:], valt[:], selA[:, i, :]), reads=['valt'], writes=['valt'])
            s.op('dve', lambda v: v.max(out=t8[:], in_=valt[:]), reads=['valt'], writes=['t8'])
            s.op('dve', lambda v, i=i: v.tensor_scalar_add(slotf[:, i * 8:(i + 1) * 8], t8[:], -1.0), reads=['t8'], writes=[('slotf', i)])
            for k in range(8):
                s.op('dve', lambda v, i=i, k=k: v.scalar_tensor_tensor(out=oht[:], in0=valt[:], scalar=t8[:, k:k + 1], in1=combA[:, i, :],
                                                                        op0=ALU.is_equal, op1=ALU.mult), reads=['valt', 't8'], writes=['oht'])
                s.op('dve', lambda v, i=i, k=k: v.reduce_sum(out=wk[:, i * 8 + k:i * 8 + k + 1], in_=oht[:], axis=mybir.AxisListType.X),
                     reads=['oht'], writes=[('wk', i)])
        s.op('dve', lambda v: v.tensor_copy(sloti[:], slotf[:]), reads=[('slotf', i) for i in range(NTB)], writes=['sloti'])
        for b_ in range(NBLK):
            s.op('dve', lambda v, b_=b_: v.tensor_scalar(out=oht[:], in0=c_bs, scalar1=float(b_), scalar2=None, op0=ALU.is_le), reads=['bs'], writes=['oht'])
            s.op('dve', lambda v, b_=b_: v.reduce_sum(out=ebf[:, b_:b_ + 1], in_=oht[:], axis=mybir.AxisListType.X), reads=['oht'], writes=[('ebf', b_)])
        s.op('dve', lambda v: v.tensor_scalar(out=ebf[:], in0=ebf[:], scalar1=-1.0, scalar2=128.0, op0=ALU.add, op1=ALU.mult),
             reads=[('ebf', b_) for b_ in range(NBLK)], writes=['ebf2'])
        s.op('dve', lambda v: v.tensor_scalar(out=ebp[:], in0=ebf[:], scalar1=pcol[:, 0:1], scalar2=None, op0=ALU.add), reads=['ebf2', 'pcol'], writes=['ebp'])
        s.op('dve', lambda v: v.tensor_copy(widx[:], ebp[:]), reads=['ebp'], writes=['widx'])
        for k in range(4):
            s.op('dve', lambda v, k=k: v.tensor_scalar_add(pk4[:, k:k + 1], pcol[:, 0:1], float(k * 128)), reads=['pcol'], writes=[('pk4', k)])
            s.op('dve', lambda v, k=k: v.tensor_scalar(out=wdf[:, :, k], in0=ebf[:], scalar1=4.0, scalar2=pk4[:, k:k + 1], op0=ALU.mult, op1=ALU.add),
                 reads=['ebf2', ('pk4', k)], writes=[('wdf', k)])
        s.op('dve', lambda v: v.tensor_copy(widxd[:], wdf[:].rearrange("p b k -> p (b k)")), reads=[('wdf', k) for k in range(4)], writes=['widxd'])
        hst = ExitStack()
        hrow = [hst.enter_context(nc.sbuf_tensor("hrow%d_p5" % i, [128, D], BF16)) for i in range(2)]

        def ind_dma(out, out_off, in_, in_off, bound, reads, writes, semkey):
            s._deps('pool', reads, writes)
            if semkey not in s.dsem:
                s.dsem[semkey] = [es.enter_context(nc.semaphore("dsem%d" % s.nsem)), 0]
                s.nsem += 1
            ent = s.dsem[semkey]
            nc.gpsimd.indirect_dma_start(out=out, out_offset=out_off, in_=in_, in_offset=in_off).then_inc(ent[0], 16)
            ent[1] += 16
            s._record((ent[0], ent[1]), reads, writes)

        tbs = list(range(NTB))
        if only_tiles is not None:
            tbs = ([0, 1, 2, 3] if 'P' in only_tiles else []) + (list(range(4, 12)) if 'S0' in only_tiles else []) + (list(range(12, 20)) if 'S1' in only_tiles else [])
        for i in tbs:
            hi = i % 2
            s.dma('sp', [(hrow[hi][:], h2d[i * 128:(i + 1) * 128, :])], writes=[('hrow', hi)], semkey=('hrow', hi))
            for k in range(8):
                ind_dma(xsort[:, :], bass.IndirectOffsetOnAxis(ap=sloti[:, i * 8 + k:i * 8 + k + 1], axis=0), hrow[hi][:, :], None, NSLOT - 1,
                        [('hrow', hi), 'sloti'], ['xsort'], ('hsc', hi))
        s.barrier()
        hst.close()
        NR5 = 6
        bst = ExitStack()
        sbq = lambda name, shape, dt: bst.enter_context(nc.sbuf_tensor(name + '_p5c', list(shape), dt))
        ring5 = [sbq("ring%d" % i, [128, 8192], BF16) for i in range(NR5)]
        xg = [sbq("xg%d" % i, [128, 4, D], BF16) for i in range(2)]
        xT = [sbq("xT%d" % i, [128, 16, 512], BF16) for i in range(2)]
        hid5 = [sbq("hid%d" % i, [128, 4, 512], BF16) for i in range(2)]
        sgf = [sbq("sgf%d" % i, [128, 512], F32) for i in range(2)]
        ob = [sbq("ob%d" % i, [128, D], F32) for i in range(1)]
        wgr = w_eg.rearrange("e (p k) n -> (e p) (k n)", k=16)
        wur = w_eu.rearrange("e (p k) n -> (e p) (k n)", k=16)
        wdr = w_ed.rearrange("e h n -> (e h) n")
        nblk_run = NBLK if nexp_dbg is None else nexp_dbg
        xsb = xsort.rearrange("(b i p) d -> b p i d", p=128, i=4)
        ysb = [y_.rearrange("(b i p) d -> b i p d", p=128, i=4) for y_ in ysorth]

        def blk_loads(b_):
            for j_, src in enumerate((wgr, wur)):
                slot = (b_ * 3 + j_) % NR5
                ind_dma(ring5[slot][:, :], None, src, bass.IndirectOffsetOnAxis(ap=widx[:, b_:b_ + 1], axis=0), NE * 128 - 1,
                        ['widx'], [('ring5', slot)], ('ring5', slot))
            slot = (b_ * 3 + 2) % NR5
            s._deps('pool', ['widxd'], [('ring5', slot)])
            for k in range(4):
                ind_dma(ring5[slot][:, k * 2048:(k + 1) * 2048], None, wdr, bass.IndirectOffsetOnAxis(ap=widxd[:, b_ * 4 + k:b_ * 4 + k + 1], axis=0), 0,
                        [], [], ('ring5', slot))
            s._record((s.dsem[('ring5', slot)][0], s.dsem[('ring5', slot)][1]), ['widxd'], [('ring5', slot)])
            s.dma('sp', [(xg[b_ % 2][:], xsb[b_])], writes=[('xg', b_ % 2)], semkey=('xg', b_ % 2))

        if nblk_run > 0:
            blk_loads(0)
        ecnt = 0
        ocnt5 = 0
        for b_ in range(nblk_run):
            if b_ + 1 < nblk_run:
                blk_loads(b_ + 1)
            bi = b_ % 2
            slots = [(b_ * 3 + j_) % NR5 for j_ in range(3)]
            wg = ring5[slots[0]][:].rearrange("p (k h m) -> p k h m", k=16, h=4)
            wu = ring5[slots[1]][:].rearrange("p (k h m) -> p k h m", k=16, h=4)
            wd = ring5[slots[2]][:].rearrange("p (k n) -> p k n", k=4)
            xgv = xg[bi][:].rearrange("p i (k q) -> p i k q", k=16)
            for kk in range(8):
                b = psn([6, 7])
                pst = ps[b].bitcast(BF16)

                def trx(pe, kk=kk, pst=pst, xgv=xgv):
                    last = None
                    for k2 in range(2):
                        for i4 in range(4):
                            last = pe.transpose(pst[:, k2 * 512 + i4 * 128:k2 * 512 + (i4 + 1) * 128], xgv[:, i4, kk * 2 + k2, :], ident[:])
                    return last
                s.op('pe', trx, reads=[('xg', bi), 'ident'], writes=[('ps', b)])
                eng = 'act' if ecnt % 2 == 0 else 'dve'
                ecnt += 1
                if eng == 'act':
                    s.op('act', lambda a, pst=pst, kk=kk, bi=bi: a.copy(xT[bi][:, kk * 2:kk * 2 + 2, :], pst.rearrange("p (k t) -> p k t", k=2)),
                         reads=[('ps', b)], writes=[('xT', bi, kk)])
                else:
                    s.op('dve', lambda v, pst=pst, kk=kk, bi=bi: v.tensor_copy(xT[bi][:, kk * 2:kk * 2 + 2, :], pst.rearrange("p (k t) -> p k t", k=2)),
                         reads=[('ps', b)], writes=[('xT', bi, kk)])
            xkeys = [('xT', bi, kk) for kk in range(8)]
            for hc in range(4):
                bg = psn([0, 1])
                bu = psn([2, 3])
                s.op('pe', lambda pe, bg=bg, hc=hc, wg=wg, bi=bi: mm_group(pe, ps[bg][:], [(wg[:, k, hc, :], xT[bi][:, k, :]) for k in range(16)]),
                     reads=[('ring5', slots[0])] + xkeys, writes=[('ps', bg)])
                s.op('pe', lambda pe, bu=bu, hc=hc, wu=wu, bi=bi: mm_group(pe, ps[bu][:], [(wu[:, k, hc, :], xT[bi][:, k, :]) for k in range(16)]),
                     reads=[('ring5', slots[1])] + xkeys, writes=[('ps', bu)])
                fi = hc % 2
                s.op('act', lambda a, bg=bg, fi=fi: a.activation(out=sgf[fi][:], in_=ps[bg][:], func=AF.Silu), reads=[('ps', bg)], writes=[('sgf', fi)])
                s.op('dve', lambda v, bu=bu, fi=fi, hc=hc, bi=bi: v.tensor_mul(hid5[bi][:, hc, :], sgf[fi][:], ps[bu][:]),
                     reads=[('ps', bu), ('sgf', fi)], writes=[('hid5', bi, hc)])
            hkeys = [('hid5', bi, hc) for hc in range(4)]
            for i4 in range(4):
                oi = 0
                for cb in range(4):
                    b = psn([4, 5])
                    s.op('pe', lambda pe, b=b, i4=i4, cb=cb, wd=wd, bi=bi: mm_group(
                        pe, ps[b][:], [(hid5[bi][:, hc, i4 * 128:(i4 + 1) * 128], wd[:, hc, cb * 512:(cb + 1) * 512]) for hc in range(4)]),
                        reads=[('ring5', slots[2])] + hkeys, writes=[('ps', b)])
                    if cb % 2 == 0:
                        s.op('act', lambda a, b=b, oi=oi, cb=cb: a.copy(ob[oi][:, cb * 512:(cb + 1) * 512], ps[b][:]), reads=[('ps', b)], writes=[('ob', oi)])
                    else:
                        s.op('dve', lambda v, b=b, oi=oi, cb=cb: v.tensor_copy(ob[oi][:, cb * 512:(cb + 1) * 512], ps[b][:]), reads=[('ps', b)], writes=[('ob', oi)])
                s.dma('sp', [(ysb[hf][b_, i4], ob[oi][:, hf * 1024:(hf + 1) * 1024]) for hf in range(2)], reads=[('ob', oi)], writes=['ysort'], semkey=('ob', oi))
        s.barrier()
        bst.close()
        sbp = lambda name, shape, dt: pes.enter_context(nc.sbuf_tensor(name + '_p6', list(shape), dt))
        bc = [sbp("bc%d" % i, [128, D], F32) for i in range(2)]
        hb = sbp("hb", [128, D], BF16)
        accb = [sbp("accb%d" % i, [128, D], F32) for i in range(2)]
        yg = [sbp("yg%d" % i, [128, D], F32) for i in range(3)]
        x1b = [sbp("x1b%d" % i, [128, D], F32) for i in range(2)]
        load_bc(0, 0, 5, 'G2')
        load_bc(1, 1, 5, 'G2')
        ygc = 0
        outs = [(yp, 0, 0)] * 4 + [(ys, 512, 1)] * 16
        for i in tbs:
            ai = i % 2
            ydst, yoff, cnd = outs[i]
            s.dma('sp', [(accb[ai][:], shd[i * 128:(i + 1) * 128, :])], writes=[('accb', ai)], semkey=('accb', ai))
            s.dma('sp', [(x1b[ai][:], x1d[i * 128:(i + 1) * 128, :])], writes=[('x1b', ai)], semkey=('x1b', ai))
            for k in range(8):
                gi_ = ygc % 3
                ygc += 1
                for hf in range(2):
                    ind_dma(yg[gi_][:, hf * 1024:(hf + 1) * 1024], None, ysorth[hf][:, :], bass.IndirectOffsetOnAxis(ap=sloti[:, i * 8 + k:i * 8 + k + 1], axis=0),
                            NSLOT - 1, ['sloti'], [('yg', gi_, hf)], ('yg', gi_, hf))
                s.op('dve', lambda v, ai=ai, gi_=gi_, i=i, k=k: v.scalar_tensor_tensor(out=accb[ai][:], in0=yg[gi_][:], scalar=wk[:, i * 8 + k:i * 8 + k + 1],
                                                                                   in1=accb[ai][:], op0=ALU.mult, op1=ALU.add),
                     reads=[('yg', gi_, 0), ('yg', gi_, 1)], writes=[('accb', ai)])
            s.op('act', lambda a, ai=ai: a.activation(out=hb[:], in_=accb[ai][:], func=AF.Square, accum_out=st1[:, 5:6]),
                 reads=[('accb', ai)], writes=['hb', ('ss', 5)])
            rstd_from_ss(5)
            s.op('dve', lambda v, ai=ai, cnd=cnd: v.scalar_tensor_tensor(out=accb[ai][:], in0=accb[ai][:], scalar=st1[:, 5:6], in1=bc[cnd][:],
                                                                         op0=ALU.mult, op1=ALU.mult),
                 reads=[('ss', 5), ('bc', cnd)], writes=[('accb', ai)])
            s.op('dve', lambda v, ai=ai: v.tensor_add(accb[ai][:], accb[ai][:], x1b[ai][:]), reads=[('x1b', ai)], writes=[('accb', ai)])
            r = i * 128 - yoff
            s.dma('sp', [(ydst[r:r + 128, :], accb[ai][:])], reads=[('accb', ai)], semkey=('yst', ai))
        s.barrier()
        pes.close()
        pes.close()
    return nc


def _prep_inputs(inp):
    f = lambda a: np.ascontiguousarray(np.asarray(a, dtype=np.float32))
    x_prompt = f(inp['x_prompt'])
    x_sample = f(inp['x_sample'])
    cache_k = f(inp['cache_k'])
    cache_v = f(inp['cache_v'])
    c = f(inp['c'])
    c_ctx = f(inp['c_ctx'])
    shared = {
        'w_ada': f(inp['w_ada'][0]), 'b_ada': f(inp['b_ada']), 'g_pre_mix': f(inp['g_pre_mix']), 'g_post_mix': f(inp['g_post_mix']),
        'w_in': f(inp['w_in'][0]), 'lq1': f(inp['lambda_q1']), 'lk1': f(inp['lambda_k1']), 'lq2': f(inp['lambda_q2']),
        'lk2': f(inp['lambda_k2']), 'g_subln': f(inp['g_subln']), 'conv_w': f(inp['conv_w'][0]), 'w_ao': f(inp['w_attn_out'][0]),
        'w_co': f(inp['w_conv_out'][0]), 'w_o': f(inp['w_o'][0]), 'g_pre_ffn': f(inp['g_pre_ffn']), 'g_post_ffn': f(inp['g_post_ffn']),
        'w_router': f(inp['w_router'][0]), 'router_bias': f(inp['router_bias']), 'w_eg': f(inp['w_exp_gate'][0]),
        'w_eu': f(inp['w_exp_up'][0]), 'w_ed': f(inp['w_exp_down'][0]), 'w_sg': f(inp['w_sh_gate'][0]), 'w_su': f(inp['w_sh_up'][0]),
        'w_sd': f(inp['w_sh_down'][0]),
    }
    maps = []
    for core in range(8):
        b = core // 2
        h = core % 2
        own = x_sample[b, h * 2048:(h + 1) * 2048]
        oth = x_sample[b, (1 - h) * 2048:(2 - h) * 2048]
        xh = np.zeros((2, D), np.float32)
        hmask = np.array([[0.0, 1.0, 1.0, 0.0]], np.float32)
        if h == 1:
            xh[0] = x_sample[b, 2047]
            hmask[0, 0] = 1.0
        else:
            xh[1] = x_sample[b, 2048]
            hmask[0, 3] = 1.0
        pidx_ = np.concatenate([np.arange(h * 2048, (h + 1) * 2048), np.arange((1 - h) * 2048, (2 - h) * 2048)])
        posv = np.ascontiguousarray(np.stack([pidx_ // 64, pidx_ % 64], axis=0).astype(np.float32))
        m = dict(shared)
        m.update({
            'xp': np.ascontiguousarray(x_prompt[2 * core:2 * core + 2].reshape(512, D)),
            'xs': np.ascontiguousarray(np.concatenate([own, oth], axis=0)),
            'xh': xh, 'pos': posv, 'hmask': hmask,
            'csel': np.ascontiguousarray(np.stack([c_ctx, c[b]], axis=0)),
            'ck': np.ascontiguousarray(cache_k[b, 0].reshape(PAST, 2048)),
            'cv': np.ascontiguousarray(cache_v[b, 0].reshape(PAST, 2048)),
        })
        maps.append(m)
    return maps


def kernel(**inputs):
    nc = build()
    maps = _prep_inputs(inputs)
    res = run_bass_kernel_spmd(nc, maps, core_ids=list(range(8)))
    r = res.results
    y_prompt = np.stack([r[c]['yp'].reshape(2, 256, D) for c in range(8)], axis=0).reshape(16, 256, D)
    y_sample = np.stack([r[c]['ys'] for c in range(8)], axis=0).reshape(4, 4096, D)
    nkk = np.stack([r[c]['nk'].reshape(2, 256, 2, NH, HD) for c in range(8)], axis=0).reshape(16, 1, 256, 2, NH, HD)
    nvv = np.stack([r[c]['nv'].reshape(2, 256, NH, VD) for c in range(8)], axis=0).reshape(16, 1, 256, NH, VD)
    return (y_prompt.astype(np.float32), y_sample.astype(np.float32), nkk.astype(np.float32), nvv.astype(np.float32))
```

```python
import math
import numpy as np
from contextlib import ExitStack
import concourse.bass as bass
import concourse.mybir as mybir
from concourse.bass_utils import run_bass_kernel_spmd

F32 = mybir.dt.float32
BF16 = mybir.dt.bfloat16
AF = mybir.ActivationFunctionType
ALU = mybir.AluOpType

D = 2048
NPROJ = 13312
NH = 8
HD = 128
VD = 256
DCONV = 1024
NE = 64
DEXP = 512
EPS = 1e-6
LAM_INIT = 0.8 - 0.6 * math.exp(-0.3 * 0)
ROUTED_SCALE = 2.5
ROPE_THETA = 10000.0
NOWN = 2560
PAST = 512


class Sched:
    def __init__(self, nc, es):
        self.nc = nc
        self.eng = {'pe': nc.tensor, 'act': nc.scalar, 'dve': nc.vector, 'pool': nc.gpsimd, 'sp': nc.sync}
        self.es = es
        self.esem = {e: es.enter_context(nc.semaphore("sem_" + e)) for e in self.eng}
        self.eseq = {e: 0 for e in self.eng}
        self.waited = {e: {} for e in self.eng}
        self.lastw = {}
        self.readers = {}
        self.dsem = {}
        self.bar = es.enter_context(nc.semaphore("sem_bar"))
        self.barc = 0
        self.nsem = 6

    def _deps(self, e, reads, writes):
        deps = {}

        def add(tok):
            if tok is None:
                return
            s, v = tok
            k = id(s)
            if k not in deps or deps[k][1] < v:
                deps[k] = (s, v)
        for r in reads:
            add(self.lastw.get(r))
            if isinstance(r, tuple) and r[0] == 'ps':
                for tok in self.readers.get(r, {}).values():
                    if tok[0] is not self.esem.get(e):
                        add(tok)
        for w in writes:
            add(self.lastw.get(w))
            for tok in self.readers.get(w, {}).values():
                add(tok)
        for k, (s, v) in deps.items():
            if e == 'pe' and s is self.esem['pe']:
                continue
            if self.waited[e].get(k, 0) >= v:
                continue
            self.eng[e].wait_ge(s, v)
            self.waited[e][k] = v

    def _record(self, tok, reads, writes):
        for r in reads:
            self.readers.setdefault(r, {})[id(tok[0])] = tok
        for w in writes:
            self.lastw[w] = tok
            self.readers[w] = {}

    def op(self, e, fn, reads=(), writes=()):
        self._deps(e, reads, writes)
        ins = fn(self.eng[e])
        self.eseq[e] += 1
        ins.then_inc(self.esem[e], 1)
        self._record((self.esem[e], self.eseq[e]), reads, writes)
        return ins

    def dma(self, q, pairs, reads=(), writes=(), semkey=None, **kw):
        self._deps(q, reads, writes)
        if semkey not in self.dsem:
            self.dsem[semkey] = [self.es.enter_context(self.nc.semaphore("dsem%d" % self.nsem)), 0]
            self.nsem += 1
        ent = self.dsem[semkey]
        for (o, i) in pairs:
            self.eng[q].dma_start(out=o, in_=i, **kw).then_inc(ent[0], 16)
            ent[1] += 16
        self._record((ent[0], ent[1]), reads, writes)

    def barrier(self):
        sp = self.eng['sp']
        for e in ('pe', 'act', 'dve', 'pool'):
            if self.eseq[e] > 0:
                sp.wait_ge(self.esem[e], self.eseq[e])
        for k, ent in self.dsem.items():
            if ent[1] > 0:
                sp.wait_ge(ent[0], ent[1])
        self.barc += 1
        sp.nop().then_inc(self.bar, 1)
        for e in ('pe', 'act', 'dve', 'pool'):
            self.eng[e].wait_ge(self.bar, self.barc)
        self.lastw = {}
        self.readers = {}


def mm_group(pe, out, pairs):
    n = len(pairs)
    last = None
    for i, (l, r) in enumerate(pairs):
        last = pe.matmul(out, lhsT=l, rhs=r, start=(i == 0), stop=(i == n - 1))
    return last


def build(phases=99, dbg=False, only_tiles=None, cut=None, nexp_dbg=None):
    nc = bass.Bass("TRN2", target_bir_lowering=False)

    def din(name, shape, dt=F32):
        return nc.dram_tensor(name, list(shape), dt, kind="ExternalInput").ap()

    def dout(name, shape, dt=F32):
        return nc.dram_tensor(name, list(shape), dt, kind="ExternalOutput").ap()

    def dscr(name, shape, dt):
        return nc.dram_tensor(name, list(shape), dt, kind="ExternalOutput" if dbg else "Internal").ap()

    xp = din("xp", [512, D])
    xs = din("xs", [4096, D])
    xh = din("xh", [2, D])
    pos = din("pos", [2, 4096])
    hmask = din("hmask", [1, 4])
    csel = din("csel", [2, D])
    ck = din("ck", [PAST, 2048])
    cv = din("cv", [PAST, 2048])
    w_ada = din("w_ada", [D, 6 * D])
    b_ada = din("b_ada", [1, 6 * D])
    g_pre_mix = din("g_pre_mix", [1, D])
    g_post_mix = din("g_post_mix", [1, D])
    w_in = din("w_in", [D, NPROJ])
    lq1 = din("lq1", [1, HD])
    lk1 = din("lk1", [1, HD])
    lq2 = din("lq2", [1, HD])
    lk2 = din("lk2", [1, HD])
    g_subln = din("g_subln", [1, VD])
    conv_w = din("conv_w", [3, DCONV])
    w_ao = din("w_ao", [2048, D])
    w_co = din("w_co", [DCONV, D])
    w_o = din("w_o", [D, D])
    g_pre_ffn = din("g_pre_ffn", [1, D])
    g_post_ffn = din("g_post_ffn", [1, D])
    w_router = din("w_router", [D, NE])
    router_bias = din("router_bias", [1, NE])
    if phases >= 4:
        w_eg = din("w_eg", [NE, D, DEXP])
        w_eu = din("w_eu", [NE, D, DEXP])
        w_ed = din("w_ed", [NE, DEXP, D])
    w_sg = din("w_sg", [D, DEXP])
    w_su = din("w_su", [D, DEXP])
    w_sd = din("w_sd", [DEXP, D])
    yp = dout("yp", [512, D])
    ys = dout("ys", [2048, D])
    nk = dout("nk", [512, 2048])
    nv = dout("nv", [512, 2048])
    modrows = dscr("modrows", [12, D], F32)
    qT = dscr("qT", [2048, NOWN], BF16)
    kTp = dscr("kTp", [2048, 512], BF16)
    kTs = dscr("kTs", [2048, 4096], BF16)
    vp = dscr("vp", [512, 2048], BF16)
    vs = dscr("vs", [4096, 2048], BF16)
    sgaT = dscr("sgaT", [2048, NOWN], BF16)
    mcT = dscr("mcT", [2048, NOWN], BF16)
    attnT = dscr("attnT", [2048, NOWN], BF16)
    x1d = dscr("x1d", [NOWN, D], F32)
    NBLK = (NOWN * 8) // 512 + NE
    NSLOT = NBLK * 512
    h2d = dscr("h2d", [NOWN, D], BF16)
    shd = dscr("shd", [NOWN, D], F32)
    xsort = dscr("xsort", [NSLOT, D], BF16)
    ysorth = [dscr("ysort%d" % i, [NSLOT, D // 2], F32) for i in range(2)]
    ropec = dscr("ropec", [128, 4096], F32)
    ropes = dscr("ropes", [128, 4096], F32)

    with ExitStack() as es:
        s = Sched(nc, es)

        def sb(name, shape, dt):
            return es.enter_context(nc.sbuf_tensor(name, list(shape), dt))

        ps = [es.enter_context(nc.psum_tensor("ps%d" % i, [128, 512], F32)) for i in range(8)]
        psc = {}

        def psn(pool):
            k = tuple(pool)
            c = psc.get(k, 0)
            psc[k] = c + 1
            return pool[c % len(pool)]

        ident_f = sb("ident_f", [128, 128], F32)
        ident = sb("ident", [128, 128], BF16)
        pmat = sb("pmat", [128, 128], BF16)
        ones_b = sb("ones_b", [128, 128], BF16)
        st1 = sb("st1", [128, 8], F32)
        negpi = sb("negpi", [128, 1], F32)
        epsb = sb("epsb", [128, 1], F32)
        lam_t = sb("lam_t", [128, 2], F32)
        gsub = sb("gsub", [128, 2], F32)
        cw = sb("cw", [128, 3, 8], F32)
        hm = sb("hm", [128, 4], F32)
        NTB = NOWN // 128
        stc = [0]
        stfc = [0]

        class WS:
            def __init__(self, ring):
                self.ring = ring
                self.NR = len(ring)
                self.items = []
                self.issued = 0
                self.consumed = 0
                self.base = 0

            def add(self, src, k):
                self.items.append((src, k))

            def _view(self, slot, src, k):
                n = src.shape[-1]
                return self.ring[slot][:, 0:k * n].rearrange("p (k n) -> p k n", k=k)

            def _issue(self, i):
                src, k = self.items[i]
                slot = (self.base + i) % self.NR
                s.dma('pool', [(self._view(slot, src, k), src)], writes=[('ring', slot)], semkey=('ring', slot))

            def get(self):
                lim = min(len(self.items), self.consumed + self.NR)
                while self.issued < lim:
                    self._issue(self.issued)
                    self.issued += 1
                slot = (self.base + self.consumed) % self.NR
                src, k = self.items[self.consumed]
                self.consumed += 1
                return slot, self._view(slot, src, k)

            def get_group(self, n):
                lim = min(len(self.items), self.consumed + self.NR)
                while self.issued < lim:
                    self._issue(self.issued)
                    self.issued += 1
                out = []
                for _ in range(n):
                    slot = (self.base + self.consumed) % self.NR
                    src, k = self.items[self.consumed]
                    assert self.consumed < self.issued
                    self.consumed += 1
                    out.append((slot, self._view(slot, src, k)))
                return out

            def finish(self):
                assert self.consumed == len(self.items), (self.consumed, len(self.items))
                self.base = (self.base + len(self.items)) % self.NR
                self.items = []
                self.issued = 0
                self.consumed = 0

        def wsrc(w2d, c0, n):
            return w2d[:, c0:c0 + n].rearrange("(k p) n -> p k n", p=128)

        s.op('pool', lambda g: g.memset(ident_f[:], 0.0), writes=['ident_f'])
        s.op('pool', lambda g: g.affine_select(out=ident_f[:], in_=ident_f[:], pattern=[[-1, 128]],
                                               compare_op=ALU.not_equal, fill=1.0, base=0, channel_multiplier=1),
             reads=['ident_f'], writes=['ident_f'])
        s.op('dve', lambda v: v.tensor_copy(ident[:], ident_f[:]), reads=['ident_f'], writes=['ident'])
        for (a, b) in ((0, 32), (32, 0), (64, 96), (96, 64)):
            s.op('dve', lambda v, a=a, b=b: v.tensor_copy(pmat[:, a:a + 32], ident_f[:, b:b + 32]),
                 reads=['ident_f'], writes=['pmat'])
        s.op('dve', lambda v: v.memset(ones_b[:], 1.0), writes=['ones_b'])
        s.op('dve', lambda v: v.memset(negpi[:], -math.pi), writes=['negpi'])
        s.op('dve', lambda v: v.memset(epsb[:], EPS), writes=['epsb'])

        pes = ExitStack()
        sbp = lambda name, shape, dt: pes.enter_context(nc.sbuf_tensor(name + '_p1', list(shape), dt))
        ring = [sbp("ring%d" % i, [128, 8192], BF16) for i in range(4)]
        ws = WS(ring)
        big = sbp("big1", [128, 12288], F32)
        tmpf = sbp("tmpf1", [128, 2 * HD], F32)
        scT = sbp("scT", [128, 16, 2], F32)
        scTb = sbp("scTb", [128, 16, 2], BF16)
        btl = [sbp("bt%d" % i, [2, 512], F32) for i in range(2)]
        tg = [sbp("tg%d" % i, [2, D], F32) for i in range(2)]
        tr_ = [sbp("tr%d" % i, [2, D], F32) for i in range(2)]
        s.dma('sp', [(scT[:, :, cc_], csel[cc_:cc_ + 1, :].rearrange("o (k p) -> p (o k)", p=128)) for cc_ in range(2)],
              writes=['scT'], semkey='scT', allow_slow_non_contiguous=True)
        s.op('act', lambda a: a.activation(out=scTb[:], in_=scT[:], func=AF.Silu), reads=['scT'], writes=['scTb'])
        modv = big[0:2, 0:6 * D]
        for n in range(24):
            ws.add(wsrc(w_ada, n * 512, 512), 16)
        for n in range(24):
            slot, wv = ws.get()
            b = psn([0, 1])
            bi = n % 2
            s.dma('sp', [(btl[bi][:], b_ada[0:1, n * 512:(n + 1) * 512].partition_broadcast(2))], writes=[('bt', bi)], semkey=('bt', bi))
            s.op('pe', lambda pe, wv=wv, b=b: mm_group(pe, ps[b][0:2, :], [(scTb[:, k, :], wv[:, k, :]) for k in range(16)]),
                 reads=[('ring', slot), 'scTb'], writes=[('ps', b)])
            s.op('dve', lambda v, n=n, b=b, bi=bi: v.tensor_add(modv[:, n * 512:(n + 1) * 512], ps[b][0:2, :], btl[bi][:]),
                 reads=[('ps', b), ('bt', bi)], writes=['modv'])
        ws.finish()
        mv = lambda i: modv[:, i * D:(i + 1) * D]
        mr3 = modrows.rearrange("(c i) d -> c i d", c=2)
        plan = [(0, 1, g_pre_mix, True), (1, 0, None, False), (2, 2, g_post_mix, False),
                (3, 4, g_pre_ffn, True), (4, 3, None, False), (5, 5, g_post_ffn, False)]
        for idx, (row, chunk, gv, plus1) in enumerate(plan):
            ti = idx % 2
            if gv is not None:
                s.dma('sp', [(tg[ti][:], gv.partition_broadcast(2))], writes=[('tg', ti)], semkey=('tg', ti))
                if plus1:
                    s.op('dve', lambda v, ti=ti, chunk=chunk: v.scalar_tensor_tensor(out=tr_[ti][:], in0=mv(chunk), scalar=1.0, in1=tg[ti][:],
                                                                                    op0=ALU.add, op1=ALU.mult),
                         reads=['modv', ('tg', ti)], writes=[('tr', ti)])
                else:
                    s.op('dve', lambda v, ti=ti, chunk=chunk: v.tensor_mul(tr_[ti][:], mv(chunk), tg[ti][:]),
                         reads=['modv', ('tg', ti)], writes=[('tr', ti)])
            else:
                s.op('dve', lambda v, ti=ti, chunk=chunk: v.tensor_copy(tr_[ti][:], mv(chunk)), reads=['modv'], writes=[('tr', ti)])
            s.dma('sp', [(mr3[:, row, :], tr_[ti][:])], reads=[('tr', ti)], semkey=('trs', ti))

        lt = sbp("lt", [128, 4, HD], F32)
        for i, l in enumerate((lq1, lk1, lq2, lk2)):
            s.dma('sp', [(lt[:, i, :], l.partition_broadcast(128))], writes=[('lt', i)], semkey=('lt', i))
        for j in range(2):
            s.op('dve', lambda v, j=j: v.tensor_tensor(out=tmpf[:, j * HD:(j + 1) * HD], in0=lt[:, 2 * j, :], in1=lt[:, 2 * j + 1, :], op=ALU.mult),
                 reads=[('lt', 2 * j), ('lt', 2 * j + 1)], writes=[('tmpf', j)])
            s.op('dve', lambda v, j=j: v.reduce_sum(out=st1[:, j:j + 1], in_=tmpf[:, j * HD:(j + 1) * HD], axis=mybir.AxisListType.X),
                 reads=[('tmpf', j)], writes=[('st1', j)])
        s.op('act', lambda a: a.activation(out=st1[:, 2:4], in_=st1[:, 0:2], func=AF.Exp), reads=[('st1', 0), ('st1', 1)], writes=['st1e'])
        s.op('dve', lambda v: v.tensor_sub(lam_t[:, 0:1], st1[:, 3:4], st1[:, 2:3]), reads=['st1e'], writes=['lam_t'])
        s.op('dve', lambda v: v.tensor_scalar_add(lam_t[:, 0:1], lam_t[:, 0:1], -LAM_INIT), reads=['lam_t'], writes=['lam_t'])
        s.dma('sp', [(gsub[:], g_subln.rearrange("o (h p) -> p (o h)", p=128))], writes=['gsub'], semkey='gsub',
              allow_slow_non_contiguous=True)
        s.op('dve', lambda v: v.tensor_scalar_mul(gsub[:], gsub[:], 1.0 - LAM_INIT), reads=['gsub'], writes=['gsub'])
        s.dma('sp', [(cw[:, i_, :], conv_w[i_:i_ + 1, :].rearrange("o (c p) -> p (o c)", p=128)) for i_ in range(3)],
              writes=['cw'], semkey='cw', allow_slow_non_contiguous=True)

        I32 = mybir.dt.int32
        ang = big[:, 0:4096]
        rt1 = big[:, 4096:8192]
        rtf = big[:, 8192:12288]
        rti = rtf.bitcast(I32)
        pidx = sbp("pidx", [128, 4], F32)
        io_i = sbp("io_i", [128, 128], I32)
        io_f = sbp("io_f", [128, 128], F32)
        s.op('pool', lambda g: g.iota(io_i[:], pattern=[[0, 4], [1, 32]], base=0, channel_multiplier=0), writes=['io_i'])
        s.op('dve', lambda v: v.tensor_copy(io_f[:], io_i[:]), reads=['io_i'], writes=['io_f'])
        s.op('dve', lambda v: v.tensor_mul(io_f[:], io_f[:], ident_f[:]), reads=['io_f', 'ident_f'], writes=['io_f'])
        s.op('dve', lambda v: v.reduce_sum(out=pidx[:, 1:2], in_=io_f[:], axis=mybir.AxisListType.X), reads=['io_f'], writes=['pidx1'])
        s.op('act', lambda a: a.activation(out=pidx[:, 2:3], in_=pidx[:, 1:2], func=AF.Exp, scale=-math.log(ROPE_THETA) / 32.0),
             reads=['pidx1'], writes=['freq'])
        s.op('pool', lambda g: g.iota(io_i[:], pattern=[[0, 2], [1, 2], [0, 32]], base=0, channel_multiplier=0), reads=['io_i'], writes=['io_i'])
        s.op('dve', lambda v: v.tensor_copy(io_f[:], io_i[:]), reads=['io_i', 'io_f'], writes=['io_f'])
        s.op('dve', lambda v: v.tensor_mul(io_f[:], io_f[:], ident_f[:]), reads=['io_f', 'ident_f'], writes=['io_f'])
        s.op('dve', lambda v: v.reduce_sum(out=pidx[:, 3:4], in_=io_f[:], axis=mybir.AxisListType.X), reads=['io_f'], writes=['sgn'])
        s.op('dve', lambda v: v.tensor_scalar(out=pidx[:, 3:4], in0=pidx[:, 3:4], scalar1=2.0, scalar2=-1.0, op0=ALU.mult, op1=ALU.add),
             reads=['sgn'], writes=['sgn'])
        s.dma('sp', [(ang[0:64, :], pos[0:1, :].partition_broadcast(64)), (ang[64:128, :], pos[1:2, :].partition_broadcast(64))],
              reads=['modv'], writes=['ang', 'modv'], semkey='posb')
        s.op('dve', lambda v: v.tensor_scalar_mul(ang, ang, pidx[:, 2:3]), reads=['ang', 'freq'], writes=['ang'])

        def sin_of(shift, dst_dram, signed, key):
            s.op('dve', lambda v: v.tensor_scalar(out=rtf, in0=ang, scalar1=shift, scalar2=1.0 / (2 * math.pi), op0=ALU.add, op1=ALU.mult),
                 reads=['ang'], writes=['rtf'])
            s.op('dve', lambda v: v.tensor_copy(rt1.bitcast(I32), rtf), reads=['rtf'], writes=['rt1'])
            s.op('dve', lambda v: v.tensor_copy(rtf, rt1.bitcast(I32)), reads=['rt1'], writes=['rtf'])
            s.op('dve', lambda v: v.scalar_tensor_tensor(out=rt1, in0=rtf, scalar=-2 * math.pi, in1=ang, op0=ALU.mult, op1=ALU.add),
                 reads=['rtf', 'ang'], writes=['rt1'])
            s.op('dve', lambda v: v.tensor_scalar(out=rt1, in0=rt1, scalar1=-3.1415925 - shift, scalar2=3.1415925 - shift, op0=ALU.max, op1=ALU.min),
                 reads=['rt1'], writes=['rt1'])
            s.op('act', lambda a: a.activation(out=rt1, in_=rt1, func=AF.Sin, bias=sh_t[:, key:key + 1]), reads=['rt1', 'sh_t'], writes=['rt1'])
            if signed:
                s.op('dve', lambda v: v.tensor_scalar_mul(rt1, rt1, pidx[:, 3:4]), reads=['rt1', 'sgn'], writes=['rt1'])
            s.dma('sp', [(dst_dram, rt1)], reads=['rt1'], writes=[('rope', key)], semkey=('rope', key))

        sh_t = sbp("sh_t", [128, 2], F32)
        s.op('dve', lambda v: v.memset(sh_t[:, 0:1], 0.0), writes=['sh_t'])
        s.op('dve', lambda v: v.memset(sh_t[:, 1:2], math.pi / 2), reads=['sh_t'], writes=['sh_t'])
        sin_of(0.0, ropes, True, 0)
        sin_of(math.pi / 2, ropec, False, 1)
        s.dma('sp', [(hm[:], hmask.partition_broadcast(128))], writes=['hm'], semkey='hm')
        s.barrier()
        pes.close()
        if phases <= 1:
            return nc

        def load_bc(i, cond, row, key):
            r = cond * 6 + row
            s.dma('sp', [(bc[i][:], modrows[r:r + 1, :].partition_broadcast(128))], writes=[('bc', i)], semkey=('bc', i))

        def rstd_from_ss(col, dim=D):
            s.op('act', lambda a: a.activation(out=st1[:, col:col + 1], in_=st1[:, col:col + 1], func=AF.Sqrt, bias=epsb[:, 0:1], scale=1.0 / dim),
                 reads=[('ss', col), 'epsb'], writes=[('ss', col)])
            s.op('dve', lambda v: v.reciprocal(st1[:, col:col + 1], st1[:, col:col + 1]), reads=[('ss', col)], writes=[('ss', col)])

        def norm_mod_transpose(src_ap, src_key, npart, dst_fn, a_bc, b_bc, hb_store=None):
            P = npart
            s.op('act', lambda a: a.activation(out=hb[0:P, :], in_=src_ap, func=AF.Square, accum_out=st1[0:P, 4:5]),
                 reads=[src_key], writes=['hb', ('ss', 4)])
            rstd_from_ss(4)
            s.op('dve', lambda v: v.scalar_tensor_tensor(out=src_ap, in0=src_ap, scalar=st1[0:P, 4:5], in1=bc[a_bc][0:P, :],
                                                          op0=ALU.mult, op1=ALU.mult),
                 reads=[('ss', 4), ('bc', a_bc)], writes=[src_key])
            s.op('dve', lambda v: v.tensor_add(hb[0:P, :], src_ap, bc[b_bc][0:P, :]), reads=[src_key, ('bc', b_bc)], writes=['hb'])
            if hb_store is not None:
                s.op('dve', lambda v: v.tensor_copy(hbp[0:P, :].rearrange("p (k q) -> p k q", k=16), hb[0:P, :].rearrange("p (q k) -> p k q", k=16)),
                     reads=['hb'], writes=['hbp'])
                s.dma('sp', [(hb_store, hbp[0:P, :])], reads=['hbp'], semkey='hbst')
            transpose16(hb, 'hb', P, dst_fn)

        def transpose16(src, src_key, P, dst_fn):
            for half in range(2):
                b = psn([6, 7])
                pst = ps[b].bitcast(BF16)

                def tr(pe, half=half, pst=pst):
                    last = None
                    for j in range(8):
                        k = half * 8 + j
                        last = pe.transpose(pst[:, j * 128:j * 128 + P], src[0:P, k * 128:(k + 1) * 128], ident[0:P, 0:P])
                    return last
                s.op('pe', tr, reads=[src_key, 'ident'], writes=[('ps', b)])
                dst, dkeys = dst_fn(half)
                s.op('act', lambda a, pst=pst, dst=dst: a.copy(dst, pst.rearrange("p (j t) -> p j t", j=8)[:, :, 0:P]),
                     reads=[('ps', b)], writes=dkeys)

        tiles2 = [
            dict(name='P', T=512, x=xp, x0=0, cond=0, rope=None, full=True, own0=0, kdst=(kTp, 0), vdst=(vp, 0),
                 segs=[(0, 256), (256, 512)], halo=None, outkv=True),
            dict(name='S0', T=1024, x=xs, x0=0, cond=1, rope=0, full=True, own0=512, kdst=(kTs, 0), vdst=(vs, 0),
                 segs=[(0, 1024)], halo=((xh, 0), (xs, 1024), 0), outkv=False),
            dict(name='S1', T=1024, x=xs, x0=1024, cond=1, rope=1024, full=True, own0=1536, kdst=(kTs, 1024), vdst=(vs, 1024),
                 segs=[(0, 1024)], halo=((xs, 1023), (xh, 1), 2), outkv=False),
            dict(name='O0', T=1024, x=xs, x0=2048, cond=1, rope=2048, full=False, own0=None, kdst=(kTs, 2048), vdst=(vs, 2048),
                 segs=None, halo=None, outkv=False),
            dict(name='O1', T=1024, x=xs, x0=3072, cond=1, rope=3072, full=False, own0=None, kdst=(kTs, 3072), vdst=(vs, 3072),
                 segs=None, halo=None, outkv=False),
        ]
        pes = ExitStack()
        sbp = lambda name, shape, dt: pes.enter_context(nc.sbuf_tensor(name + '_p2', list(shape), dt))
        ring = [sbp("ring%d" % i, [128, 8192], BF16) for i in range(4)]
        ws = WS(ring)
        hT = sbp("hT", [128, 16, 1024], BF16)
        hTh = sbp("hTh", [128, 16, 2], BF16)
        bc = [sbp("bc%d" % i, [128, D], F32) for i in range(2)]
        xt = [sbp("xt%d" % i, [128, D], F32) for i in range(2)]
        hb = sbp("hb", [128, D], BF16)
        stage = [sbp("stage%d" % i, [128, 512], BF16) for i in range(4)]
        stagef = [sbp("stagef%d" % i, [128, 512], F32) for i in range(3)]
        ropeC = sbp("ropeC", [128, 1024], F32)
        ropeS = sbp("ropeS", [128, 1024], F32)
        ccu = sbp("ccu", [128, 4, 1026], F32)
        yv = sbp("yv", [128, 1024], F32)
        convT = sbp("convT", [128, 8, 1024], BF16)
        hbv = sbp("sgcb", [128, 4, 1024], BF16)
        xh2 = sbp("xh2", [2, D], F32)

        def stg():
            i = stc[0] % len(stage)
            stc[0] += 1
            return i

        def stgf():
            i = stfc[0] % len(stagef)
            stfc[0] += 1
            return i
        xcnt = [0]

        for tl in tiles2:
            if only_tiles is not None and tl['name'] not in only_tiles:
                continue
            T = tl['T']
            nb = T // 128
            nm = T // 512
            cond = tl['cond']
            full = tl['full']
            load_bc(0, cond, 0, 'A1')
            load_bc(1, cond, 1, 'B1')
            if tl['rope'] is not None:
                r0 = tl['rope']
                s.dma('sp', [(ropeC[:, 0:T], ropec[:, r0:r0 + T])], writes=['ropeC'], semkey='ropeC')
                s.dma('sp', [(ropeS[:, 0:T], ropes[:, r0:r0 + T])], writes=['ropeS'], semkey='ropeS')
            for i in range(nb):
                xi = xcnt[0] % 2
                xcnt[0] += 1
                r = tl['x0'] + i * 128
                s.dma('sp', [(xt[xi][:], tl['x'][r:r + 128, :])], writes=[('xt', xi)], semkey=('xt', xi))
                norm_mod_transpose(xt[xi][:], ('xt', xi), 128,
                                   lambda half, i=i: (hT[:, half * 8:(half + 1) * 8, i * 128:(i + 1) * 128], [('hT', i, half)]),
                                   0, 1)
            hT_keys = [('hT', i, h) for i in range(nb) for h in range(2)]
            if tl['halo'] is not None:
                (lsrc, lrow), (rsrc, rrow), hmc = tl['halo']
                s.dma('sp', [(xh2[0:1, :], lsrc[lrow:lrow + 1, :]), (xh2[1:2, :], rsrc[rrow:rrow + 1, :])], writes=['xh2'], semkey='xh2')
                norm_mod_transpose(xh2[:], 'xh2', 2,
                                   lambda half: (hTh[:, half * 8:(half + 1) * 8, :], [('hTh', half)]), 0, 1)
            hTh_keys = [('hTh', 0), ('hTh', 1)]

            if full:
                order = [('q', c) for c in range(4)] + [('k', c) for c in range(4, 8)] + [('v', c) for c in range(8, 12)]
                for j in range(2):
                    order += [('cc', 14 + j), ('cx', 16 + j), ('cb', 12 + j)]
                order += [('ga', c) for c in range(18, 22)]
                for c in range(22, 26):
                    order += [('gc', c), ('wco', c - 22)]
            else:
                order = [('k', c) for c in range(4, 8)] + [('v', c) for c in range(8, 12)]
            if cut is not None:
                order = order[:cut]
            for kind, c in order:
                if kind == 'wco':
                    ws.add(wsrc(w_co, c * 512, 512), 8)
                else:
                    ws.add(wsrc(w_in, c * 512, 512), 16)
            sgc_stage = None
            for kind, c in order:
                slot, wv = ws.get()
                wkey = ('ring', slot)
                if kind in ('q', 'k'):
                    for sc in range(4):
                        prow = ((c % 4) * 4 + sc) * 128
                        for m in range(nm):
                            b = psn([0, 1, 2, 3])
                            s.op('pe', lambda pe, b=b, sc=sc, m=m, wv=wv: mm_group(
                                pe, ps[b][:], [(wv[:, k, sc * 128:(sc + 1) * 128], hT[:, k, m * 512:(m + 1) * 512]) for k in range(16)]),
                                reads=[wkey] + hT_keys, writes=[('ps', b)])
                            si = stg()
                            if tl['rope'] is None:
                                s.op('act', lambda a, b=b, si=si: a.copy(stage[si][:], ps[b][:]), reads=[('ps', b)], writes=[('stage', si)])
                            else:
                                sx = stg()
                                s.op('act', lambda a, b=b, sx=sx: a.copy(stage[sx][:], ps[b][:]), reads=[('ps', b)], writes=[('stage', sx)])
                                b2 = psn([4, 5])
                                s.op('pe', lambda pe, b2=b2, sx=sx: pe.matmul(ps[b2][:], lhsT=pmat[:], rhs=stage[sx][:], start=True, stop=True),
                                     reads=[('stage', sx), 'pmat'], writes=[('ps', b2)])
                                f1 = stgf()
                                f2 = stgf()
                                s.op('dve', lambda v, b=b, f1=f1, m=m: v.tensor_mul(stagef[f1][:], ps[b][:], ropeC[:, m * 512:(m + 1) * 512]),
                                     reads=[('ps', b), 'ropeC'], writes=[('stagef', f1)])
                                s.op('dve', lambda v, b2=b2, f2=f2, m=m: v.tensor_mul(stagef[f2][:], ps[b2][:], ropeS[:, m * 512:(m + 1) * 512]),
                                     reads=[('ps', b2), 'ropeS'], writes=[('stagef', f2)])
                                s.op('dve', lambda v, f1=f1, f2=f2, si=si: v.tensor_add(stage[si][:], stagef[f1][:], stagef[f2][:]),
                                     reads=[('stagef', f1), ('stagef', f2)], writes=[('stage', si)])
                            if kind == 'q':
                                c0 = tl['own0'] + m * 512
                                s.dma('sp', [(qT[prow:prow + 128, c0:c0 + 512], stage[si][:])], reads=[('stage', si)], semkey=('stq', si))
                            else:
                                kd, k0 = tl['kdst']
                                c0 = k0 + m * 512
                                s.dma('sp', [(kd[prow:prow + 128, c0:c0 + 512], stage[si][:])], reads=[('stage', si)], semkey=('stq', si))
                    if kind == 'k' and tl['outkv']:
                        for i in range(nb):
                            b = psn([0, 1, 2, 3])
                            s.op('pe', lambda pe, b=b, i=i, wv=wv: mm_group(
                                pe, ps[b][:], [(hT[:, k, i * 128:(i + 1) * 128], wv[:, k, :]) for k in range(16)]),
                                reads=[wkey] + hT_keys, writes=[('ps', b)])
                            f1 = stgf()
                            s.op('act', lambda a, b=b, f1=f1: a.copy(stagef[f1][:], ps[b][:]), reads=[('ps', b)], writes=[('stagef', f1)])
                            cc0 = (c - 4) * 512
                            s.dma('sp', [(nk[i * 128:(i + 1) * 128, cc0:cc0 + 512], stagef[f1][:])], reads=[('stagef', f1)], semkey=('stf', f1))
                elif kind == 'v':
                    vd, v0 = tl['vdst']
                    cc0 = (c - 8) * 512
                    for i in range(nb):
                        b = psn([0, 1, 2, 3])
                        s.op('pe', lambda pe, b=b, i=i, wv=wv: mm_group(
                            pe, ps[b][:], [(hT[:, k, i * 128:(i + 1) * 128], wv[:, k, :]) for k in range(16)]),
                            reads=[wkey] + hT_keys, writes=[('ps', b)])
                        si = stg()
                        r = v0 + i * 128
                        if tl['outkv']:
                            f1 = stgf()
                            s.op('act', lambda a, b=b, f1=f1: a.copy(stagef[f1][:], ps[b][:]), reads=[('ps', b)], writes=[('stagef', f1)])
                            s.dma('sp', [(nv[i * 128:(i + 1) * 128, cc0:cc0 + 512], stagef[f1][:])], reads=[('stagef', f1)], semkey=('stf', f1))
                            s.op('dve', lambda v, si=si, f1=f1: v.tensor_copy(stage[si][:], stagef[f1][:]), reads=[('stagef', f1)], writes=[('stage', si)])
                        else:
                            s.op('act', lambda a, b=b, si=si: a.copy(stage[si][:], ps[b][:]), reads=[('ps', b)], writes=[('stage', si)])
                        s.dma('sp', [(vd[r:r + 128, cc0:cc0 + 512], stage[si][:])], reads=[('stage', si)], semkey=('stq', si))
                elif kind in ('cc', 'cx'):
                    for sc in range(4):
                        for m in range(nm):
                            b = psn([0, 1, 2, 3])
                            s.op('pe', lambda pe, b=b, sc=sc, m=m, wv=wv: mm_group(
                                pe, ps[b][:], [(wv[:, k, sc * 128:(sc + 1) * 128], hT[:, k, m * 512:(m + 1) * 512]) for k in range(16)]),
                                reads=[wkey] + hT_keys, writes=[('ps', b)])
                            dst = ccu[:, sc, 1 + m * 512:1 + (m + 1) * 512]
                            if kind == 'cc':
                                s.op('act', lambda a, b=b, dst=dst: a.copy(dst, ps[b][:]), reads=[('ps', b)], writes=[('ccu', sc, m)])
                            else:
                                s.op('dve', lambda v, b=b, dst=dst: v.tensor_mul(dst, dst, ps[b][:]), reads=[('ps', b), ('ccu', sc, m)],
                                     writes=[('ccu', sc, m)])
                        for hc, col in ((0, 0), (1, T + 1)):
                            dsth = ccu[:, sc, col:col + 1]
                            if tl['halo'] is None:
                                if kind == 'cc':
                                    s.op('dve', lambda v, dsth=dsth: v.memset(dsth, 0.0), writes=[('ccuh', sc, hc)])
                                continue
                            b = psn([0, 1, 2, 3])
                            s.op('pe', lambda pe, b=b, sc=sc, hc=hc, wv=wv: mm_group(
                                pe, ps[b][:, 0:1], [(wv[:, k, sc * 128:(sc + 1) * 128], hTh[:, k, hc:hc + 1]) for k in range(16)]),
                                reads=[wkey] + hTh_keys, writes=[('ps', b)])
                            if kind == 'cc':
                                s.op('act', lambda a, b=b, dsth=dsth: a.copy(dsth, ps[b][:, 0:1]), reads=[('ps', b)], writes=[('ccuh', sc, hc)])
                            else:
                                mcol = tl['halo'][2] + hc
                                s.op('dve', lambda v, b=b, dsth=dsth, mcol=mcol: v.scalar_tensor_tensor(
                                    out=dsth, in0=ps[b][:, 0:1], scalar=hm[:, mcol:mcol + 1], in1=dsth, op0=ALU.mult, op1=ALU.mult),
                                    reads=[('ps', b), ('ccuh', sc, hc), 'hm'], writes=[('ccuh', sc, hc)])
                elif kind == 'cb':
                    j = c - 12
                    for sc in range(4):
                        ch = j * 4 + sc
                        ukeys = [('ccu', sc, m) for m in range(nm)] + [('ccuh', sc, 0), ('ccuh', sc, 1)]
                        u = ccu[:, sc, :]
                        first = True
                        for (a0, b0) in tl['segs']:
                            has_halo = tl['halo'] is not None
                            s.op('dve', lambda v, a0=a0, b0=b0, ch=ch, u=u: v.tensor_scalar(
                                out=yv[:, a0:b0], in0=u[:, 1 + a0:1 + b0], scalar1=cw[:, 1, ch:ch + 1], scalar2=None, op0=ALU.mult),
                                reads=ukeys + ['cw'], writes=['yv'])
                            la = a0 if has_halo else a0 + 1
                            s.op('dve', lambda v, la=la, b0=b0, ch=ch, u=u: v.scalar_tensor_tensor(
                                out=yv[:, la:b0], in0=u[:, la:b0], scalar=cw[:, 0, ch:ch + 1], in1=yv[:, la:b0], op0=ALU.mult, op1=ALU.add),
                                reads=ukeys + ['cw', 'yv'], writes=['yv'])
                            rb = b0 if has_halo else b0 - 1
                            s.op('dve', lambda v, a0=a0, rb=rb, ch=ch, u=u: v.scalar_tensor_tensor(
                                out=yv[:, a0:rb], in0=u[:, a0 + 2:rb + 2], scalar=cw[:, 2, ch:ch + 1], in1=yv[:, a0:rb], op0=ALU.mult, op1=ALU.add),
                                reads=ukeys + ['cw', 'yv'], writes=['yv'])
                        for m in range(nm):
                            b = psn([0, 1, 2, 3])
                            s.op('pe', lambda pe, b=b, sc=sc, m=m, wv=wv: mm_group(
                                pe, ps[b][:], [(wv[:, k, sc * 128:(sc + 1) * 128], hT[:, k, m * 512:(m + 1) * 512]) for k in range(16)]),
                                reads=[wkey] + hT_keys, writes=[('ps', b)])
                            s.op('dve', lambda v, b=b, ch=ch, m=m: v.tensor_mul(convT[:, ch, m * 512:(m + 1) * 512], ps[b][:], yv[:, m * 512:(m + 1) * 512]),
                                 reads=[('ps', b), 'yv'], writes=[('convT', ch, m)])
                elif kind in ('ga', 'gc'):
                    if kind == 'gc':
                        sgc_stage = {}
                    for sc in range(4):
                        drow = ((c - (18 if kind == 'ga' else 22)) * 4 + sc) * 128
                        for m in range(nm):
                            b = psn([0, 1, 2, 3])
                            s.op('pe', lambda pe, b=b, sc=sc, m=m, wv=wv: mm_group(
                                pe, ps[b][:], [(wv[:, k, sc * 128:(sc + 1) * 128], hT[:, k, m * 512:(m + 1) * 512]) for k in range(16)]),
                                reads=[wkey] + hT_keys, writes=[('ps', b)])
                            if kind == 'ga':
                                si = stg()
                                s.op('act', lambda a, b=b, si=si: a.activation(out=stage[si][:], in_=ps[b][:], func=AF.Sigmoid),
                                     reads=[('ps', b)], writes=[('stage', si)])
                                c0 = tl['own0'] + m * 512
                                s.dma('sp', [(sgaT[drow:drow + 128, c0:c0 + 512], stage[si][:])], reads=[('stage', si)], semkey=('stq', si))
                            else:
                                dst = hbv[:, sc, m * 512:(m + 1) * 512]
                                s.op('act', lambda a, b=b, dst=dst: a.activation(out=dst, in_=ps[b][:], func=AF.Sigmoid),
                                     reads=[('ps', b)], writes=[('sgc', sc, m)])
                elif kind == 'wco':
                    cvkeys = [('convT', ch, m) for ch in range(8) for m in range(nm)]
                    for sc in range(4):
                        drow = (c * 4 + sc) * 128
                        for m in range(nm):
                            b = psn([0, 1, 2, 3])
                            s.op('pe', lambda pe, b=b, sc=sc, m=m, wv=wv: mm_group(
                                pe, ps[b][:], [(wv[:, k, sc * 128:(sc + 1) * 128], convT[:, k, m * 512:(m + 1) * 512]) for k in range(8)]),
                                reads=[wkey] + cvkeys, writes=[('ps', b)])
                            si = stg()
                            s.op('dve', lambda v, b=b, si=si, sc=sc, m=m: v.tensor_mul(stage[si][:], ps[b][:], hbv[:, sc, m * 512:(m + 1) * 512]),
                                 reads=[('ps', b), ('sgc', sc, m)], writes=[('stage', si)])
                            c0 = tl['own0'] + m * 512
                            s.dma('sp', [(mcT[drow:drow + 128, c0:c0 + 512], stage[si][:])], reads=[('stage', si)], semkey=('stq', si))
            ws.finish()
        s.barrier()
        pes.close()
        if phases <= 2:
            return nc

        pes = ExitStack()
        sbp = lambda name, shape, dt: pes.enter_context(nc.sbuf_tensor(name + '_p3', list(shape), dt))
        NKMAX = 4096 + PAST
        kTh = [sbp("kTh%d" % i, [128, 2, NKMAX], BF16) for i in range(2)]
        vh = [sbp("vh%d" % i, [128, 32, VD], BF16) for i in range(2)]
        qh = [sbp("qh%d" % i, [128, 2, 2048], BF16) for i in range(2)]
        ckb = sbp("ckb", [128, 4, 2048], BF16)
        cvb = sbp("cvb", [128, 4, 2048], BF16)
        pT = [sbp("pT%d" % i, [128, 512], BF16) for i in range(4)]
        onrm = sbp("onrm", [128, 2, 2, 512], F32)
        rl = sbp("rl", [128, 512], F32)
        av = sbp("av", [128, 2, 512], F32)
        sq = sbp("sq", [128, 2, 512], BF16)
        rs3 = sbp("rs3", [128, 512], F32)
        ost = [sbp("ost%d" % i, [128, 512], BF16) for i in range(2)]
        zt = sbp("zt", [128, 4, D], BF16)
        s.op('dve', lambda v: v.memset(zt[:], 0.0), writes=['zt'])
        xsv = xsort.rearrange("(b i p) d -> b p i d", p=128, i=4)
        for b_ in range(NBLK):
            s.dma('sp', [(xsv[b_], zt[:])], reads=['zt'], semkey='ztst')
        s.dma('pool', [(ckb[:], ck.rearrange("(b p) n -> p b n", p=128))], writes=['ckb'], semkey='ckb')
        s.dma('pool', [(cvb[:], cv.rearrange("(b p) n -> p b n", p=128))], writes=['cvb'], semkey='cvb')
        SCALE = HD ** -0.5
        seqs = [dict(q0=0, nq=256, QT=256, ksrc=kTp, k0=0, vsrc=vp, nkb=2, cache=False),
                dict(q0=256, nq=256, QT=256, ksrc=kTp, k0=256, vsrc=vp, nkb=2, cache=False),
                dict(q0=512, nq=2048, QT=512, ksrc=kTs, k0=0, vsrc=vs, nkb=32, cache=True)]
        jobs = [(sq_, h) for sq_ in seqs for h in range(NH)]
        if only_tiles is not None:
            jobs = [jb for jb in jobs if (('P' in only_tiles and not jb[0]['cache']) or ('S0' in only_tiles and jb[0]['cache']))]
            if cut is not None:
                jobs = jobs[:cut]

        def attn_load(ji):
            sd, h = jobs[ji]
            bi = ji % 2
            nk_ = sd['nkb'] * 128
            s.dma('sp', [(kTh[bi][:, j, 0:nk_], sd['ksrc'][(j * NH + h) * 128:(j * NH + h + 1) * 128, sd['k0']:sd['k0'] + nk_]) for j in range(2)],
                  writes=[('kTh', bi)], semkey=('kTh', bi))
            s.dma('sp', [(vh[bi][:, 0:sd['nkb'], :], sd['vsrc'][sd['k0']:sd['k0'] + nk_, h * VD:(h + 1) * VD].rearrange("(kb p) e -> p kb e", p=128))],
                  writes=[('vh', bi)], semkey=('vh', bi))
            s.dma('sp', [(qh[bi][:, j, 0:sd['nq']], qT[(j * NH + h) * 128:(j * NH + h + 1) * 128, sd['q0']:sd['q0'] + sd['nq']]) for j in range(2)],
                  writes=[('qh', bi)], semkey=('qh', bi))

        pcnt = [0]
        ocnt = [0]
        if jobs:
            attn_load(0)
        for ji, (sd, h) in enumerate(jobs):
            bi = ji % 2
            if ji + 1 < len(jobs):
                attn_load(ji + 1)
            nkb = sd['nkb'] + (4 if sd['cache'] else 0)
            QT = sd['QT']
            if sd['cache']:
                b = psn([6, 7])
                pst = ps[b].bitcast(BF16)

                def trc(pe, pst=pst, h=h):
                    last = None
                    for j in range(2):
                        for blk in range(4):
                            c0 = (j * NH + h) * 128
                            last = pe.transpose(pst[:, (j * 4 + blk) * 128:(j * 4 + blk + 1) * 128], ckb[:, blk, c0:c0 + 128], ident[:])
                    return last
                s.op('pe', trc, reads=['ckb', 'ident'], writes=[('ps', b)])
                s.op('act', lambda a, pst=pst, bi=bi: a.copy(kTh[bi][:, :, 4096:4096 + PAST], pst.rearrange("p (j t) -> p j t", j=2)),
                     reads=[('ps', b), ('kTh', bi)], writes=[('kThc', bi)])
            kkeys = [('kTh', bi), ('kThc', bi)]

            def vblk(kb, half, bi=bi, sd=sd, h=h):
                if kb < sd['nkb']:
                    return vh[bi][:, kb, half * 128:(half + 1) * 128]
                return cvb[:, kb - sd['nkb'], h * VD + half * 128:h * VD + (half + 1) * 128]
            for qt in range(sd['nq'] // QT):
                qsl = slice(qt * QT, (qt + 1) * QT)
                for j in range(2):
                    accb = [0, 1, 2] if j == 0 else [3, 4, 5]

                    def emit_s(kb, j=j, qsl=qsl, bi=bi):
                        sbk = psn([6, 7])
                        s.op('pe', lambda pe: pe.matmul(ps[sbk][:, 0:QT], lhsT=kTh[bi][:, j, kb * 128:(kb + 1) * 128], rhs=qh[bi][:, j, qsl],
                                                        start=True, stop=True),
                             reads=kkeys + [('qh', bi)], writes=[('ps', sbk)])
                        pi = pcnt[0] % 4
                        pcnt[0] += 1
                        s.op('act', lambda a: a.activation(out=pT[pi][:, 0:QT], in_=ps[sbk][:, 0:QT], func=AF.Exp, scale=SCALE),
                             reads=[('ps', sbk)], writes=[('pT', pi)])
                        return pi
                    pis = {0: emit_s(0)}
                    for kb in range(nkb):
                        if kb + 1 < nkb:
                            pis[kb + 1] = emit_s(kb + 1)
                        pi = pis.pop(kb)

                        def pv(pe, kb=kb, pi=pi):
                            st_, sp_ = (kb == 0), (kb == nkb - 1)
                            pe.matmul(ps[accb[0]][:, 0:QT], lhsT=vblk(kb, 0), rhs=pT[pi][:, 0:QT], start=st_, stop=sp_)
                            pe.matmul(ps[accb[1]][:, 0:QT], lhsT=vblk(kb, 1), rhs=pT[pi][:, 0:QT], start=st_, stop=sp_)
                            return pe.matmul(ps[accb[2]][:, 0:QT], lhsT=ones_b[:], rhs=pT[pi][:, 0:QT], start=st_, stop=sp_)
                        s.op('pe', pv, reads=[('pT', pi), ('vh', bi), 'cvb', 'ones_b'], writes=[('ps', accb[0]), ('ps', accb[1]), ('ps', accb[2])])
                    s.op('dve', lambda v: v.reciprocal(rl[:, 0:QT], ps[accb[2]][:, 0:QT]), reads=[('ps', accb[2])], writes=['rl'])
                    for half in range(2):
                        s.op('dve', lambda v, half=half, j=j: v.tensor_mul(onrm[:, j, half, 0:QT], ps[accb[half]][:, 0:QT], rl[:, 0:QT]),
                             reads=[('ps', accb[half]), 'rl'], writes=[('onrm', j, half)])
                for half in range(2):
                    s.op('dve', lambda v, half=half: v.scalar_tensor_tensor(out=av[:, half, 0:QT], in0=onrm[:, 1, half, 0:QT], scalar=lam_t[:, 0:1],
                                                                            in1=onrm[:, 0, half, 0:QT], op0=ALU.mult, op1=ALU.add),
                         reads=[('onrm', 1, half), ('onrm', 0, half), 'lam_t'], writes=[('av', half)])
                    s.op('dve', lambda v, half=half: v.tensor_mul(sq[:, half, 0:QT], av[:, half, 0:QT], av[:, half, 0:QT]),
                         reads=[('av', half)], writes=[('sq', half)])
                sbk = psn([6, 7])
                s.op('pe', lambda pe, sbk=sbk: mm_group(pe, ps[sbk][:, 0:QT], [(ones_b[:], sq[:, hf, 0:QT]) for hf in range(2)]),
                     reads=[('sq', 0), ('sq', 1), 'ones_b'], writes=[('ps', sbk)])
                s.op('act', lambda a, sbk=sbk: a.activation(out=rs3[:, 0:QT], in_=ps[sbk][:, 0:QT], func=AF.Sqrt, bias=epsb[:, 0:1], scale=1.0 / VD),
                     reads=[('ps', sbk), 'epsb'], writes=['rs3'])
                s.op('dve', lambda v: v.reciprocal(rs3[:, 0:QT], rs3[:, 0:QT]), reads=['rs3'], writes=['rs3'])
                for half in range(2):
                    oi = ocnt[0] % 2
                    ocnt[0] += 1
                    s.op('dve', lambda v, half=half, oi=oi: v.scalar_tensor_tensor(out=ost[oi][:, 0:QT], in0=av[:, half, 0:QT], scalar=gsub[:, half:half + 1],
                                                                                   in1=rs3[:, 0:QT], op0=ALU.mult, op1=ALU.mult),
                         reads=[('av', half), 'rs3', 'gsub'], writes=[('ost', oi)])
                    r0 = h * VD + half * 128
                    c0 = sd['q0'] + qt * QT
                    s.dma('sp', [(attnT[r0:r0 + 128, c0:c0 + QT], ost[oi][:, 0:QT])], reads=[('ost', oi)], semkey=('ost', oi))
        s.barrier()
        pes.close()
        if phases <= 3:
            return nc

        combA = sb("combA", [128, NTB, NE], F32)
        selA = sb("selA", [128, NTB, NE], BF16)
        s.op('dve', lambda v: v.memset(combA[:], 0.0), writes=['combA0'])
        s.op('dve', lambda v: v.memset(selA[:], 0.0), writes=['selA0'])
        pes = ExitStack()
        sbp = lambda name, shape, dt: pes.enter_context(nc.sbuf_tensor(name + '_p4', list(shape), dt))
        ring = [sbp("ring%d" % i, [128, 8192], BF16) for i in range(4)]
        ws = WS(ring)
        big = sbp("big", [128, 8, D], F32)
        big_bf = big[:].rearrange("p a d -> p (a d)").bitcast(BF16)
        hT = sbp("hT", [128, 16, 1024], BF16)
        bc = [sbp("bc%d" % i, [128, D], F32) for i in range(2)]
        hb = sbp("hb", [128, D], BF16)
        stagef = [sbp("stagef%d" % i, [128, 512], F32) for i in range(1)]
        wr = sbp("wr", [128, 16, NE], BF16)
        rbias = sbp("rbias", [128, NE], F32)
        rt = sbp("rt", [128, 6, NE], F32)
        m8 = sbp("m8", [128, 8, 8], F32)
        g8 = sbp("g8", [128, 4, 8], F32)
        s.dma('pool', [(wr[:], w_router.rearrange("(k p) n -> p k n", p=128))], writes=['wr'], semkey='wr')
        s.dma('sp', [(rbias[:], router_bias.partition_broadcast(128))], writes=['rbias'], semkey='rbias')
        tiles4 = [dict(name='P', T=512, x=xp, x0=0, cond=0, own0=0, y=yp, y0=0),
                  dict(name='S0', T=1024, x=xs, x0=0, cond=1, own0=512, y=ys, y0=0),
                  dict(name='S1', T=1024, x=xs, x0=1024, cond=1, own0=1536, y=ys, y0=1024)]
        for tl in tiles4:
            if only_tiles is not None and tl['name'] not in only_tiles:
                continue
            T = tl['T']
            nb = T // 128
            nm = T // 512
            own0 = tl['own0']
            cond = tl['cond']
            ls = ExitStack()
            sbl = lambda name, shape, dt: ls.enter_context(nc.sbuf_tensor(name + '_4a' + tl['name'], list(shape), dt))
            gbuf = [sbl("gbuf%d" % i, [128, 1024], BF16) for i in range(2)]
            mbuf = [sbl("mbuf%d" % i, [128, 1024], BF16) for i in range(1)]
            aT = big_bf[:, 0:16 * T].rearrange("p (k t) -> p k t", k=16)
            s.dma('sp', [(aT, attnT[:, own0:own0 + T].rearrange("(k p) t -> p k t", p=128))], writes=['aT'], semkey='aT')
            for c in range(4):
                ws.add(wsrc(w_ao, c * 512, 512), 16)
            for c in range(4):
                ws.add(wsrc(w_o, c * 512, 512), 16)
            gcnt = 0
            for c in range(4):
                slot, wv = ws.get()
                for sc in range(4):
                    drow = (c * 4 + sc) * 128
                    gi = gcnt % 2
                    gcnt += 1
                    s.dma('sp', [(gbuf[gi][:, 0:T], sgaT[drow:drow + 128, own0:own0 + T])], writes=[('gbuf', gi)], semkey=('gbuf', gi))
                    s.dma('sp', [(mbuf[0][:, 0:T], mcT[drow:drow + 128, own0:own0 + T])], writes=[('mbuf', 0)], semkey=('mbuf', 0))
                    for m in range(nm):
                        b = psn([0, 1, 2, 3])
                        s.op('pe', lambda pe, b=b, sc=sc, m=m, wv=wv: mm_group(
                            pe, ps[b][:], [(wv[:, k, sc * 128:(sc + 1) * 128], aT[:, k, m * 512:(m + 1) * 512]) for k in range(16)]),
                            reads=[('ring', slot), 'aT'], writes=[('ps', b)])
                        f1 = stgf()
                        s.op('dve', lambda v, b=b, f1=f1, gi=gi, m=m: v.tensor_mul(stagef[f1][:], ps[b][:], gbuf[gi][:, m * 512:(m + 1) * 512]),
                             reads=[('ps', b), ('gbuf', gi)], writes=[('stagef', f1)])
                        s.op('dve', lambda v, f1=f1, gi=gi, m=m, c=c, sc=sc: v.tensor_add(hT[:, c * 4 + sc, m * 512:(m + 1) * 512], stagef[f1][:],
                                                                                      mbuf[0][:, m * 512:(m + 1) * 512]),
                             reads=[('stagef', f1), ('mbuf', 0)], writes=[('hT', c * 4 + sc, m)])
            s.barrier()
            ls.close()
            ls = ExitStack()
            sbl = lambda name, shape, dt: ls.enter_context(nc.sbuf_tensor(name + '_4c' + tl['name'], list(shape), dt))
            xt4 = [sbl("xt%d" % i, [128, D], F32) for i in range(1)]
            hbp = sbl("hbp", [128, D], BF16)
            for c in range(4):
                slot, wv = ws.get()
                for i in range(nb):
                    b = psn([0, 1, 2, 3])
                    s.op('pe', lambda pe, b=b, i=i, wv=wv: mm_group(
                        pe, ps[b][:], [(hT[:, k, i * 128:(i + 1) * 128], wv[:, k, :]) for k in range(16)]),
                        reads=[('ring', slot)], writes=[('ps', b)])
                    s.op('act', lambda a, b=b, i=i, c=c: a.copy(big[:, i, c * 512:(c + 1) * 512], ps[b][:]), reads=[('ps', b)], writes=[('o1', i)])
            ws.finish()
            s.barrier()
            load_bc(0, cond, 2, 'G1')
            for i in range(nb):
                r = tl['x0'] + i * 128
                s.dma('sp', [(xt4[0][:], tl['x'][r:r + 128, :])], writes=[('xt', 0)], semkey=('xt4', 0))
                s.op('act', lambda a, i=i: a.activation(out=hb[:], in_=big[:, i, :], func=AF.Square, accum_out=st1[:, 5:6]),
                     reads=[('o1', i)], writes=['hb', ('ss', 5)])
                rstd_from_ss(5)
                s.op('dve', lambda v, i=i: v.scalar_tensor_tensor(out=big[:, i, :], in0=big[:, i, :], scalar=st1[:, 5:6], in1=bc[0][:],
                                                                  op0=ALU.mult, op1=ALU.mult),
                     reads=[('ss', 5), ('bc', 0)], writes=[('o1', i)])
                s.op('dve', lambda v, i=i: v.tensor_add(big[:, i, :], big[:, i, :], xt4[0][:]), reads=[('xt', 0)], writes=[('o1', i)])
                s.dma('sp', [(x1d[own0 + i * 128:own0 + (i + 1) * 128, :], big[:, i, :])], reads=[('o1', i)], writes=[('x1d', i)], semkey=('x1s', i))
            load_bc(1, cond, 3, 'A2')
            load_bc(0, cond, 4, 'B2')
            for i in range(nb):
                norm_mod_transpose(big[:, i, :], ('o1', i), 128,
                                   lambda half, i=i: (hT[:, half * 8:(half + 1) * 8, i * 128:(i + 1) * 128], [('h2T', i, half)]), 1, 0,
                                   hb_store=h2d[own0 + i * 128:own0 + (i + 1) * 128, :])
                b = psn([4, 5])
                s.op('pe', lambda pe, b=b, i=i: mm_group(pe, ps[b][:, 0:NE], [(hT[:, k, i * 128:(i + 1) * 128], wr[:, k, :]) for k in range(16)]),
                     reads=['wr', ('h2T', i, 0), ('h2T', i, 1)], writes=[('ps', b)])
                sg_, bi_, mk_, sel_, w_, pen_ = [rt[:, q_, :] for q_ in range(6)]
                s.op('act', lambda a, b=b: a.activation(out=sg_, in_=ps[b][:, 0:NE], func=AF.Sigmoid), reads=[('ps', b)], writes=['rt_sg'])
                s.op('dve', lambda v: v.tensor_add(bi_, sg_, rbias[:]), reads=['rt_sg', 'rbias'], writes=['rt_bi'])
                for g in range(8):
                    s.op('dve', lambda v, g=g: v.max(out=m8[:, g, :], in_=bi_[:, g * 8:(g + 1) * 8]), reads=['rt_bi'], writes=[('m8', g)])
                s.op('dve', lambda v: v.tensor_add(g8[:, 0, :], m8[:, :, 0], m8[:, :, 1]), reads=[('m8', g) for g in range(8)], writes=['g8_0'])
                s.op('dve', lambda v: v.max(out=g8[:, 1, :], in_=g8[:, 0, :]), reads=['g8_0'], writes=['g8_1'])
                s.op('dve', lambda v: v.tensor_scalar(out=g8[:, 2, :], in0=g8[:, 0, :], scalar1=g8[:, 1, 3:4], scalar2=None, op0=ALU.is_ge),
                     reads=['g8_0', 'g8_1'], writes=['g8_2'])
                s.op('dve', lambda v: v.tensor_scalar(out=g8[:, 3, :], in0=g8[:, 2, :], scalar1=-1.0, scalar2=1e30, op0=ALU.add, op1=ALU.mult),
                     reads=['g8_2'], writes=['g8_3'])
                for g in range(8):
                    s.op('dve', lambda v, g=g: v.tensor_scalar(out=mk_[:, g * 8:(g + 1) * 8], in0=bi_[:, g * 8:(g + 1) * 8], scalar1=g8[:, 2, g:g + 1],
                                                               scalar2=g8[:, 3, g:g + 1], op0=ALU.mult, op1=ALU.add),
                         reads=['rt_bi', 'g8_2', 'g8_3'], writes=[('rt_mk', g)])
                s.op('dve', lambda v: v.max(out=g8[:, 1, :], in_=mk_), reads=[('rt_mk', g) for g in range(8)] + ['g8_2'], writes=['g8_1'])
                s.op('dve', lambda v: v.tensor_scalar(out=sel_, in0=mk_, scalar1=g8[:, 1, 7:8], scalar2=None, op0=ALU.is_ge),
                     reads=[('rt_mk', g) for g in range(8)] + ['g8_1'], writes=['rt_sel'])
                s.op('dve', lambda v: v.tensor_mul(w_, sg_, sel_), reads=['rt_sg', 'rt_sel'], writes=['rt_w'])
                s.op('dve', lambda v: v.reduce_sum(out=st1[:, 6:7], in_=w_, axis=mybir.AxisListType.X), reads=['rt_w'], writes=[('ss', 6)])
                s.op('dve', lambda v: v.reciprocal(st1[:, 6:7], st1[:, 6:7]), reads=[('ss', 6)], writes=[('ss', 6)])
                gi_ = own0 // 128 + i
                s.op('dve', lambda v, gi_=gi_: v.tensor_scalar(out=combA[:, gi_, :], in0=w_, scalar1=st1[:, 6:7], scalar2=ROUTED_SCALE, op0=ALU.mult, op1=ALU.mult),
                     reads=['rt_w', ('ss', 6)], writes=[('combA', gi_)])
                s.op('dve', lambda v, gi_=gi_: v.tensor_copy(selA[:, gi_, :], sel_), reads=['rt_sel'], writes=[('selA', gi_)])
            s.barrier()
            ls.close()
            ls = ExitStack()
            sbl = lambda name, shape, dt: ls.enter_context(nc.sbuf_tensor(name + '_4d' + tl['name'], list(shape), dt))
            hid = sbl("hid", [128, 4, 1024], BF16)
            ws.add(wsrc(w_sg, 0, DEXP), 16)
            ws.add(wsrc(w_su, 0, DEXP), 16)
            ws.add(w_sd.rearrange("(k p) n -> p k n", p=128), 4)
            (gslot, wg), (uslot, wu), (dslot, wd) = ws.get_group(3)
            for hc in range(4):
                for m in range(nm):
                    bg = psn([0, 1])
                    bu = psn([2, 3])
                    s.op('pe', lambda pe, bg=bg, hc=hc, m=m, wg=wg: mm_group(
                        pe, ps[bg][:], [(wg[:, k, hc * 128:(hc + 1) * 128], hT[:, k, m * 512:(m + 1) * 512]) for k in range(16)]),
                        reads=[('ring', gslot)], writes=[('ps', bg)])
                    s.op('pe', lambda pe, bu=bu, hc=hc, m=m, wu=wu: mm_group(
                        pe, ps[bu][:], [(wu[:, k, hc * 128:(hc + 1) * 128], hT[:, k, m * 512:(m + 1) * 512]) for k in range(16)]),
                        reads=[('ring', uslot)], writes=[('ps', bu)])
                    f1 = stgf()
                    s.op('act', lambda a, bg=bg, f1=f1: a.activation(out=stagef[f1][:], in_=ps[bg][:], func=AF.Silu),
                         reads=[('ps', bg)], writes=[('stagef', f1)])
                    s.op('dve', lambda v, bu=bu, f1=f1, hc=hc, m=m: v.tensor_mul(hid[:, hc, m * 512:(m + 1) * 512], stagef[f1][:], ps[bu][:]),
                         reads=[('ps', bu), ('stagef', f1)], writes=[('hid', hc, m)])
            hkeys = [('hid', hc, m) for hc in range(4) for m in range(nm)]
            for i in range(nb):
                for cb in range(4):
                    b = psn([4, 5, 6, 7])
                    s.op('pe', lambda pe, b=b, i=i, cb=cb, wd=wd: mm_group(
                        pe, ps[b][:], [(hid[:, hc, i * 128:(i + 1) * 128], wd[:, hc, cb * 512:(cb + 1) * 512]) for hc in range(4)]),
                        reads=[('ring', dslot)] + hkeys, writes=[('ps', b)])
                    s.op('act', lambda a, b=b, i=i, cb=cb: a.copy(big[:, i, cb * 512:(cb + 1) * 512], ps[b][:]), reads=[('ps', b)], writes=[('acc', i)])
                s.dma('sp', [(shd[own0 + i * 128:own0 + (i + 1) * 128, :], big[:, i, :])], reads=[('acc', i)], semkey=('x1s', i))
            ws.finish()
            s.barrier()
            ls.close()
        pes.close()
        if phases <= 4:
            return nc

        I32 = mybir.dt.int32
        pes = ExitStack()
        sbp = lambda name, shape, dt: pes.enter_context(nc.sbuf_tensor(name + '_p5', list(shape), dt))
        selb = selA
        sloti = sbp("sloti", [128, NTB * 8], I32)
        wk = sbp("wk", [128, NTB * 8], F32)
        widx = sbp("widx", [128, NBLK], I32)
        widxd = sbp("widxd", [128, NBLK * 4], I32)
        yidx = sbp("yidx", [128, NBLK * 4], I32)
        tst = ExitStack()
        sbt = lambda name, shape, dt: tst.enter_context(nc.sbuf_tensor(name + '_p5t', list(shape), dt))
        ltri = sbt("ltri", [128, 128], BF16)
        ltf = sbt("ltf", [128, 128], F32)
        rankA = sbt("rankA", [128, NTB, NE], F32)
        cnt = sbt("cnt", [128, 6, NE], F32)
        cnti = sbt("cnti", [128, NE], I32)
        valt = sbt("valt", [128, NE], F32)
        oht = sbt("oht", [128, NE], F32)
        t8 = sbt("t8", [128, 8], F32)
        slotf = sbt("slotf", [128, NTB * 8], F32)
        ebf = sbt("ebf", [128, NBLK], F32)
        pcol_i = sbt("pcol_i", [128, 1], I32)
        pcol = sbt("pcol", [128, 1], F32)
        ebp = sbt("ebp", [128, NBLK], F32)
        wdf = sbt("wdf", [128, NBLK, 4], F32)
        pk4 = sbt("pk4", [128, 4], F32)
        bio_i = sbt("bio_i", [128, NBLK], I32)
        bio = sbt("bio", [128, NBLK], F32)
        oob = sbt("oob", [128, NBLK], F32)
        wyf = sbt("wyf", [128, NBLK, 4], F32)
        s.op('pool', lambda g: g.memset(ltf[:], 1.0), writes=['ltf'])
        s.op('pool', lambda g: g.affine_select(out=ltf[:], in_=ltf[:], pattern=[[1, 128]], compare_op=ALU.is_gt, fill=0.0, base=0,
                                               channel_multiplier=-1), reads=['ltf'], writes=['ltf'])
        s.op('dve', lambda v: v.tensor_copy(ltri[:], ltf[:]), reads=['ltf'], writes=['ltri'])
        s.op('pool', lambda g: g.iota(pcol_i[:], pattern=[[0, 1]], base=0, channel_multiplier=1), writes=['pcol_i'])
        s.op('dve', lambda v: v.tensor_copy(pcol[:], pcol_i[:]), reads=['pcol_i'], writes=['pcol'])
        for i in range(NTB):
            b = psn([0, 1, 2, 3])
            s.op('pe', lambda pe, b=b, i=i: mm_group(pe, ps[b][:, 0:NE], [(ltri[:], selb[:, i, :])] + [(ones_b[:], selb[:, i2, :]) for i2 in range(i)]),
                 reads=['selb', 'ltri', 'ones_b'], writes=[('ps', b)])
            s.op('act', lambda a, b=b, i=i: a.copy(rankA[:, i, :], ps[b][:, 0:NE]), reads=[('ps', b)], writes=[('rank', i)])
        b = psn([0, 1, 2, 3])
        s.op('pe', lambda pe, b=b: mm_group(pe, ps[b][:, 0:NE], [(ones_b[:], selb[:, i2, :]) for i2 in range(NTB)]),
             reads=['selb', 'ones_b'], writes=[('ps', b)])
        c_cnt, c_nb, c_a, c_b, c_bs, c_sb = [cnt[:, q_, :] for q_ in range(6)]
        s.op('act', lambda a, b=b: a.copy(c_cnt, ps[b][:, 0:NE]), reads=[('ps', b)], writes=['cnt'])
        s.op('dve', lambda v: v.tensor_scalar(out=c_nb, in0=c_cnt, scalar1=511.0, scalar2=1.0 / 512.0, op0=ALU.add, op1=ALU.mult), reads=['cnt'], writes=['nb'])
        s.op('dve', lambda v: v.tensor_scalar_add(c_nb, c_nb, -0.5 + 2.0 ** -11), reads=['nb'], writes=['nb'])
        s.op('dve', lambda v: v.tensor_copy(cnti[:], c_nb), reads=['nb'], writes=['cnti'])
        s.op('dve', lambda v: v.tensor_copy(c_nb, cnti[:]), reads=['cnti'], writes=['nb'])
        s.op('dve', lambda v: v.tensor_copy(c_a, c_nb), reads=['nb'], writes=['sa'])
        cur, nxt, kc, kn = c_a, c_b, 'sa', 'sb'
        for st_ in (1, 2, 4, 8, 16, 32):
            s.op('dve', lambda v, cur=cur, nxt=nxt, st_=st_: v.tensor_copy(nxt[:, 0:st_], cur[:, 0:st_]), reads=[kc], writes=[kn])
            s.op('dve', lambda v, cur=cur, nxt=nxt, st_=st_: v.tensor_add(nxt[:, st_:NE], cur[:, st_:NE], cur[:, 0:NE - st_]), reads=[kc, kn], writes=[kn])
            cur, nxt, kc, kn = nxt, cur, kn, kc
        s.op('dve', lambda v, cur=cur: v.tensor_sub(c_bs, cur, c_nb), reads=[kc, 'nb'], writes=['bs'])
        s.op('dve', lambda v: v.tensor_scalar(out=c_sb, in0=c_bs, scalar1=512.0, scalar2=1.0, op0=ALU.mult, op1=ALU.add), reads=['bs'], writes=['sbase'])
        for i in range(NTB):
            s.op('dve', lambda v, i=i: v.tensor_add(valt[:], rankA[:, i, :], c_sb), reads=[('rank', i), 'sbase'], writes=['valt'])
            s.op('dve', lambda v, i=i: v.tensor_mul(valt[:], valt[:], selA[:, i, :]), reads=['valt'], writes=['valt'])
            s.op('dve', lambda v: v.max(out=t8[:], in_=valt[:]), reads=['valt'], writes=['t8'])
            s.op('dve', lambda v, i=i: v.tensor_scalar_add(slotf[:, i * 8:(i + 1) * 8], t8[:], -1.0), reads=['t8'], writes=[('slotf', i)])
            for k in range(8):
                s.op('dve', lambda v, i=i, k=k: v.scalar_tensor_tensor(out=oht[:], in0=valt[:], scalar=t8[:, k:k + 1], in1=combA[:, i, :],
                                                                        op0=ALU.is_equal, op1=ALU.mult), reads=['valt', 't8'], writes=['oht'])
                s.op('dve', lambda v, i=i, k=k: v.reduce_sum(out=wk[:, i * 8 + k:i * 8 + k + 1], in_=oht[:], axis=mybir.AxisListType.X),
                     reads=['oht'], writes=[('wk', i)])
        s.op('dve', lambda v: v.tensor_copy(sloti[:], slotf[:]), reads=[('slotf', i) for i in range(NTB)], writes=['sloti'])
        for b_ in range(NBLK):
            s.op('dve', lambda v, b_=b_: v.tensor_scalar(out=oht[:], in0=c_bs, scalar1=float(b_), scalar2=None, op0=ALU.is_le), reads=['bs'], writes=['oht'])
            s.op('dve', lambda v, b_=b_: v.reduce_sum(out=ebf[:, b_:b_ + 1], in_=oht[:], axis=mybir.AxisListType.X), reads=['oht'], writes=[('ebf', b_)])
        s.op('dve', lambda v: v.tensor_scalar(out=ebf[:], in0=ebf[:], scalar1=-1.0, scalar2=128.0, op0=ALU.add, op1=ALU.mult),
             reads=[('ebf', b_) for b_ in range(NBLK)], writes=['ebf2'])
        OOBV = float(1 << 20)
        s.op('pool', lambda g: g.iota(bio_i[:], pattern=[[1, NBLK]], base=0, channel_multiplier=0), writes=['bio_i'])
        s.op('dve', lambda v: v.tensor_copy(bio[:], bio_i[:]), reads=['bio_i'], writes=['bio'])
        s.op('dve', lambda v, cur=cur: v.tensor_scalar(out=oob[:], in0=bio[:], scalar1=cur[:, NE - 1:NE], scalar2=OOBV, op0=ALU.is_ge, op1=ALU.mult),
             reads=['bio', kc], writes=['oob'])
        s.op('dve', lambda v: v.tensor_add(ebf[:], ebf[:], oob[:]), reads=['ebf2', 'oob'], writes=['ebf2'])
        s.op('dve', lambda v: v.tensor_scalar(out=ebp[:], in0=ebf[:], scalar1=pcol[:, 0:1], scalar2=None, op0=ALU.add), reads=['ebf2', 'pcol'], writes=['ebp'])
        s.op('dve', lambda v: v.tensor_copy(widx[:], ebp[:]), reads=['ebp'], writes=['widx'])
        for k in range(4):
            s.op('dve', lambda v, k=k: v.tensor_scalar_add(pk4[:, k:k + 1], pcol[:, 0:1], float(k * 128)), reads=['pcol'], writes=[('pk4', k)])
            s.op('dve', lambda v, k=k: v.tensor_scalar(out=wdf[:, :, k], in0=ebf[:], scalar1=4.0, scalar2=pk4[:, k:k + 1], op0=ALU.mult, op1=ALU.add),
                 reads=['ebf2', ('pk4', k)], writes=[('wdf', k)])
        s.op('dve', lambda v: v.tensor_copy(widxd[:], wdf[:].rearrange("p b k -> p (b k)")), reads=[('wdf', k) for k in range(4)], writes=['widxd'])
        s.op('dve', lambda v: v.scalar_tensor_tensor(out=bio[:], in0=bio[:], scalar=512.0, in1=oob[:], op0=ALU.mult, op1=ALU.add), reads=['bio', 'oob'], writes=['bio'])
        for k in range(4):
            s.op('dve', lambda v, k=k: v.tensor_scalar(out=wyf[:, :, k], in0=bio[:], scalar1=pk4[:, k:k + 1], scalar2=None, op0=ALU.add),
                 reads=['bio', ('pk4', k)], writes=[('wyf', k)])
        s.op('dve', lambda v: v.tensor_copy(yidx[:], wyf[:].rearrange("p b k -> p (b k)")), reads=[('wyf', k) for k in range(4)], writes=['yidx'])
        bnd_w = nc.gpsimd.alloc_register("bnd_w")
        nc.gpsimd.reg_mov(bnd_w, NE * 128 - 1)
        bnd_d = nc.gpsimd.alloc_register("bnd_d")
        nc.gpsimd.reg_mov(bnd_d, NE * DEXP - 1)
        bnd_y = nc.gpsimd.alloc_register("bnd_y")
        nc.gpsimd.reg_mov(bnd_y, NSLOT - 1)
        hst = ExitStack()
        hrow = [hst.enter_context(nc.sbuf_tensor("hrow%d_p5" % i, [128, D], BF16)) for i in range(2)]

        def ind_dma(out, out_off, in_, in_off, bound, reads, writes, semkey):
            s._deps('pool', reads, writes)
            if semkey not in s.dsem:
                s.dsem[semkey] = [es.enter_context(nc.semaphore("dsem%d" % s.nsem)), 0]
                s.nsem += 1
            ent = s.dsem[semkey]
            if isinstance(bound, int):
                nc.gpsimd.indirect_dma_start(out=out, out_offset=out_off, in_=in_, in_offset=in_off).then_inc(ent[0], 16)
            else:
                nc.gpsimd.indirect_dma_start(out=out, out_offset=out_off, in_=in_, in_offset=in_off, bounds_check=bound, oob_is_err=False).then_inc(ent[0], 16)
            ent[1] += 16
            s._record((ent[0], ent[1]), reads, writes)

        tbs = list(range(NTB))
        if only_tiles is not None:
            tbs = ([0, 1, 2, 3] if 'P' in only_tiles else []) + (list(range(4, 12)) if 'S0' in only_tiles else []) + (list(range(12, 20)) if 'S1' in only_tiles else [])
        for i in tbs:
            hi = i % 2
            s.dma('sp', [(hrow[hi][:], h2d[i * 128:(i + 1) * 128, :])], writes=[('hrow', hi)], semkey=('hrow', hi))
            for k in range(8):
                ind_dma(xsort[:, :], bass.IndirectOffsetOnAxis(ap=sloti[:, i * 8 + k:i * 8 + k + 1], axis=0), hrow[hi][:, :], None, NSLOT - 1,
                        [('hrow', hi), 'sloti'], ['xsort'], ('hsc', hi))
        s.barrier()
        hst.close()
        tst.close()
        NR5 = 6
        bst = ExitStack()
        sbq = lambda name, shape, dt: bst.enter_context(nc.sbuf_tensor(name + '_p5c', list(shape), dt))
        ring5 = [sbq("ring%d" % i, [128, 8192], BF16) for i in range(NR5)]
        xg = [sbq("xg%d" % i, [128, 4, D], BF16) for i in range(2)]
        xT = [sbq("xT%d" % i, [128, 16, 512], BF16) for i in range(2)]
        hid5 = [sbq("hid%d" % i, [128, 4, 512], BF16) for i in range(2)]
        sgf = [sbq("sgf%d" % i, [128, 512], F32) for i in range(2)]
        ob = [sbq("ob%d" % i, [128, D], F32) for i in range(1)]
        wgr = w_eg.rearrange("e (p k) n -> (e p) (k n)", k=16)
        wur = w_eu.rearrange("e (p k) n -> (e p) (k n)", k=16)
        wdr = w_ed.rearrange("e h n -> (e h) n")
        nblk_run = NBLK if nexp_dbg is None else nexp_dbg
        xsb = xsort.rearrange("(b i p) d -> b p i d", p=128, i=4)
        ysb = [y_.rearrange("(b i p) d -> b i p d", p=128, i=4) for y_ in ysorth]

        def blk_loads(b_):
            for j_, src in enumerate((wgr, wur)):
                slot = (b_ * 3 + j_) % NR5
                ind_dma(ring5[slot][:, :], None, src, bass.IndirectOffsetOnAxis(ap=widx[:, b_:b_ + 1], axis=0), bnd_w,
                        ['widx'], [('ring5', slot)], ('ring5', slot))
            slot = (b_ * 3 + 2) % NR5
            s._deps('pool', ['widxd'], [('ring5', slot)])
            for k in range(4):
                ind_dma(ring5[slot][:, k * 2048:(k + 1) * 2048], None, wdr, bass.IndirectOffsetOnAxis(ap=widxd[:, b_ * 4 + k:b_ * 4 + k + 1], axis=0), bnd_d,
                        [], [], ('ring5', slot))
            s._record((s.dsem[('ring5', slot)][0], s.dsem[('ring5', slot)][1]), ['widxd'], [('ring5', slot)])
            s.dma('sp', [(xg[b_ % 2][:], xsb[b_])], writes=[('xg', b_ % 2)], semkey=('xg', b_ % 2))

        if nblk_run > 0:
            blk_loads(0)
        ecnt = 0
        ocnt5 = 0
        for b_ in range(nblk_run):
            if b_ + 1 < nblk_run:
                blk_loads(b_ + 1)
            bi = b_ % 2
            slots = [(b_ * 3 + j_) % NR5 for j_ in range(3)]
            wg = ring5[slots[0]][:].rearrange("p (k h m) -> p k h m", k=16, h=4)
            wu = ring5[slots[1]][:].rearrange("p (k h m) -> p k h m", k=16, h=4)
            wd = ring5[slots[2]][:].rearrange("p (k n) -> p k n", k=4)
            xgv = xg[bi][:].rearrange("p i (k q) -> p i k q", k=16)
            for kk in range(8):
                b = psn([6, 7])
                pst = ps[b].bitcast(BF16)

                def trx(pe, kk=kk, pst=pst, xgv=xgv):
                    last = None
                    for k2 in range(2):
                        for i4 in range(4):
                            last = pe.transpose(pst[:, k2 * 512 + i4 * 128:k2 * 512 + (i4 + 1) * 128], xgv[:, i4, kk * 2 + k2, :], ident[:])
                    return last
                s.op('pe', trx, reads=[('xg', bi), 'ident'], writes=[('ps', b)])
                eng = 'act' if ecnt % 2 == 0 else 'dve'
                ecnt += 1
                if eng == 'act':
                    s.op('act', lambda a, pst=pst, kk=kk, bi=bi: a.copy(xT[bi][:, kk * 2:kk * 2 + 2, :], pst.rearrange("p (k t) -> p k t", k=2)),
                         reads=[('ps', b)], writes=[('xT', bi, kk)])
                else:
                    s.op('dve', lambda v, pst=pst, kk=kk, bi=bi: v.tensor_copy(xT[bi][:, kk * 2:kk * 2 + 2, :], pst.rearrange("p (k t) -> p k t", k=2)),
                         reads=[('ps', b)], writes=[('xT', bi, kk)])
            xkeys = [('xT', bi, kk) for kk in range(8)]
            for hc in range(4):
                bg = psn([0, 1])
                bu = psn([2, 3])
                s.op('pe', lambda pe, bg=bg, hc=hc, wg=wg, bi=bi: mm_group(pe, ps[bg][:], [(wg[:, k, hc, :], xT[bi][:, k, :]) for k in range(16)]),
                     reads=[('ring5', slots[0])] + xkeys, writes=[('ps', bg)])
                s.op('pe', lambda pe, bu=bu, hc=hc, wu=wu, bi=bi: mm_group(pe, ps[bu][:], [(wu[:, k, hc, :], xT[bi][:, k, :]) for k in range(16)]),
                     reads=[('ring5', slots[1])] + xkeys, writes=[('ps', bu)])
                fi = hc % 2
                s.op('act', lambda a, bg=bg, fi=fi: a.activation(out=sgf[fi][:], in_=ps[bg][:], func=AF.Silu), reads=[('ps', bg)], writes=[('sgf', fi)])
                s.op('dve', lambda v, bu=bu, fi=fi, hc=hc, bi=bi: v.tensor_mul(hid5[bi][:, hc, :], sgf[fi][:], ps[bu][:]),
                     reads=[('ps', bu), ('sgf', fi)], writes=[('hid5', bi, hc)])
            hkeys = [('hid5', bi, hc) for hc in range(4)]
            for i4 in range(4):
                oi = 0
                for cb in range(4):
                    b = psn([4, 5])
                    s.op('pe', lambda pe, b=b, i4=i4, cb=cb, wd=wd, bi=bi: mm_group(
                        pe, ps[b][:], [(hid5[bi][:, hc, i4 * 128:(i4 + 1) * 128], wd[:, hc, cb * 512:(cb + 1) * 512]) for hc in range(4)]),
                        reads=[('ring5', slots[2])] + hkeys, writes=[('ps', b)])
                    if cb % 2 == 0:
                        s.op('act', lambda a, b=b, oi=oi, cb=cb: a.copy(ob[oi][:, cb * 512:(cb + 1) * 512], ps[b][:]), reads=[('ps', b)], writes=[('ob', oi)])
                    else:
                        s.op('dve', lambda v, b=b, oi=oi, cb=cb: v.tensor_copy(ob[oi][:, cb * 512:(cb + 1) * 512], ps[b][:]), reads=[('ps', b)], writes=[('ob', oi)])
                s.dma('sp', [(ysb[hf][b_, i4], ob[oi][:, hf * 1024:(hf + 1) * 1024]) for hf in range(2)], reads=[('ob', oi)], writes=['ysort'], semkey=('ob', oi))
        s.barrier()
        bst.close()
        sbp = lambda name, shape, dt: pes.enter_context(nc.sbuf_tensor(name + '_p6', list(shape), dt))
        bc = [sbp("bc%d" % i, [128, D], F32) for i in range(2)]
        hb = sbp("hb", [128, D], BF16)
        accb = [sbp("accb%d" % i, [128, D], F32) for i in range(2)]
        yg = [sbp("yg%d" % i, [128, D], F32) for i in range(3)]
        x1b = [sbp("x1b%d" % i, [128, D], F32) for i in range(2)]
        load_bc(0, 0, 5, 'G2')
        load_bc(1, 1, 5, 'G2')
        ygc = 0
        outs = [(yp, 0, 0)] * 4 + [(ys, 512, 1)] * 16
        for i in tbs:
            ai = i % 2
            ydst, yoff, cnd = outs[i]
            s.dma('sp', [(accb[ai][:], shd[i * 128:(i + 1) * 128, :])], writes=[('accb', ai)], semkey=('accb', ai))
            s.dma('sp', [(x1b[ai][:], x1d[i * 128:(i + 1) * 128, :])], writes=[('x1b', ai)], semkey=('x1b', ai))
            for k in range(8):
                gi_ = ygc % 3
                ygc += 1
                for hf in range(2):
                    ind_dma(yg[gi_][:, hf * 1024:(hf + 1) * 1024], None, ysorth[hf][:, :], bass.IndirectOffsetOnAxis(ap=sloti[:, i * 8 + k:i * 8 + k + 1], axis=0),
                            NSLOT - 1, ['sloti'], [('yg', gi_, hf)], ('yg', gi_, hf))
                s.op('dve', lambda v, ai=ai, gi_=gi_, i=i, k=k: v.scalar_tensor_tensor(out=accb[ai][:], in0=yg[gi_][:], scalar=wk[:, i * 8 + k:i * 8 + k + 1],
                                                                                   in1=accb[ai][:], op0=ALU.mult, op1=ALU.add),
                     reads=[('yg', gi_, 0), ('yg', gi_, 1)], writes=[('accb', ai)])
            s.op('act', lambda a, ai=ai: a.activation(out=hb[:], in_=accb[ai][:], func=AF.Square, accum_out=st1[:, 5:6]),
                 reads=[('accb', ai)], writes=['hb', ('ss', 5)])
            rstd_from_ss(5)
            s.op('dve', lambda v, ai=ai, cnd=cnd: v.scalar_tensor_tensor(out=accb[ai][:], in0=accb[ai][:], scalar=st1[:, 5:6], in1=bc[cnd][:],
                                                                         op0=ALU.mult, op1=ALU.mult),
                 reads=[('ss', 5), ('bc', cnd)], writes=[('accb', ai)])
            s.op('dve', lambda v, ai=ai: v.tensor_add(accb[ai][:], accb[ai][:], x1b[ai][:]), reads=[('x1b', ai)], writes=[('accb', ai)])
            r = i * 128 - yoff
            s.dma('sp', [(ydst[r:r + 128, :], accb[ai][:])], reads=[('accb', ai)], semkey=('yst', ai))
        s.barrier()
        pes.close()
        pes.close()
    return nc


def _prep_inputs(inp):
    f = lambda a: np.ascontiguousarray(np.asarray(a, dtype=np.float32))
    x_prompt = f(inp['x_prompt'])
    x_sample = f(inp['x_sample'])
    cache_k = f(inp['cache_k'])
    cache_v = f(inp['cache_v'])
    c = f(inp['c'])
    c_ctx = f(inp['c_ctx'])
    shared = {
        'w_ada': f(inp['w_ada'][0]), 'b_ada': f(inp['b_ada']), 'g_pre_mix': f(inp['g_pre_mix']), 'g_post_mix': f(inp['g_post_mix']),
        'w_in': f(inp['w_in'][0]), 'lq1': f(inp['lambda_q1']), 'lk1': f(inp['lambda_k1']), 'lq2': f(inp['lambda_q2']),
        'lk2': f(inp['lambda_k2']), 'g_subln': f(inp['g_subln']), 'conv_w': f(inp['conv_w'][0]), 'w_ao': f(inp['w_attn_out'][0]),
        'w_co': f(inp['w_conv_out'][0]), 'w_o': f(inp['w_o'][0]), 'g_pre_ffn': f(inp['g_pre_ffn']), 'g_post_ffn': f(inp['g_post_ffn']),
        'w_router': f(inp['w_router'][0]), 'router_bias': f(inp['router_bias']), 'w_eg': f(inp['w_exp_gate'][0]),
        'w_eu': f(inp['w_exp_up'][0]), 'w_ed': f(inp['w_exp_down'][0]), 'w_sg': f(inp['w_sh_gate'][0]), 'w_su': f(inp['w_sh_up'][0]),
        'w_sd': f(inp['w_sh_down'][0]),
    }
    maps = []
    for core in range(8):
        b = core // 2
        h = core % 2
        own = x_sample[b, h * 2048:(h + 1) * 2048]
        oth = x_sample[b, (1 - h) * 2048:(2 - h) * 2048]
        xh = np.zeros((2, D), np.float32)
        hmask = np.array([[0.0, 1.0, 1.0, 0.0]], np.float32)
        if h == 1:
            xh[0] = x_sample[b, 2047]
            hmask[0, 0] = 1.0
        else:
            xh[1] = x_sample[b, 2048]
            hmask[0, 3] = 1.0
        pidx_ = np.concatenate([np.arange(h * 2048, (h + 1) * 2048), np.arange((1 - h) * 2048, (2 - h) * 2048)])
        posv = np.ascontiguousarray(np.stack([pidx_ // 64, pidx_ % 64], axis=0).astype(np.float32))
        m = dict(shared)
        m.update({
            'xp': np.ascontiguousarray(x_prompt[2 * core:2 * core + 2].reshape(512, D)),
            'xs': np.ascontiguousarray(np.concatenate([own, oth], axis=0)),
            'xh': xh, 'pos': posv, 'hmask': hmask,
            'csel': np.ascontiguousarray(np.stack([c_ctx, c[b]], axis=0)),
            'ck': np.ascontiguousarray(cache_k[b, 0].reshape(PAST, 2048)),
            'cv': np.ascontiguousarray(cache_v[b, 0].reshape(PAST, 2048)),
        })
        maps.append(m)
    return maps


def kernel(**inputs):
    nc = build()
    maps = _prep_inputs(inputs)
    res = run_bass_kernel_spmd(nc, maps, core_ids=list(range(8)))
    r = res.results
    y_prompt = np.stack([r[c]['yp'].reshape(2, 256, D) for c in range(8)], axis=0).reshape(16, 256, D)
    y_sample = np.stack([r[c]['ys'] for c in range(8)], axis=0).reshape(4, 4096, D)
    nkk = np.stack([r[c]['nk'].reshape(2, 256, 2, NH, HD) for c in range(8)], axis=0).reshape(16, 1, 256, 2, NH, HD)
    nvv = np.stack([r[c]['nv'].reshape(2, 256, NH, VD) for c in range(8)], axis=0).reshape(16, 1, 256, NH, VD)
    return (y_prompt.astype(np.float32), y_sample.astype(np.float32), nkk.astype(np.float32), nvv.astype(np.float32))
```

```python
import math
import numpy as np
from contextlib import ExitStack
import concourse.bass as bass
import concourse.mybir as mybir
from concourse.bass_utils import run_bass_kernel_spmd

F32 = mybir.dt.float32
BF16 = mybir.dt.bfloat16
AF = mybir.ActivationFunctionType
ALU = mybir.AluOpType

D = 2048
NPROJ = 13312
NH = 8
HD = 128
VD = 256
DCONV = 1024
NE = 64
DEXP = 512
EPS = 1e-6
LAM_INIT = 0.8 - 0.6 * math.exp(-0.3 * 0)
ROUTED_SCALE = 2.5
ROPE_THETA = 10000.0
NOWN = 2560
PAST = 512


class Sched:
    def __init__(self, nc, es):
        self.nc = nc
        self.eng = {'pe': nc.tensor, 'act': nc.scalar, 'dve': nc.vector, 'pool': nc.gpsimd, 'sp': nc.sync}
        self.es = es
        self.esem = {e: es.enter_context(nc.semaphore("sem_" + e)) for e in self.eng}
        self.eseq = {e: 0 for e in self.eng}
        self.waited = {e: {} for e in self.eng}
        self.lastw = {}
        self.readers = {}
        self.dsem = {}
        self.bar = es.enter_context(nc.semaphore("sem_bar"))
        self.barc = 0
        self.nsem = 6

    def _deps(self, e, reads, writes):
        deps = {}

        def add(tok):
            if tok is None:
                return
            s, v = tok
            k = id(s)
            if k not in deps or deps[k][1] < v:
                deps[k] = (s, v)
        for r in reads:
            add(self.lastw.get(r))
            if isinstance(r, tuple) and r[0] == 'ps':
                for tok in self.readers.get(r, {}).values():
                    if tok[0] is not self.esem.get(e):
                        add(tok)
        for w in writes:
            add(self.lastw.get(w))
            for tok in self.readers.get(w, {}).values():
                add(tok)
        for k, (s, v) in deps.items():
            if e == 'pe' and s is self.esem['pe']:
                continue
            if self.waited[e].get(k, 0) >= v:
                continue
            self.eng[e].wait_ge(s, v)
            self.waited[e][k] = v

    def _record(self, tok, reads, writes):
        for r in reads:
            self.readers.setdefault(r, {})[id(tok[0])] = tok
        for w in writes:
            self.lastw[w] = tok
            self.readers[w] = {}

    def op(self, e, fn, reads=(), writes=()):
        self._deps(e, reads, writes)
        ins = fn(self.eng[e])
        self.eseq[e] += 1
        ins.then_inc(self.esem[e], 1)
        self._record((self.esem[e], self.eseq[e]), reads, writes)
        return ins

    def dma(self, q, pairs, reads=(), writes=(), semkey=None, **kw):
        self._deps(q, reads, writes)
        if semkey not in self.dsem:
            self.dsem[semkey] = [self.es.enter_context(self.nc.semaphore("dsem%d" % self.nsem)), 0]
            self.nsem += 1
        ent = self.dsem[semkey]
        for (o, i) in pairs:
            self.eng[q].dma_start(out=o, in_=i, **kw).then_inc(ent[0], 16)
            ent[1] += 16
        self._record((ent[0], ent[1]), reads, writes)

    def barrier(self):
        sp = self.eng['sp']
        for e in ('pe', 'act', 'dve', 'pool'):
            if self.eseq[e] > 0:
                sp.wait_ge(self.esem[e], self.eseq[e])
        for k, ent in self.dsem.items():
            if ent[1] > 0:
                sp.wait_ge(ent[0], ent[1])
        self.barc += 1
        sp.nop().then_inc(self.bar, 1)
        for e in ('pe', 'act', 'dve', 'pool'):
            self.eng[e].wait_ge(self.bar, self.barc)
        self.lastw = {}
        self.readers = {}


def mm_group(pe, out, pairs):
    n = len(pairs)
    last = None
    for i, (l, r) in enumerate(pairs):
        last = pe.matmul(out, lhsT=l, rhs=r, start=(i == 0), stop=(i == n - 1))
    return last


def build(phases=99, dbg=False, only_tiles=None, cut=None, nexp_dbg=None):
    nc = bass.Bass("TRN2", target_bir_lowering=False)

    def din(name, shape, dt=F32):
        return nc.dram_tensor(name, list(shape), dt, kind="ExternalInput").ap()

    def dout(name, shape, dt=F32):
        return nc.dram_tensor(name, list(shape), dt, kind="ExternalOutput").ap()

    def dscr(name, shape, dt):
        return nc.dram_tensor(name, list(shape), dt, kind="ExternalOutput" if dbg else "Internal").ap()

    xp = din("xp", [512, D])
    xs = din("xs", [4096, D])
    xh = din("xh", [2, D])
    pos = din("pos", [2, 4096])
    hmask = din("hmask", [1, 4])
    csel = din("csel", [2, D])
    ck = din("ck", [PAST, 2048])
    cv = din("cv", [PAST, 2048])
    w_ada = din("w_ada", [D, 6 * D])
    b_ada = din("b_ada", [1, 6 * D])
    g_pre_mix = din("g_pre_mix", [1, D])
    g_post_mix = din("g_post_mix", [1, D])
    w_in = din("w_in", [D, NPROJ])
    lq1 = din("lq1", [1, HD])
    lk1 = din("lk1", [1, HD])
    lq2 = din("lq2", [1, HD])
    lk2 = din("lk2", [1, HD])
    g_subln = din("g_subln", [1, VD])
    conv_w = din("conv_w", [3, DCONV])
    w_ao = din("w_ao", [2048, D])
    w_co = din("w_co", [DCONV, D])
    w_o = din("w_o", [D, D])
    g_pre_ffn = din("g_pre_ffn", [1, D])
    g_post_ffn = din("g_post_ffn", [1, D])
    w_router = din("w_router", [D, NE])
    router_bias = din("router_bias", [1, NE])
    if phases >= 4:
        w_eg = din("w_eg", [NE, D, DEXP])
        w_eu = din("w_eu", [NE, D, DEXP])
        w_ed = din("w_ed", [NE, DEXP, D])
    w_sg = din("w_sg", [D, DEXP])
    w_su = din("w_su", [D, DEXP])
    w_sd = din("w_sd", [DEXP, D])
    yp = dout("yp", [512, D])
    ys = dout("ys", [2048, D])
    nk = dout("nk", [512, 2048])
    nv = dout("nv", [512, 2048])
    modrows = dscr("modrows", [12, D], F32)
    qT = dscr("qT", [2048, NOWN], BF16)
    kTp = dscr("kTp", [2048, 512], BF16)
    kTs = dscr("kTs", [2048, 4096], BF16)
    vp = dscr("vp", [512, 2048], BF16)
    vs = dscr("vs", [4096, 2048], BF16)
    sgaT = dscr("sgaT", [2048, NOWN], BF16)
    mcT = dscr("mcT", [2048, NOWN], BF16)
    attnT = dscr("attnT", [2048, NOWN], BF16)
    x1d = dscr("x1d", [NOWN, D], F32)
    NBLK = (NOWN * 8) // 512 + NE
    NSLOT = NBLK * 512
    h2d = dscr("h2d", [NOWN, D], BF16)
    shd = dscr("shd", [NOWN, D], F32)
    xsort = dscr("xsort", [NSLOT, D], BF16)
    ysorth = [dscr("ysort%d" % i, [NSLOT, D // 2], F32) for i in range(2)]
    ropec = dscr("ropec", [128, 4096], F32)
    ropes = dscr("ropes", [128, 4096], F32)

    with ExitStack() as es:
        s = Sched(nc, es)

        def sb(name, shape, dt):
            return es.enter_context(nc.sbuf_tensor(name, list(shape), dt))

        ps = [es.enter_context(nc.psum_tensor("ps%d" % i, [128, 512], F32)) for i in range(8)]
        psc = {}

        def psn(pool):
            k = tuple(pool)
            c = psc.get(k, 0)
            psc[k] = c + 1
            return pool[c % len(pool)]

        ident_f = sb("ident_f", [128, 128], F32)
        ident = sb("ident", [128, 128], BF16)
        pmat = sb("pmat", [128, 128], BF16)
        ones_b = sb("ones_b", [128, 128], BF16)
        st1 = sb("st1", [128, 8], F32)
        negpi = sb("negpi", [128, 1], F32)
        epsb = sb("epsb", [128, 1], F32)
        lam_t = sb("lam_t", [128, 2], F32)
        gsub = sb("gsub", [128, 2], F32)
        cw = sb("cw", [128, 3, 8], F32)
        hm = sb("hm", [128, 4], F32)
        NTB = NOWN // 128
        stc = [0]
        stfc = [0]

        class WS:
            def __init__(self, ring):
                self.ring = ring
                self.NR = len(ring)
                self.items = []
                self.issued = 0
                self.consumed = 0
                self.base = 0

            def add(self, src, k):
                self.items.append((src, k))

            def _view(self, slot, src, k):
                n = src.shape[-1]
                return self.ring[slot][:, 0:k * n].rearrange("p (k n) -> p k n", k=k)

            def _issue(self, i):
                src, k = self.items[i]
                slot = (self.base + i) % self.NR
                s.dma('pool', [(self._view(slot, src, k), src)], writes=[('ring', slot)], semkey=('ring', slot))

            def get(self):
                lim = min(len(self.items), self.consumed + self.NR)
                while self.issued < lim:
                    self._issue(self.issued)
                    self.issued += 1
                slot = (self.base + self.consumed) % self.NR
                src, k = self.items[self.consumed]
                self.consumed += 1
                return slot, self._view(slot, src, k)

            def get_group(self, n):
                lim = min(len(self.items), self.consumed + self.NR)
                while self.issued < lim:
                    self._issue(self.issued)
                    self.issued += 1
                out = []
                for _ in range(n):
                    slot = (self.base + self.consumed) % self.NR
                    src, k = self.items[self.consumed]
                    assert self.consumed < self.issued
                    self.consumed += 1
                    out.append((slot, self._view(slot, src, k)))
                return out

            def finish(self):
                assert self.consumed == len(self.items), (self.consumed, len(self.items))
                self.base = (self.base + len(self.items)) % self.NR
                self.items = []
                self.issued = 0
                self.consumed = 0

        def wsrc(w2d, c0, n):
            return w2d[:, c0:c0 + n].rearrange("(k p) n -> p k n", p=128)

        s.op('pool', lambda g: g.memset(ident_f[:], 0.0), writes=['ident_f'])
        s.op('pool', lambda g: g.affine_select(out=ident_f[:], in_=ident_f[:], pattern=[[-1, 128]],
                                               compare_op=ALU.not_equal, fill=1.0, base=0, channel_multiplier=1),
             reads=['ident_f'], writes=['ident_f'])
        s.op('dve', lambda v: v.tensor_copy(ident[:], ident_f[:]), reads=['ident_f'], writes=['ident'])
        for (a, b) in ((0, 32), (32, 0), (64, 96), (96, 64)):
            s.op('dve', lambda v, a=a, b=b: v.tensor_copy(pmat[:, a:a + 32], ident_f[:, b:b + 32]),
                 reads=['ident_f'], writes=['pmat'])
        s.op('dve', lambda v: v.memset(ones_b[:], 1.0), writes=['ones_b'])
        s.op('dve', lambda v: v.memset(negpi[:], -math.pi), writes=['negpi'])
        s.op('dve', lambda v: v.memset(epsb[:], EPS), writes=['epsb'])

        pes = ExitStack()
        sbp = lambda name, shape, dt: pes.enter_context(nc.sbuf_tensor(name + '_p1', list(shape), dt))
        ring = [sbp("ring%d" % i, [128, 8192], BF16) for i in range(4)]
        ws = WS(ring)
        big = sbp("big1", [128, 12288], F32)
        tmpf = sbp("tmpf1", [128, 2 * HD], F32)
        scT = sbp("scT", [128, 16, 2], F32)
        scTb = sbp("scTb", [128, 16, 2], BF16)
        btl = [sbp("bt%d" % i, [2, 512], F32) for i in range(2)]
        tg = [sbp("tg%d" % i, [2, D], F32) for i in range(2)]
        tr_ = [sbp("tr%d" % i, [2, D], F32) for i in range(2)]
        s.dma('sp', [(scT[:, :, cc_], csel[cc_:cc_ + 1, :].rearrange("o (k p) -> p (o k)", p=128)) for cc_ in range(2)],
              writes=['scT'], semkey='scT', allow_slow_non_contiguous=True)
        s.op('act', lambda a: a.activation(out=scTb[:], in_=scT[:], func=AF.Silu), reads=['scT'], writes=['scTb'])
        modv = big[0:2, 0:6 * D]
        for n in range(24):
            ws.add(wsrc(w_ada, n * 512, 512), 16)
        for n in range(24):
            slot, wv = ws.get()
            b = psn([0, 1])
            bi = n % 2
            s.dma('sp', [(btl[bi][:], b_ada[0:1, n * 512:(n + 1) * 512].partition_broadcast(2))], writes=[('bt', bi)], semkey=('bt', bi))
            s.op('pe', lambda pe, wv=wv, b=b: mm_group(pe, ps[b][0:2, :], [(scTb[:, k, :], wv[:, k, :]) for k in range(16)]),
                 reads=[('ring', slot), 'scTb'], writes=[('ps', b)])
            s.op('dve', lambda v, n=n, b=b, bi=bi: v.tensor_add(modv[:, n * 512:(n + 1) * 512], ps[b][0:2, :], btl[bi][:]),
                 reads=[('ps', b), ('bt', bi)], writes=['modv'])
        ws.finish()
        mv = lambda i: modv[:, i * D:(i + 1) * D]
        mr3 = modrows.rearrange("(c i) d -> c i d", c=2)
        plan = [(0, 1, g_pre_mix, True), (1, 0, None, False), (2, 2, g_post_mix, False),
                (3, 4, g_pre_ffn, True), (4, 3, None, False), (5, 5, g_post_ffn, False)]
        for idx, (row, chunk, gv, plus1) in enumerate(plan):
            ti = idx % 2
            if gv is not None:
                s.dma('sp', [(tg[ti][:], gv.partition_broadcast(2))], writes=[('tg', ti)], semkey=('tg', ti))
                if plus1:
                    s.op('dve', lambda v, ti=ti, chunk=chunk: v.scalar_tensor_tensor(out=tr_[ti][:], in0=mv(chunk), scalar=1.0, in1=tg[ti][:],
                                                                                    op0=ALU.add, op1=ALU.mult),
                         reads=['modv', ('tg', ti)], writes=[('tr', ti)])
                else:
                    s.op('dve', lambda v, ti=ti, chunk=chunk: v.tensor_mul(tr_[ti][:], mv(chunk), tg[ti][:]),
                         reads=['modv', ('tg', ti)], writes=[('tr', ti)])
            else:
                s.op('dve', lambda v, ti=ti, chunk=chunk: v.tensor_copy(tr_[ti][:], mv(chunk)), reads=['modv'], writes=[('tr', ti)])
            s.dma('sp', [(mr3[:, row, :], tr_[ti][:])], reads=[('tr', ti)], semkey=('trs', ti))

        lt = sbp("lt", [128, 4, HD], F32)
        for i, l in enumerate((lq1, lk1, lq2, lk2)):
            s.dma('sp', [(lt[:, i, :], l.partition_broadcast(128))], writes=[('lt', i)], semkey=('lt', i))
        for j in range(2):
            s.op('dve', lambda v, j=j: v.tensor_tensor(out=tmpf[:, j * HD:(j + 1) * HD], in0=lt[:, 2 * j, :], in1=lt[:, 2 * j + 1, :], op=ALU.mult),
                 reads=[('lt', 2 * j), ('lt', 2 * j + 1)], writes=[('tmpf', j)])
            s.op('dve', lambda v, j=j: v.reduce_sum(out=st1[:, j:j + 1], in_=tmpf[:, j * HD:(j + 1) * HD], axis=mybir.AxisListType.X),
                 reads=[('tmpf', j)], writes=[('st1', j)])
        s.op('act', lambda a: a.activation(out=st1[:, 2:4], in_=st1[:, 0:2], func=AF.Exp), reads=[('st1', 0), ('st1', 1)], writes=['st1e'])
        s.op('dve', lambda v: v.tensor_sub(lam_t[:, 0:1], st1[:, 3:4], st1[:, 2:3]), reads=['st1e'], writes=['lam_t'])
        s.op('dve', lambda v: v.tensor_scalar_add(lam_t[:, 0:1], lam_t[:, 0:1], -LAM_INIT), reads=['lam_t'], writes=['lam_t'])
        s.dma('sp', [(gsub[:], g_subln.rearrange("o (h p) -> p (o h)", p=128))], writes=['gsub'], semkey='gsub',
              allow_slow_non_contiguous=True)
        s.op('dve', lambda v: v.tensor_scalar_mul(gsub[:], gsub[:], 1.0 - LAM_INIT), reads=['gsub'], writes=['gsub'])
        s.dma('sp', [(cw[:, i_, :], conv_w[i_:i_ + 1, :].rearrange("o (c p) -> p (o c)", p=128)) for i_ in range(3)],
              writes=['cw'], semkey='cw', allow_slow_non_contiguous=True)

        I32 = mybir.dt.int32
        ang = big[:, 0:4096]
        rt1 = big[:, 4096:8192]
        rtf = big[:, 8192:12288]
        rti = rtf.bitcast(I32)
        pidx = sbp("pidx", [128, 4], F32)
        io_i = sbp("io_i", [128, 128], I32)
        io_f = sbp("io_f", [128, 128], F32)
        s.op('pool', lambda g: g.iota(io_i[:], pattern=[[0, 4], [1, 32]], base=0, channel_multiplier=0), writes=['io_i'])
        s.op('dve', lambda v: v.tensor_copy(io_f[:], io_i[:]), reads=['io_i'], writes=['io_f'])
        s.op('dve', lambda v: v.tensor_mul(io_f[:], io_f[:], ident_f[:]), reads=['io_f', 'ident_f'], writes=['io_f'])
        s.op('dve', lambda v: v.reduce_sum(out=pidx[:, 1:2], in_=io_f[:], axis=mybir.AxisListType.X), reads=['io_f'], writes=['pidx1'])
        s.op('act', lambda a: a.activation(out=pidx[:, 2:3], in_=pidx[:, 1:2], func=AF.Exp, scale=-math.log(ROPE_THETA) / 32.0),
             reads=['pidx1'], writes=['freq'])
        s.op('pool', lambda g: g.iota(io_i[:], pattern=[[0, 2], [1, 2], [0, 32]], base=0, channel_multiplier=0), reads=['io_i'], writes=['io_i'])
        s.op('dve', lambda v: v.tensor_copy(io_f[:], io_i[:]), reads=['io_i', 'io_f'], writes=['io_f'])
        s.op('dve', lambda v: v.tensor_mul(io_f[:], io_f[:], ident_f[:]), reads=['io_f', 'ident_f'], writes=['io_f'])
        s.op('dve', lambda v: v.reduce_sum(out=pidx[:, 3:4], in_=io_f[:], axis=mybir.AxisListType.X), reads=['io_f'], writes=['sgn'])
        s.op('dve', lambda v: v.tensor_scalar(out=pidx[:, 3:4], in0=pidx[:, 3:4], scalar1=2.0, scalar2=-1.0, op0=ALU.mult, op1=ALU.add),
             reads=['sgn'], writes=['sgn'])
        s.dma('sp', [(ang[0:64, :], pos[0:1, :].partition_broadcast(64)), (ang[64:128, :], pos[1:2, :].partition_broadcast(64))],
              reads=['modv'], writes=['ang', 'modv'], semkey='posb')
        s.op('dve', lambda v: v.tensor_scalar_mul(ang, ang, pidx[:, 2:3]), reads=['ang', 'freq'], writes=['ang'])

        def sin_of(shift, dst_dram, signed, key):
            s.op('dve', lambda v: v.tensor_scalar(out=rtf, in0=ang, scalar1=shift, scalar2=1.0 / (2 * math.pi), op0=ALU.add, op1=ALU.mult),
                 reads=['ang'], writes=['rtf'])
            s.op('dve', lambda v: v.tensor_copy(rt1.bitcast(I32), rtf), reads=['rtf'], writes=['rt1'])
            s.op('dve', lambda v: v.tensor_copy(rtf, rt1.bitcast(I32)), reads=['rt1'], writes=['rtf'])
            s.op('dve', lambda v: v.scalar_tensor_tensor(out=rt1, in0=rtf, scalar=-2 * math.pi, in1=ang, op0=ALU.mult, op1=ALU.add),
                 reads=['rtf', 'ang'], writes=['rt1'])
            s.op('dve', lambda v: v.tensor_scalar(out=rt1, in0=rt1, scalar1=-3.1415925 - shift, scalar2=3.1415925 - shift, op0=ALU.max, op1=ALU.min),
                 reads=['rt1'], writes=['rt1'])
            s.op('act', lambda a: a.activation(out=rt1, in_=rt1, func=AF.Sin, bias=sh_t[:, key:key + 1]), reads=['rt1', 'sh_t'], writes=['rt1'])
            if signed:
                s.op('dve', lambda v: v.tensor_scalar_mul(rt1, rt1, pidx[:, 3:4]), reads=['rt1', 'sgn'], writes=['rt1'])
            s.dma('sp', [(dst_dram, rt1)], reads=['rt1'], writes=[('rope', key)], semkey=('rope', key))

        sh_t = sbp("sh_t", [128, 2], F32)
        s.op('dve', lambda v: v.memset(sh_t[:, 0:1], 0.0), writes=['sh_t'])
        s.op('dve', lambda v: v.memset(sh_t[:, 1:2], math.pi / 2), reads=['sh_t'], writes=['sh_t'])
        sin_of(0.0, ropes, True, 0)
        sin_of(math.pi / 2, ropec, False, 1)
        s.dma('sp', [(hm[:], hmask.partition_broadcast(128))], writes=['hm'], semkey='hm')
        s.barrier()
        pes.close()
        if phases <= 1:
            return nc

        def load_bc(i, cond, row, key):
            r = cond * 6 + row
            s.dma('sp', [(bc[i][:], modrows[r:r + 1, :].partition_broadcast(128))], writes=[('bc', i)], semkey=('bc', i))

        def rstd_from_ss(col, dim=D):
            s.op('act', lambda a: a.activation(out=st1[:, col:col + 1], in_=st1[:, col:col + 1], func=AF.Sqrt, bias=epsb[:, 0:1], scale=1.0 / dim),
                 reads=[('ss', col), 'epsb'], writes=[('ss', col)])
            s.op('dve', lambda v: v.reciprocal(st1[:, col:col + 1], st1[:, col:col + 1]), reads=[('ss', col)], writes=[('ss', col)])

        def norm_mod_transpose(src_ap, src_key, npart, dst_fn, a_bc, b_bc, hb_store=None):
            P = npart
            s.op('act', lambda a: a.activation(out=hb[0:P, :], in_=src_ap, func=AF.Square, accum_out=st1[0:P, 4:5]),
                 reads=[src_key], writes=['hb', ('ss', 4)])
            rstd_from_ss(4)
            s.op('dve', lambda v: v.scalar_tensor_tensor(out=src_ap, in0=src_ap, scalar=st1[0:P, 4:5], in1=bc[a_bc][0:P, :],
                                                          op0=ALU.mult, op1=ALU.mult),
                 reads=[('ss', 4), ('bc', a_bc)], writes=[src_key])
            s.op('dve', lambda v: v.tensor_add(hb[0:P, :], src_ap, bc[b_bc][0:P, :]), reads=[src_key, ('bc', b_bc)], writes=['hb'])
            if hb_store is not None:
                s.op('dve', lambda v: v.tensor_copy(hbp[0:P, :].rearrange("p (k q) -> p k q", k=16), hb[0:P, :].rearrange("p (q k) -> p k q", k=16)),
                     reads=['hb'], writes=['hbp'])
                s.dma('sp', [(hb_store, hbp[0:P, :])], reads=['hbp'], semkey='hbst')
            transpose16(hb, 'hb', P, dst_fn)

        def transpose16(src, src_key, P, dst_fn):
            for half in range(2):
                b = psn([6, 7])
                pst = ps[b].bitcast(BF16)

                def tr(pe, half=half, pst=pst):
                    last = None
                    for j in range(8):
                        k = half * 8 + j
                        last = pe.transpose(pst[:, j * 128:j * 128 + P], src[0:P, k * 128:(k + 1) * 128], ident[0:P, 0:P])
                    return last
                s.op('pe', tr, reads=[src_key, 'ident'], writes=[('ps', b)])
                dst, dkeys = dst_fn(half)
                s.op('act', lambda a, pst=pst, dst=dst: a.copy(dst, pst.rearrange("p (j t) -> p j t", j=8)[:, :, 0:P]),
                     reads=[('ps', b)], writes=dkeys)

        tiles2 = [
            dict(name='P', T=512, x=xp, x0=0, cond=0, rope=None, full=True, own0=0, kdst=(kTp, 0), vdst=(vp, 0),
                 segs=[(0, 256), (256, 512)], halo=None, outkv=True),
            dict(name='S0', T=1024, x=xs, x0=0, cond=1, rope=0, full=True, own0=512, kdst=(kTs, 0), vdst=(vs, 0),
                 segs=[(0, 1024)], halo=((xh, 0), (xs, 1024), 0), outkv=False),
            dict(name='S1', T=1024, x=xs, x0=1024, cond=1, rope=1024, full=True, own0=1536, kdst=(kTs, 1024), vdst=(vs, 1024),
                 segs=[(0, 1024)], halo=((xs, 1023), (xh, 1), 2), outkv=False),
            dict(name='O0', T=1024, x=xs, x0=2048, cond=1, rope=2048, full=False, own0=None, kdst=(kTs, 2048), vdst=(vs, 2048),
                 segs=None, halo=None, outkv=False),
            dict(name='O1', T=1024, x=xs, x0=3072, cond=1, rope=3072, full=False, own0=None, kdst=(kTs, 3072), vdst=(vs, 3072),
                 segs=None, halo=None, outkv=False),
        ]
        pes = ExitStack()
        sbp = lambda name, shape, dt: pes.enter_context(nc.sbuf_tensor(name + '_p2', list(shape), dt))
        ring = [sbp("ring%d" % i, [128, 8192], BF16) for i in range(4)]
        ws = WS(ring)
        hT = sbp("hT", [128, 16, 1024], BF16)
        hTh = sbp("hTh", [128, 16, 2], BF16)
        bc = [sbp("bc%d" % i, [128, D], F32) for i in range(2)]
        xt = [sbp("xt%d" % i, [128, D], F32) for i in range(2)]
        hb = sbp("hb", [128, D], BF16)
        stage = [sbp("stage%d" % i, [128, 512], BF16) for i in range(4)]
        stagef = [sbp("stagef%d" % i, [128, 512], F32) for i in range(3)]
        ropeC = sbp("ropeC", [128, 1024], F32)
        ropeS = sbp("ropeS", [128, 1024], F32)
        ccu = sbp("ccu", [128, 4, 1026], F32)
        yv = sbp("yv", [128, 1024], F32)
        convT = sbp("convT", [128, 8, 1024], BF16)
        hbv = sbp("sgcb", [128, 4, 1024], BF16)
        xh2 = sbp("xh2", [2, D], F32)

        def stg():
            i = stc[0] % len(stage)
            stc[0] += 1
            return i

        def stgf():
            i = stfc[0] % len(stagef)
            stfc[0] += 1
            return i
        xcnt = [0]

        for tl in tiles2:
            if only_tiles is not None and tl['name'] not in only_tiles:
                continue
            T = tl['T']
            nb = T // 128
            nm = T // 512
            cond = tl['cond']
            full = tl['full']
            load_bc(0, cond, 0, 'A1')
            load_bc(1, cond, 1, 'B1')
            if tl['rope'] is not None:
                r0 = tl['rope']
                s.dma('sp', [(ropeC[:, 0:T], ropec[:, r0:r0 + T])], writes=['ropeC'], semkey='ropeC')
                s.dma('sp', [(ropeS[:, 0:T], ropes[:, r0:r0 + T])], writes=['ropeS'], semkey='ropeS')
            for i in range(nb):
                xi = xcnt[0] % 2
                xcnt[0] += 1
                r = tl['x0'] + i * 128
                s.dma('sp', [(xt[xi][:], tl['x'][r:r + 128, :])], writes=[('xt', xi)], semkey=('xt', xi))
                norm_mod_transpose(xt[xi][:], ('xt', xi), 128,
                                   lambda half, i=i: (hT[:, half * 8:(half + 1) * 8, i * 128:(i + 1) * 128], [('hT', i, half)]),
                                   0, 1)
            hT_keys = [('hT', i, h) for i in range(nb) for h in range(2)]
            if tl['halo'] is not None:
                (lsrc, lrow), (rsrc, rrow), hmc = tl['halo']
                s.dma('sp', [(xh2[0:1, :], lsrc[lrow:lrow + 1, :]), (xh2[1:2, :], rsrc[rrow:rrow + 1, :])], writes=['xh2'], semkey='xh2')
                norm_mod_transpose(xh2[:], 'xh2', 2,
                                   lambda half: (hTh[:, half * 8:(half + 1) * 8, :], [('hTh', half)]), 0, 1)
            hTh_keys = [('hTh', 0), ('hTh', 1)]

            if full:
                order = [('q', c) for c in range(4)] + [('k', c) for c in range(4, 8)] + [('v', c) for c in range(8, 12)]
                for j in range(2):
                    order += [('cc', 14 + j), ('cx', 16 + j), ('cb', 12 + j)]
                order += [('ga', c) for c in range(18, 22)]
                for c in range(22, 26):
                    order += [('gc', c), ('wco', c - 22)]
            else:
                order = [('k', c) for c in range(4, 8)] + [('v', c) for c in range(8, 12)]
            if cut is not None:
                order = order[:cut]
            for kind, c in order:
                if kind == 'wco':
                    ws.add(wsrc(w_co, c * 512, 512), 8)
                else:
                    ws.add(wsrc(w_in, c * 512, 512), 16)
            sgc_stage = None
            for kind, c in order:
                slot, wv = ws.get()
                wkey = ('ring', slot)
                if kind in ('q', 'k'):
                    for sc in range(4):
                        prow = ((c % 4) * 4 + sc) * 128
                        for m in range(nm):
                            b = psn([0, 1, 2, 3])
                            s.op('pe', lambda pe, b=b, sc=sc, m=m, wv=wv: mm_group(
                                pe, ps[b][:], [(wv[:, k, sc * 128:(sc + 1) * 128], hT[:, k, m * 512:(m + 1) * 512]) for k in range(16)]),
                                reads=[wkey] + hT_keys, writes=[('ps', b)])
                            si = stg()
                            if tl['rope'] is None:
                                s.op('act', lambda a, b=b, si=si: a.copy(stage[si][:], ps[b][:]), reads=[('ps', b)], writes=[('stage', si)])
                            else:
                                sx = stg()
                                s.op('act', lambda a, b=b, sx=sx: a.copy(stage[sx][:], ps[b][:]), reads=[('ps', b)], writes=[('stage', sx)])
                                b2 = psn([4, 5])
                                s.op('pe', lambda pe, b2=b2, sx=sx: pe.matmul(ps[b2][:], lhsT=pmat[:], rhs=stage[sx][:], start=True, stop=True),
                                     reads=[('stage', sx), 'pmat'], writes=[('ps', b2)])
                                f1 = stgf()
                                f2 = stgf()
                                s.op('dve', lambda v, b=b, f1=f1, m=m: v.tensor_mul(stagef[f1][:], ps[b][:], ropeC[:, m * 512:(m + 1) * 512]),
                                     reads=[('ps', b), 'ropeC'], writes=[('stagef', f1)])
                                s.op('dve', lambda v, b2=b2, f2=f2, m=m: v.tensor_mul(stagef[f2][:], ps[b2][:], ropeS[:, m * 512:(m + 1) * 512]),
                                     reads=[('ps', b2), 'ropeS'], writes=[('stagef', f2)])
                                s.op('dve', lambda v, f1=f1, f2=f2, si=si: v.tensor_add(stage[si][:], stagef[f1][:], stagef[f2][:]),
                                     reads=[('stagef', f1), ('stagef', f2)], writes=[('stage', si)])
                            if kind == 'q':
                                c0 = tl['own0'] + m * 512
                                s.dma('sp', [(qT[prow:prow + 128, c0:c0 + 512], stage[si][:])], reads=[('stage', si)], semkey=('stq', si))
                            else:
                                kd, k0 = tl['kdst']
                                c0 = k0 + m * 512
                                s.dma('sp', [(kd[prow:prow + 128, c0:c0 + 512], stage[si][:])], reads=[('stage', si)], semkey=('stq', si))
                    if kind == 'k' and tl['outkv']:
                        for i in range(nb):
                            b = psn([0, 1, 2, 3])
                            s.op('pe', lambda pe, b=b, i=i, wv=wv: mm_group(
                                pe, ps[b][:], [(hT[:, k, i * 128:(i + 1) * 128], wv[:, k, :]) for k in range(16)]),
                                reads=[wkey] + hT_keys, writes=[('ps', b)])
                            f1 = stgf()
                            s.op('act', lambda a, b=b, f1=f1: a.copy(stagef[f1][:], ps[b][:]), reads=[('ps', b)], writes=[('stagef', f1)])
                            cc0 = (c - 4) * 512
                            s.dma('sp', [(nk[i * 128:(i + 1) * 128, cc0:cc0 + 512], stagef[f1][:])], reads=[('stagef', f1)], semkey=('stf', f1))
                elif kind == 'v':
                    vd, v0 = tl['vdst']
                    cc0 = (c - 8) * 512
                    for i in range(nb):
                        b = psn([0, 1, 2, 3])
                        s.op('pe', lambda pe, b=b, i=i, wv=wv: mm_group(
                            pe, ps[b][:], [(hT[:, k, i * 128:(i + 1) * 128], wv[:, k, :]) for k in range(16)]),
                            reads=[wkey] + hT_keys, writes=[('ps', b)])
                        si = stg()
                        r = v0 + i * 128
                        if tl['outkv']:
                            f1 = stgf()
                            s.op('act', lambda a, b=b, f1=f1: a.copy(stagef[f1][:], ps[b][:]), reads=[('ps', b)], writes=[('stagef', f1)])
                            s.dma('sp', [(nv[i * 128:(i + 1) * 128, cc0:cc0 + 512], stagef[f1][:])], reads=[('stagef', f1)], semkey=('stf', f1))
                            s.op('dve', lambda v, si=si, f1=f1: v.tensor_copy(stage[si][:], stagef[f1][:]), reads=[('stagef', f1)], writes=[('stage', si)])
                        else:
                            s.op('act', lambda a, b=b, si=si: a.copy(stage[si][:], ps[b][:]), reads=[('ps', b)], writes=[('stage', si)])
                        s.dma('sp', [(vd[r:r + 128, cc0:cc0 + 512], stage[si][:])], reads=[('stage', si)], semkey=('stq', si))
                elif kind in ('cc', 'cx'):
                    for sc in range(4):
                        for m in range(nm):
                            b = psn([0, 1, 2, 3])
                            s.op('pe', lambda pe, b=b, sc=sc, m=m, wv=wv: mm_group(
                                pe, ps[b][:], [(wv[:, k, sc * 128:(sc + 1) * 128], hT[:, k, m * 512:(m + 1) * 512]) for k in range(16)]),
                                reads=[wkey] + hT_keys, writes=[('ps', b)])
                            dst = ccu[:, sc, 1 + m * 512:1 + (m + 1) * 512]
                            if kind == 'cc':
                                s.op('act', lambda a, b=b, dst=dst: a.copy(dst, ps[b][:]), reads=[('ps', b)], writes=[('ccu', sc, m)])
                            else:
                                s.op('dve', lambda v, b=b, dst=dst: v.tensor_mul(dst, dst, ps[b][:]), reads=[('ps', b), ('ccu', sc, m)],
                                     writes=[('ccu', sc, m)])
                        for hc, col in ((0, 0), (1, T + 1)):
                            dsth = ccu[:, sc, col:col + 1]
                            if tl['halo'] is None:
                                if kind == 'cc':
                                    s.op('dve', lambda v, dsth=dsth: v.memset(dsth, 0.0), writes=[('ccuh', sc, hc)])
                                continue
                            b = psn([0, 1, 2, 3])
                            s.op('pe', lambda pe, b=b, sc=sc, hc=hc, wv=wv: mm_group(
                                pe, ps[b][:, 0:1], [(wv[:, k, sc * 128:(sc + 1) * 128], hTh[:, k, hc:hc + 1]) for k in range(16)]),
                                reads=[wkey] + hTh_keys, writes=[('ps', b)])
                            if kind == 'cc':
                                s.op('act', lambda a, b=b, dsth=dsth: a.copy(dsth, ps[b][:, 0:1]), reads=[('ps', b)], writes=[('ccuh', sc, hc)])
                            else:
                                mcol = tl['halo'][2] + hc
                                s.op('dve', lambda v, b=b, dsth=dsth, mcol=mcol: v.scalar_tensor_tensor(
                                    out=dsth, in0=ps[b][:, 0:1], scalar=hm[:, mcol:mcol + 1], in1=dsth, op0=ALU.mult, op1=ALU.mult),
                                    reads=[('ps', b), ('ccuh', sc, hc), 'hm'], writes=[('ccuh', sc, hc)])
                elif kind == 'cb':
                    j = c - 12
                    for sc in range(4):
                        ch = j * 4 + sc
                        ukeys = [('ccu', sc, m) for m in range(nm)] + [('ccuh', sc, 0), ('ccuh', sc, 1)]
                        u = ccu[:, sc, :]
                        first = True
                        for (a0, b0) in tl['segs']:
                            has_halo = tl['halo'] is not None
                            s.op('dve', lambda v, a0=a0, b0=b0, ch=ch, u=u: v.tensor_scalar(
                                out=yv[:, a0:b0], in0=u[:, 1 + a0:1 + b0], scalar1=cw[:, 1, ch:ch + 1], scalar2=None, op0=ALU.mult),
                                reads=ukeys + ['cw'], writes=['yv'])
                            la = a0 if has_halo else a0 + 1
                            s.op('dve', lambda v, la=la, b0=b0, ch=ch, u=u: v.scalar_tensor_tensor(
                                out=yv[:, la:b0], in0=u[:, la:b0], scalar=cw[:, 0, ch:ch + 1], in1=yv[:, la:b0], op0=ALU.mult, op1=ALU.add),
                                reads=ukeys + ['cw', 'yv'], writes=['yv'])
                            rb = b0 if has_halo else b0 - 1
                            s.op('dve', lambda v, a0=a0, rb=rb, ch=ch, u=u: v.scalar_tensor_tensor(
                                out=yv[:, a0:rb], in0=u[:, a0 + 2:rb + 2], scalar=cw[:, 2, ch:ch + 1], in1=yv[:, a0:rb], op0=ALU.mult, op1=ALU.add),
                                reads=ukeys + ['cw', 'yv'], writes=['yv'])
                        for m in range(nm):
                            b = psn([0, 1, 2, 3])
                            s.op('pe', lambda pe, b=b, sc=sc, m=m, wv=wv: mm_group(
                                pe, ps[b][:], [(wv[:, k, sc * 128:(sc + 1) * 128], hT[:, k, m * 512:(m + 1) * 512]) for k in range(16)]),
                                reads=[wkey] + hT_keys, writes=[('ps', b)])
                            s.op('dve', lambda v, b=b, ch=ch, m=m: v.tensor_mul(convT[:, ch, m * 512:(m + 1) * 512], ps[b][:], yv[:, m * 512:(m + 1) * 512]),
                                 reads=[('ps', b), 'yv'], writes=[('convT', ch, m)])
                elif kind in ('ga', 'gc'):
                    if kind == 'gc':
                        sgc_stage = {}
                    for sc in range(4):
                        drow = ((c - (18 if kind == 'ga' else 22)) * 4 + sc) * 128
                        for m in range(nm):
                            b = psn([0, 1, 2, 3])
                            s.op('pe', lambda pe, b=b, sc=sc, m=m, wv=wv: mm_group(
                                pe, ps[b][:], [(wv[:, k, sc * 128:(sc + 1) * 128], hT[:, k, m * 512:(m + 1) * 512]) for k in range(16)]),
                                reads=[wkey] + hT_keys, writes=[('ps', b)])
                            if kind == 'ga':
                                si = stg()
                                s.op('act', lambda a, b=b, si=si: a.activation(out=stage[si][:], in_=ps[b][:], func=AF.Sigmoid),
                                     reads=[('ps', b)], writes=[('stage', si)])
                                c0 = tl['own0'] + m * 512
                                s.dma('sp', [(sgaT[drow:drow + 128, c0:c0 + 512], stage[si][:])], reads=[('stage', si)], semkey=('stq', si))
                            else:
                                dst = hbv[:, sc, m * 512:(m + 1) * 512]
                                s.op('act', lambda a, b=b, dst=dst: a.activation(out=dst, in_=ps[b][:], func=AF.Sigmoid),
                                     reads=[('ps', b)], writes=[('sgc', sc, m)])
                elif kind == 'wco':
                    cvkeys = [('convT', ch, m) for ch in range(8) for m in range(nm)]
                    for sc in range(4):
                        drow = (c * 4 + sc) * 128
                        for m in range(nm):
                            b = psn([0, 1, 2, 3])
                            s.op('pe', lambda pe, b=b, sc=sc, m=m, wv=wv: mm_group(
                                pe, ps[b][:], [(wv[:, k, sc * 128:(sc + 1) * 128], convT[:, k, m * 512:(m + 1) * 512]) for k in range(8)]),
                                reads=[wkey] + cvkeys, writes=[('ps', b)])
                            si = stg()
                            s.op('dve', lambda v, b=b, si=si, sc=sc, m=m: v.tensor_mul(stage[si][:], ps[b][:], hbv[:, sc, m * 512:(m + 1) * 512]),
                                 reads=[('ps', b), ('sgc', sc, m)], writes=[('stage', si)])
                            c0 = tl['own0'] + m * 512
                            s.dma('sp', [(mcT[drow:drow + 128, c0:c0 + 512], stage[si][:])], reads=[('stage', si)], semkey=('stq', si))
            ws.finish()
        s.barrier()
        pes.close()
        if phases <= 2:
            return nc

        pes = ExitStack()
        sbp = lambda name, shape, dt: pes.enter_context(nc.sbuf_tensor(name + '_p3', list(shape), dt))
        NKMAX = 4096 + PAST
        kTh = [sbp("kTh%d" % i, [128, 2, NKMAX], BF16) for i in range(2)]
        vh = [sbp("vh%d" % i, [128, 32, VD], BF16) for i in range(2)]
        qh = [sbp("qh%d" % i, [128, 2, 2048], BF16) for i in range(2)]
        ckb = sbp("ckb", [128, 4, 2048], BF16)
        cvb = sbp("cvb", [128, 4, 2048], BF16)
        pT = [sbp("pT%d" % i, [128, 512], BF16) for i in range(4)]
        onrm = sbp("onrm", [128, 2, 2, 512], F32)
        rl = sbp("rl", [128, 512], F32)
        av = sbp("av", [128, 2, 512], F32)
        sq = sbp("sq", [128, 2, 512], BF16)
        rs3 = sbp("rs3", [128, 512], F32)
        ost = [sbp("ost%d" % i, [128, 512], BF16) for i in range(2)]
        zt = sbp("zt", [128, 4, D], BF16)
        s.op('dve', lambda v: v.memset(zt[:], 0.0), writes=['zt'])
        xsv = xsort.rearrange("(b i p) d -> b p i d", p=128, i=4)
        zq = list(range(NBLK))

        def zero_some(n):
            for _ in range(n):
                if zq:
                    s.dma('sp', [(xsv[zq.pop(0)], zt[:])], reads=['zt'], semkey='ztst')
        s.dma('pool', [(ckb[:], ck.rearrange("(b p) n -> p b n", p=128))], writes=['ckb'], semkey='ckb')
        s.dma('pool', [(cvb[:], cv.rearrange("(b p) n -> p b n", p=128))], writes=['cvb'], semkey='cvb')
        SCALE = HD ** -0.5
        seqs = [dict(q0=0, nq=256, QT=256, ksrc=kTp, k0=0, vsrc=vp, nkb=2, cache=False),
                dict(q0=256, nq=256, QT=256, ksrc=kTp, k0=256, vsrc=vp, nkb=2, cache=False),
                dict(q0=512, nq=2048, QT=512, ksrc=kTs, k0=0, vsrc=vs, nkb=32, cache=True)]
        jobs = [(sq_, h) for sq_ in seqs for h in range(NH)]
        if only_tiles is not None:
            jobs = [jb for jb in jobs if (('P' in only_tiles and not jb[0]['cache']) or ('S0' in only_tiles and jb[0]['cache']))]
            if cut is not None:
                jobs = jobs[:cut]

        def attn_load(ji):
            sd, h = jobs[ji]
            bi = ji % 2
            nk_ = sd['nkb'] * 128
            s.dma('sp', [(kTh[bi][:, j, 0:nk_], sd['ksrc'][(j * NH + h) * 128:(j * NH + h + 1) * 128, sd['k0']:sd['k0'] + nk_]) for j in range(2)],
                  writes=[('kTh', bi)], semkey=('kTh', bi))
            s.dma('sp', [(vh[bi][:, 0:sd['nkb'], :], sd['vsrc'][sd['k0']:sd['k0'] + nk_, h * VD:(h + 1) * VD].rearrange("(kb p) e -> p kb e", p=128))],
                  writes=[('vh', bi)], semkey=('vh', bi))
            s.dma('sp', [(qh[bi][:, j, 0:sd['nq']], qT[(j * NH + h) * 128:(j * NH + h + 1) * 128, sd['q0']:sd['q0'] + sd['nq']]) for j in range(2)],
                  writes=[('qh', bi)], semkey=('qh', bi))

        pcnt = [0]
        ocnt = [0]
        if jobs:
            attn_load(0)
        for ji, (sd, h) in enumerate(jobs):
            bi = ji % 2
            if ji + 1 < len(jobs):
                attn_load(ji + 1)
            zero_some(5)
            nkb = sd['nkb'] + (4 if sd['cache'] else 0)
            QT = sd['QT']
            if sd['cache']:
                b = psn([6, 7])
                pst = ps[b].bitcast(BF16)

                def trc(pe, pst=pst, h=h):
                    last = None
                    for j in range(2):
                        for blk in range(4):
                            c0 = (j * NH + h) * 128
                            last = pe.transpose(pst[:, (j * 4 + blk) * 128:(j * 4 + blk + 1) * 128], ckb[:, blk, c0:c0 + 128], ident[:])
                    return last
                s.op('pe', trc, reads=['ckb', 'ident'], writes=[('ps', b)])
                s.op('act', lambda a, pst=pst, bi=bi: a.copy(kTh[bi][:, :, 4096:4096 + PAST], pst.rearrange("p (j t) -> p j t", j=2)),
                     reads=[('ps', b), ('kTh', bi)], writes=[('kThc', bi)])
            kkeys = [('kTh', bi), ('kThc', bi)]

            def vblk(kb, half, bi=bi, sd=sd, h=h):
                if kb < sd['nkb']:
                    return vh[bi][:, kb, half * 128:(half + 1) * 128]
                return cvb[:, kb - sd['nkb'], h * VD + half * 128:h * VD + (half + 1) * 128]
            for qt in range(sd['nq'] // QT):
                qsl = slice(qt * QT, (qt + 1) * QT)
                for j in range(2):
                    accb = [0, 1, 2] if j == 0 else [3, 4, 5]

                    def emit_s(kb, j=j, qsl=qsl, bi=bi):
                        sbk = psn([6, 7])
                        s.op('pe', lambda pe: pe.matmul(ps[sbk][:, 0:QT], lhsT=kTh[bi][:, j, kb * 128:(kb + 1) * 128], rhs=qh[bi][:, j, qsl],
                                                        start=True, stop=True),
                             reads=kkeys + [('qh', bi)], writes=[('ps', sbk)])
                        pi = pcnt[0] % 4
                        pcnt[0] += 1
                        s.op('act', lambda a: a.activation(out=pT[pi][:, 0:QT], in_=ps[sbk][:, 0:QT], func=AF.Exp, scale=SCALE),
                             reads=[('ps', sbk)], writes=[('pT', pi)])
                        return pi
                    pis = {0: emit_s(0)}
                    for kb in range(nkb):
                        if kb + 1 < nkb:
                            pis[kb + 1] = emit_s(kb + 1)
                        pi = pis.pop(kb)

                        def pv(pe, kb=kb, pi=pi):
                            st_, sp_ = (kb == 0), (kb == nkb - 1)
                            pe.matmul(ps[accb[0]][:, 0:QT], lhsT=vblk(kb, 0), rhs=pT[pi][:, 0:QT], start=st_, stop=sp_)
                            pe.matmul(ps[accb[1]][:, 0:QT], lhsT=vblk(kb, 1), rhs=pT[pi][:, 0:QT], start=st_, stop=sp_)
                            return pe.matmul(ps[accb[2]][:, 0:QT], lhsT=ones_b[:], rhs=pT[pi][:, 0:QT], start=st_, stop=sp_)
                        s.op('pe', pv, reads=[('pT', pi), ('vh', bi), 'cvb', 'ones_b'], writes=[('ps', accb[0]), ('ps', accb[1]), ('ps', accb[2])])
                    s.op('dve', lambda v: v.reciprocal(rl[:, 0:QT], ps[accb[2]][:, 0:QT]), reads=[('ps', accb[2])], writes=['rl'])
                    for half in range(2):
                        s.op('dve', lambda v, half=half, j=j: v.tensor_mul(onrm[:, j, half, 0:QT], ps[accb[half]][:, 0:QT], rl[:, 0:QT]),
                             reads=[('ps', accb[half]), 'rl'], writes=[('onrm', j, half)])
                for half in range(2):
                    s.op('dve', lambda v, half=half: v.scalar_tensor_tensor(out=av[:, half, 0:QT], in0=onrm[:, 1, half, 0:QT], scalar=lam_t[:, 0:1],
                                                                            in1=onrm[:, 0, half, 0:QT], op0=ALU.mult, op1=ALU.add),
                         reads=[('onrm', 1, half), ('onrm', 0, half), 'lam_t'], writes=[('av', half)])
                    s.op('dve', lambda v, half=half: v.tensor_mul(sq[:, half, 0:QT], av[:, half, 0:QT], av[:, half, 0:QT]),
                         reads=[('av', half)], writes=[('sq', half)])
                sbk = psn([6, 7])
                s.op('pe', lambda pe, sbk=sbk: mm_group(pe, ps[sbk][:, 0:QT], [(ones_b[:], sq[:, hf, 0:QT]) for hf in range(2)]),
                     reads=[('sq', 0), ('sq', 1), 'ones_b'], writes=[('ps', sbk)])
                s.op('act', lambda a, sbk=sbk: a.activation(out=rs3[:, 0:QT], in_=ps[sbk][:, 0:QT], func=AF.Sqrt, bias=epsb[:, 0:1], scale=1.0 / VD),
                     reads=[('ps', sbk), 'epsb'], writes=['rs3'])
                s.op('dve', lambda v: v.reciprocal(rs3[:, 0:QT], rs3[:, 0:QT]), reads=['rs3'], writes=['rs3'])
                for half in range(2):
                    oi = ocnt[0] % 2
                    ocnt[0] += 1
                    s.op('dve', lambda v, half=half, oi=oi: v.scalar_tensor_tensor(out=ost[oi][:, 0:QT], in0=av[:, half, 0:QT], scalar=gsub[:, half:half + 1],
                                                                                   in1=rs3[:, 0:QT], op0=ALU.mult, op1=ALU.mult),
                         reads=[('av', half), 'rs3', 'gsub'], writes=[('ost', oi)])
                    r0 = h * VD + half * 128
                    c0 = sd['q0'] + qt * QT
                    s.dma('sp', [(attnT[r0:r0 + 128, c0:c0 + QT], ost[oi][:, 0:QT])], reads=[('ost', oi)], semkey=('ost', oi))
        zero_some(NBLK)
        s.barrier()
        pes.close()
        if phases <= 3:
            return nc

        combA = sb("combA", [128, NTB, NE], F32)
        selA = sb("selA", [128, NTB, NE], BF16)
        s.op('dve', lambda v: v.memset(combA[:], 0.0), writes=['combA0'])
        s.op('dve', lambda v: v.memset(selA[:], 0.0), writes=['selA0'])
        pes = ExitStack()
        sbp = lambda name, shape, dt: pes.enter_context(nc.sbuf_tensor(name + '_p4', list(shape), dt))
        ring = [sbp("ring%d" % i, [128, 8192], BF16) for i in range(4)]
        ws = WS(ring)
        big = sbp("big", [128, 8, D], F32)
        big_bf = big[:].rearrange("p a d -> p (a d)").bitcast(BF16)
        hT = sbp("hT", [128, 16, 1024], BF16)
        bc = [sbp("bc%d" % i, [128, D], F32) for i in range(2)]
        hb = sbp("hb", [128, D], BF16)
        stagef = [sbp("stagef%d" % i, [128, 512], F32) for i in range(1)]
        wr = sbp("wr", [128, 16, NE], BF16)
        rbias = sbp("rbias", [128, NE], F32)
        rt = sbp("rt", [128, 6, NE], F32)
        m8 = sbp("m8", [128, 8, 8], F32)
        g8 = sbp("g8", [128, 4, 8], F32)
        s.dma('pool', [(wr[:], w_router.rearrange("(k p) n -> p k n", p=128))], writes=['wr'], semkey='wr')
        s.dma('sp', [(rbias[:], router_bias.partition_broadcast(128))], writes=['rbias'], semkey='rbias')
        tiles4 = [dict(name='P', T=512, x=xp, x0=0, cond=0, own0=0, y=yp, y0=0),
                  dict(name='S0', T=1024, x=xs, x0=0, cond=1, own0=512, y=ys, y0=0),
                  dict(name='S1', T=1024, x=xs, x0=1024, cond=1, own0=1536, y=ys, y0=1024)]
        for tl in tiles4:
            if only_tiles is not None and tl['name'] not in only_tiles:
                continue
            T = tl['T']
            nb = T // 128
            nm = T // 512
            own0 = tl['own0']
            cond = tl['cond']
            ls = ExitStack()
            sbl = lambda name, shape, dt: ls.enter_context(nc.sbuf_tensor(name + '_4a' + tl['name'], list(shape), dt))
            gbuf = [sbl("gbuf%d" % i, [128, 1024], BF16) for i in range(2)]
            mbuf = [sbl("mbuf%d" % i, [128, 1024], BF16) for i in range(1)]
            aT = big_bf[:, 0:16 * T].rearrange("p (k t) -> p k t", k=16)
            s.dma('sp', [(aT, attnT[:, own0:own0 + T].rearrange("(k p) t -> p k t", p=128))], writes=['aT'], semkey='aT')
            for c in range(4):
                ws.add(wsrc(w_ao, c * 512, 512), 16)
            for c in range(4):
                ws.add(wsrc(w_o, c * 512, 512), 16)
            gcnt = 0
            for c in range(4):
                slot, wv = ws.get()
                for sc in range(4):
                    drow = (c * 4 + sc) * 128
                    gi = gcnt % 2
                    gcnt += 1
                    s.dma('sp', [(gbuf[gi][:, 0:T], sgaT[drow:drow + 128, own0:own0 + T])], writes=[('gbuf', gi)], semkey=('gbuf', gi))
                    s.dma('sp', [(mbuf[0][:, 0:T], mcT[drow:drow + 128, own0:own0 + T])], writes=[('mbuf', 0)], semkey=('mbuf', 0))
                    for m in range(nm):
                        b = psn([0, 1, 2, 3])
                        s.op('pe', lambda pe, b=b, sc=sc, m=m, wv=wv: mm_group(
                            pe, ps[b][:], [(wv[:, k, sc * 128:(sc + 1) * 128], aT[:, k, m * 512:(m + 1) * 512]) for k in range(16)]),
                            reads=[('ring', slot), 'aT'], writes=[('ps', b)])
                        f1 = stgf()
                        s.op('dve', lambda v, b=b, f1=f1, gi=gi, m=m: v.tensor_mul(stagef[f1][:], ps[b][:], gbuf[gi][:, m * 512:(m + 1) * 512]),
                             reads=[('ps', b), ('gbuf', gi)], writes=[('stagef', f1)])
                        s.op('dve', lambda v, f1=f1, gi=gi, m=m, c=c, sc=sc: v.tensor_add(hT[:, c * 4 + sc, m * 512:(m + 1) * 512], stagef[f1][:],
                                                                                      mbuf[0][:, m * 512:(m + 1) * 512]),
                             reads=[('stagef', f1), ('mbuf', 0)], writes=[('hT', c * 4 + sc, m)])
            s.barrier()
            ls.close()
            ls = ExitStack()
            sbl = lambda name, shape, dt: ls.enter_context(nc.sbuf_tensor(name + '_4c' + tl['name'], list(shape), dt))
            xt4 = [sbl("xt%d" % i, [128, D], F32) for i in range(1)]
            hbp = sbl("hbp", [128, D], BF16)
            for c in range(4):
                slot, wv = ws.get()
                for i in range(nb):
                    b = psn([0, 1, 2, 3])
                    s.op('pe', lambda pe, b=b, i=i, wv=wv: mm_group(
                        pe, ps[b][:], [(hT[:, k, i * 128:(i + 1) * 128], wv[:, k, :]) for k in range(16)]),
                        reads=[('ring', slot)], writes=[('ps', b)])
                    s.op('act', lambda a, b=b, i=i, c=c: a.copy(big[:, i, c * 512:(c + 1) * 512], ps[b][:]), reads=[('ps', b)], writes=[('o1', i)])
            ws.finish()
            s.barrier()
            load_bc(0, cond, 2, 'G1')
            for i in range(nb):
                r = tl['x0'] + i * 128
                s.dma('sp', [(xt4[0][:], tl['x'][r:r + 128, :])], writes=[('xt', 0)], semkey=('xt4', 0))
                s.op('act', lambda a, i=i: a.activation(out=hb[:], in_=big[:, i, :], func=AF.Square, accum_out=st1[:, 5:6]),
                     reads=[('o1', i)], writes=['hb', ('ss', 5)])
                rstd_from_ss(5)
                s.op('dve', lambda v, i=i: v.scalar_tensor_tensor(out=big[:, i, :], in0=big[:, i, :], scalar=st1[:, 5:6], in1=bc[0][:],
                                                                  op0=ALU.mult, op1=ALU.mult),
                     reads=[('ss', 5), ('bc', 0)], writes=[('o1', i)])
                s.op('dve', lambda v, i=i: v.tensor_add(big[:, i, :], big[:, i, :], xt4[0][:]), reads=[('xt', 0)], writes=[('o1', i)])
                s.dma('sp', [(x1d[own0 + i * 128:own0 + (i + 1) * 128, :], big[:, i, :])], reads=[('o1', i)], writes=[('x1d', i)], semkey=('x1s', i))
            load_bc(1, cond, 3, 'A2')
            load_bc(0, cond, 4, 'B2')
            for i in range(nb):
                norm_mod_transpose(big[:, i, :], ('o1', i), 128,
                                   lambda half, i=i: (hT[:, half * 8:(half + 1) * 8, i * 128:(i + 1) * 128], [('h2T', i, half)]), 1, 0,
                                   hb_store=h2d[own0 + i * 128:own0 + (i + 1) * 128, :])
                b = psn([4, 5])
                s.op('pe', lambda pe, b=b, i=i: mm_group(pe, ps[b][:, 0:NE], [(hT[:, k, i * 128:(i + 1) * 128], wr[:, k, :]) for k in range(16)]),
                     reads=['wr', ('h2T', i, 0), ('h2T', i, 1)], writes=[('ps', b)])
                sg_, bi_, mk_, sel_, w_, pen_ = [rt[:, q_, :] for q_ in range(6)]
                s.op('act', lambda a, b=b: a.activation(out=sg_, in_=ps[b][:, 0:NE], func=AF.Sigmoid), reads=[('ps', b)], writes=['rt_sg'])
                s.op('dve', lambda v: v.tensor_add(bi_, sg_, rbias[:]), reads=['rt_sg', 'rbias'], writes=['rt_bi'])
                for g in range(8):
                    s.op('dve', lambda v, g=g: v.max(out=m8[:, g, :], in_=bi_[:, g * 8:(g + 1) * 8]), reads=['rt_bi'], writes=[('m8', g)])
                s.op('dve', lambda v: v.tensor_add(g8[:, 0, :], m8[:, :, 0], m8[:, :, 1]), reads=[('m8', g) for g in range(8)], writes=['g8_0'])
                s.op('dve', lambda v: v.max(out=g8[:, 1, :], in_=g8[:, 0, :]), reads=['g8_0'], writes=['g8_1'])
                s.op('dve', lambda v: v.tensor_scalar(out=g8[:, 2, :], in0=g8[:, 0, :], scalar1=g8[:, 1, 3:4], scalar2=None, op0=ALU.is_ge),
                     reads=['g8_0', 'g8_1'], writes=['g8_2'])
                s.op('dve', lambda v: v.tensor_scalar(out=g8[:, 3, :], in0=g8[:, 2, :], scalar1=-1.0, scalar2=1e30, op0=ALU.add, op1=ALU.mult),
                     reads=['g8_2'], writes=['g8_3'])
                for g in range(8):
                    s.op('dve', lambda v, g=g: v.tensor_scalar(out=mk_[:, g * 8:(g + 1) * 8], in0=bi_[:, g * 8:(g + 1) * 8], scalar1=g8[:, 2, g:g + 1],
                                                               scalar2=g8[:, 3, g:g + 1], op0=ALU.mult, op1=ALU.add),
                         reads=['rt_bi', 'g8_2', 'g8_3'], writes=[('rt_mk', g)])
                s.op('dve', lambda v: v.max(out=g8[:, 1, :], in_=mk_), reads=[('rt_mk', g) for g in range(8)] + ['g8_2'], writes=['g8_1'])
                s.op('dve', lambda v: v.tensor_scalar(out=sel_, in0=mk_, scalar1=g8[:, 1, 7:8], scalar2=None, op0=ALU.is_ge),
                     reads=[('rt_mk', g) for g in range(8)] + ['g8_1'], writes=['rt_sel'])
                s.op('dve', lambda v: v.tensor_mul(w_, sg_, sel_), reads=['rt_sg', 'rt_sel'], writes=['rt_w'])
                s.op('dve', lambda v: v.reduce_sum(out=st1[:, 6:7], in_=w_, axis=mybir.AxisListType.X), reads=['rt_w'], writes=[('ss', 6)])
                s.op('dve', lambda v: v.reciprocal(st1[:, 6:7], st1[:, 6:7]), reads=[('ss', 6)], writes=[('ss', 6)])
                gi_ = own0 // 128 + i
                s.op('dve', lambda v, gi_=gi_: v.tensor_scalar(out=combA[:, gi_, :], in0=w_, scalar1=st1[:, 6:7], scalar2=ROUTED_SCALE, op0=ALU.mult, op1=ALU.mult),
                     reads=['rt_w', ('ss', 6)], writes=[('combA', gi_)])
                s.op('dve', lambda v, gi_=gi_: v.tensor_copy(selA[:, gi_, :], sel_), reads=['rt_sel'], writes=[('selA', gi_)])
            s.barrier()
            ls.close()
            ls = ExitStack()
            sbl = lambda name, shape, dt: ls.enter_context(nc.sbuf_tensor(name + '_4d' + tl['name'], list(shape), dt))
            hid = sbl("hid", [128, 4, 1024], BF16)
            ws.add(wsrc(w_sg, 0, DEXP), 16)
            ws.add(wsrc(w_su, 0, DEXP), 16)
            ws.add(w_sd.rearrange("(k p) n -> p k n", p=128), 4)
            (gslot, wg), (uslot, wu), (dslot, wd) = ws.get_group(3)
            for hc in range(4):
                for m in range(nm):
                    bg = psn([0, 1])
                    bu = psn([2, 3])
                    s.op('pe', lambda pe, bg=bg, hc=hc, m=m, wg=wg: mm_group(
                        pe, ps[bg][:], [(wg[:, k, hc * 128:(hc + 1) * 128], hT[:, k, m * 512:(m + 1) * 512]) for k in range(16)]),
                        reads=[('ring', gslot)], writes=[('ps', bg)])
                    s.op('pe', lambda pe, bu=bu, hc=hc, m=m, wu=wu: mm_group(
                        pe, ps[bu][:], [(wu[:, k, hc * 128:(hc + 1) * 128], hT[:, k, m * 512:(m + 1) * 512]) for k in range(16)]),
                        reads=[('ring', uslot)], writes=[('ps', bu)])
                    f1 = stgf()
                    s.op('act', lambda a, bg=bg, f1=f1: a.activation(out=stagef[f1][:], in_=ps[bg][:], func=AF.Silu),
                         reads=[('ps', bg)], writes=[('stagef', f1)])
                    s.op('dve', lambda v, bu=bu, f1=f1, hc=hc, m=m: v.tensor_mul(hid[:, hc, m * 512:(m + 1) * 512], stagef[f1][:], ps[bu][:]),
                         reads=[('ps', bu), ('stagef', f1)], writes=[('hid', hc, m)])
            hkeys = [('hid', hc, m) for hc in range(4) for m in range(nm)]
            for i in range(nb):
                for cb in range(4):
                    b = psn([4, 5, 6, 7])
                    s.op('pe', lambda pe, b=b, i=i, cb=cb, wd=wd: mm_group(
                        pe, ps[b][:], [(hid[:, hc, i * 128:(i + 1) * 128], wd[:, hc, cb * 512:(cb + 1) * 512]) for hc in range(4)]),
                        reads=[('ring', dslot)] + hkeys, writes=[('ps', b)])
                    s.op('act', lambda a, b=b, i=i, cb=cb: a.copy(big[:, i, cb * 512:(cb + 1) * 512], ps[b][:]), reads=[('ps', b)], writes=[('acc', i)])
                s.dma('sp', [(shd[own0 + i * 128:own0 + (i + 1) * 128, :], big[:, i, :])], reads=[('acc', i)], semkey=('x1s', i))
            ws.finish()
            s.barrier()
            ls.close()
        pes.close()
        if phases <= 4:
            return nc

        I32 = mybir.dt.int32
        pes = ExitStack()
        sbp = lambda name, shape, dt: pes.enter_context(nc.sbuf_tensor(name + '_p5', list(shape), dt))
        selb = selA
        sloti = sbp("sloti", [128, NTB * 8], I32)
        wk = sbp("wk", [128, NTB * 8], F32)
        widx = sbp("widx", [128, NBLK], I32)
        widxd = sbp("widxd", [128, NBLK * 4], I32)
        yidx = sbp("yidx", [128, NBLK * 4], I32)
        tst = ExitStack()
        sbt = lambda name, shape, dt: tst.enter_context(nc.sbuf_tensor(name + '_p5t', list(shape), dt))
        ltri = sbt("ltri", [128, 128], BF16)
        ltf = sbt("ltf", [128, 128], F32)
        rankA = sbt("rankA", [128, NTB, NE], F32)
        cnt = sbt("cnt", [128, 6, NE], F32)
        cnti = sbt("cnti", [128, NE], I32)
        valt = sbt("valt", [128, NE], F32)
        oht = sbt("oht", [128, NE], F32)
        t8 = sbt("t8", [128, 8], F32)
        slotf = sbt("slotf", [128, NTB * 8], F32)
        ebf = sbt("ebf", [128, NBLK], F32)
        pcol_i = sbt("pcol_i", [128, 1], I32)
        pcol = sbt("pcol", [128, 1], F32)
        ebp = sbt("ebp", [128, NBLK], F32)
        wdf = sbt("wdf", [128, NBLK, 4], F32)
        pk4 = sbt("pk4", [128, 4], F32)
        bio_i = sbt("bio_i", [128, NBLK], I32)
        bio = sbt("bio", [128, NBLK], F32)
        oob = sbt("oob", [128, NBLK], F32)
        wyf = sbt("wyf", [128, NBLK, 4], F32)
        s.op('pool', lambda g: g.memset(ltf[:], 1.0), writes=['ltf'])
        s.op('pool', lambda g: g.affine_select(out=ltf[:], in_=ltf[:], pattern=[[1, 128]], compare_op=ALU.is_gt, fill=0.0, base=0,
                                               channel_multiplier=-1), reads=['ltf'], writes=['ltf'])
        s.op('dve', lambda v: v.tensor_copy(ltri[:], ltf[:]), reads=['ltf'], writes=['ltri'])
        s.op('pool', lambda g: g.iota(pcol_i[:], pattern=[[0, 1]], base=0, channel_multiplier=1), writes=['pcol_i'])
        s.op('dve', lambda v: v.tensor_copy(pcol[:], pcol_i[:]), reads=['pcol_i'], writes=['pcol'])
        for i in range(NTB):
            b = psn([0, 1, 2, 3])
            s.op('pe', lambda pe, b=b, i=i: mm_group(pe, ps[b][:, 0:NE], [(ltri[:], selb[:, i, :])] + [(ones_b[:], selb[:, i2, :]) for i2 in range(i)]),
                 reads=['selb', 'ltri', 'ones_b'], writes=[('ps', b)])
            s.op('act', lambda a, b=b, i=i: a.copy(rankA[:, i, :], ps[b][:, 0:NE]), reads=[('ps', b)], writes=[('rank', i)])
        b = psn([0, 1, 2, 3])
        s.op('pe', lambda pe, b=b: mm_group(pe, ps[b][:, 0:NE], [(ones_b[:], selb[:, i2, :]) for i2 in range(NTB)]),
             reads=['selb', 'ones_b'], writes=[('ps', b)])
        c_cnt, c_nb, c_a, c_b, c_bs, c_sb = [cnt[:, q_, :] for q_ in range(6)]
        s.op('act', lambda a, b=b: a.copy(c_cnt, ps[b][:, 0:NE]), reads=[('ps', b)], writes=['cnt'])
        s.op('dve', lambda v: v.tensor_scalar(out=c_nb, in0=c_cnt, scalar1=511.0, scalar2=1.0 / 512.0, op0=ALU.add, op1=ALU.mult), reads=['cnt'], writes=['nb'])
        s.op('dve', lambda v: v.tensor_scalar_add(c_nb, c_nb, -0.5 + 2.0 ** -11), reads=['nb'], writes=['nb'])
        s.op('dve', lambda v: v.tensor_copy(cnti[:], c_nb), reads=['nb'], writes=['cnti'])
        s.op('dve', lambda v: v.tensor_copy(c_nb, cnti[:]), reads=['cnti'], writes=['nb'])
        s.op('dve', lambda v: v.tensor_copy(c_a, c_nb), reads=['nb'], writes=['sa'])
        cur, nxt, kc, kn = c_a, c_b, 'sa', 'sb'
        for st_ in (1, 2, 4, 8, 16, 32):
            s.op('dve', lambda v, cur=cur, nxt=nxt, st_=st_: v.tensor_copy(nxt[:, 0:st_], cur[:, 0:st_]), reads=[kc], writes=[kn])
            s.op('dve', lambda v, cur=cur, nxt=nxt, st_=st_: v.tensor_add(nxt[:, st_:NE], cur[:, st_:NE], cur[:, 0:NE - st_]), reads=[kc, kn], writes=[kn])
            cur, nxt, kc, kn = nxt, cur, kn, kc
        s.op('dve', lambda v, cur=cur: v.tensor_sub(c_bs, cur, c_nb), reads=[kc, 'nb'], writes=['bs'])
        s.op('dve', lambda v: v.tensor_scalar(out=c_sb, in0=c_bs, scalar1=512.0, scalar2=1.0, op0=ALU.mult, op1=ALU.add), reads=['bs'], writes=['sbase'])
        for i in range(NTB):
            s.op('dve', lambda v, i=i: v.tensor_add(valt[:], rankA[:, i, :], c_sb), reads=[('rank', i), 'sbase'], writes=['valt'])
            s.op('dve', lambda v, i=i: v.tensor_mul(valt[:], valt[:], selA[:, i, :]), reads=['valt'], writes=['valt'])
            s.op('dve', lambda v: v.max(out=t8[:], in_=valt[:]), reads=['valt'], writes=['t8'])
            s.op('dve', lambda v, i=i: v.tensor_scalar_add(slotf[:, i * 8:(i + 1) * 8], t8[:], -1.0), reads=['t8'], writes=[('slotf', i)])
            for k in range(8):
                s.op('dve', lambda v, i=i, k=k: v.scalar_tensor_tensor(out=oht[:], in0=valt[:], scalar=t8[:, k:k + 1], in1=combA[:, i, :],
                                                                        op0=ALU.is_equal, op1=ALU.mult), reads=['valt', 't8'], writes=['oht'])
                s.op('dve', lambda v, i=i, k=k: v.reduce_sum(out=wk[:, i * 8 + k:i * 8 + k + 1], in_=oht[:], axis=mybir.AxisListType.X),
                     reads=['oht'], writes=[('wk', i)])
        s.op('dve', lambda v: v.tensor_copy(sloti[:], slotf[:]), reads=[('slotf', i) for i in range(NTB)], writes=['sloti'])
        for b_ in range(NBLK):
            s.op('dve', lambda v, b_=b_: v.tensor_scalar(out=oht[:], in0=c_bs, scalar1=float(b_), scalar2=None, op0=ALU.is_le), reads=['bs'], writes=['oht'])
            s.op('dve', lambda v, b_=b_: v.reduce_sum(out=ebf[:, b_:b_ + 1], in_=oht[:], axis=mybir.AxisListType.X), reads=['oht'], writes=[('ebf', b_)])
        s.op('dve', lambda v: v.tensor_scalar(out=ebf[:], in0=ebf[:], scalar1=-1.0, scalar2=128.0, op0=ALU.add, op1=ALU.mult),
             reads=[('ebf', b_) for b_ in range(NBLK)], writes=['ebf2'])
        OOBV = float(1 << 20)
        s.op('pool', lambda g: g.iota(bio_i[:], pattern=[[1, NBLK]], base=0, channel_multiplier=0), writes=['bio_i'])
        s.op('dve', lambda v: v.tensor_copy(bio[:], bio_i[:]), reads=['bio_i'], writes=['bio'])
        s.op('dve', lambda v, cur=cur: v.tensor_scalar(out=oob[:], in0=bio[:], scalar1=cur[:, NE - 1:NE], scalar2=OOBV, op0=ALU.is_ge, op1=ALU.mult),
             reads=['bio', kc], writes=['oob'])
        s.op('dve', lambda v: v.tensor_add(ebf[:], ebf[:], oob[:]), reads=['ebf2', 'oob'], writes=['ebf2'])
        s.op('dve', lambda v: v.tensor_scalar(out=ebp[:], in0=ebf[:], scalar1=pcol[:, 0:1], scalar2=None, op0=ALU.add), reads=['ebf2', 'pcol'], writes=['ebp'])
        s.op('dve', lambda v: v.tensor_copy(widx[:], ebp[:]), reads=['ebp'], writes=['widx'])
        for k in range(4):
            s.op('dve', lambda v, k=k: v.tensor_scalar_add(pk4[:, k:k + 1], pcol[:, 0:1], float(k * 128)), reads=['pcol'], writes=[('pk4', k)])
            s.op('dve', lambda v, k=k: v.tensor_scalar(out=wdf[:, :, k], in0=ebf[:], scalar1=4.0, scalar2=pk4[:, k:k + 1], op0=ALU.mult, op1=ALU.add),
                 reads=['ebf2', ('pk4', k)], writes=[('wdf', k)])
        s.op('dve', lambda v: v.tensor_copy(widxd[:], wdf[:].rearrange("p b k -> p (b k)")), reads=[('wdf', k) for k in range(4)], writes=['widxd'])
        s.op('dve', lambda v: v.scalar_tensor_tensor(out=bio[:], in0=bio[:], scalar=512.0, in1=oob[:], op0=ALU.mult, op1=ALU.add), reads=['bio', 'oob'], writes=['bio'])
        for k in range(4):
            s.op('dve', lambda v, k=k: v.tensor_scalar(out=wyf[:, :, k], in0=bio[:], scalar1=pk4[:, k:k + 1], scalar2=None, op0=ALU.add),
                 reads=['bio', ('pk4', k)], writes=[('wyf', k)])
        s.op('dve', lambda v: v.tensor_copy(yidx[:], wyf[:].rearrange("p b k -> p (b k)")), reads=[('wyf', k) for k in range(4)], writes=['yidx'])
        bnd_w = nc.gpsimd.alloc_register("bnd_w")
        nc.gpsimd.reg_mov(bnd_w, NE * 128 - 1)
        bnd_d = nc.gpsimd.alloc_register("bnd_d")
        nc.gpsimd.reg_mov(bnd_d, NE * DEXP - 1)
        bnd_y = nc.gpsimd.alloc_register("bnd_y")
        nc.gpsimd.reg_mov(bnd_y, NSLOT - 1)
        hst = ExitStack()
        hrow = [hst.enter_context(nc.sbuf_tensor("hrow%d_p5" % i, [128, D], BF16)) for i in range(2)]

        def ind_dma(out, out_off, in_, in_off, bound, reads, writes, semkey):
            s._deps('pool', reads, writes)
            if semkey not in s.dsem:
                s.dsem[semkey] = [es.enter_context(nc.semaphore("dsem%d" % s.nsem)), 0]
                s.nsem += 1
            ent = s.dsem[semkey]
            if isinstance(bound, int):
                nc.gpsimd.indirect_dma_start(out=out, out_offset=out_off, in_=in_, in_offset=in_off).then_inc(ent[0], 16)
            else:
                nc.gpsimd.indirect_dma_start(out=out, out_offset=out_off, in_=in_, in_offset=in_off, bounds_check=bound, oob_is_err=False).then_inc(ent[0], 16)
            ent[1] += 16
            s._record((ent[0], ent[1]), reads, writes)

        tbs = list(range(NTB))
        if only_tiles is not None:
            tbs = ([0, 1, 2, 3] if 'P' in only_tiles else []) + (list(range(4, 12)) if 'S0' in only_tiles else []) + (list(range(12, 20)) if 'S1' in only_tiles else [])
        for i in tbs:
            hi = i % 2
            s.dma('sp', [(hrow[hi][:], h2d[i * 128:(i + 1) * 128, :])], writes=[('hrow', hi)], semkey=('hrow', hi))
            for k in range(8):
                ind_dma(xsort[:, :], bass.IndirectOffsetOnAxis(ap=sloti[:, i * 8 + k:i * 8 + k + 1], axis=0), hrow[hi][:, :], None, NSLOT - 1,
                        [('hrow', hi), 'sloti'], ['xsort'], ('hsc', hi))
        s.barrier()
        hst.close()
        tst.close()
        NR5 = 6
        bst = ExitStack()
        sbq = lambda name, shape, dt: bst.enter_context(nc.sbuf_tensor(name + '_p5c', list(shape), dt))
        ring5 = [sbq("ring%d" % i, [128, 8192], BF16) for i in range(NR5)]
        xg = [sbq("xg%d" % i, [128, 4, D], BF16) for i in range(2)]
        xT = [sbq("xT%d" % i, [128, 16, 512], BF16) for i in range(2)]
        hid5 = [sbq("hid%d" % i, [128, 4, 512], BF16) for i in range(2)]
        sgf = [sbq("sgf%d" % i, [128, 512], F32) for i in range(2)]
        ob = [sbq("ob%d" % i, [128, D], F32) for i in range(1)]
        wgr = w_eg.rearrange("e (p k) n -> (e p) (k n)", k=16)
        wur = w_eu.rearrange("e (p k) n -> (e p) (k n)", k=16)
        wdr = w_ed.rearrange("e h n -> (e h) n")
        nblk_run = NBLK if nexp_dbg is None else nexp_dbg
        xsb = xsort.rearrange("(b i p) d -> b p i d", p=128, i=4)
        ysb = [y_.rearrange("(b i p) d -> b i p d", p=128, i=4) for y_ in ysorth]

        def blk_loads(b_):
            for j_, src in enumerate((wgr, wur)):
                slot = (b_ * 3 + j_) % NR5
                ind_dma(ring5[slot][:, :], None, src, bass.IndirectOffsetOnAxis(ap=widx[:, b_:b_ + 1], axis=0), bnd_w,
                        ['widx'], [('ring5', slot)], ('ring5', slot))
            slot = (b_ * 3 + 2) % NR5
            s._deps('pool', ['widxd'], [('ring5', slot)])
            for k in range(4):
                ind_dma(ring5[slot][:, k * 2048:(k + 1) * 2048], None, wdr, bass.IndirectOffsetOnAxis(ap=widxd[:, b_ * 4 + k:b_ * 4 + k + 1], axis=0), bnd_d,
                        [], [], ('ring5', slot))
            s._record((s.dsem[('ring5', slot)][0], s.dsem[('ring5', slot)][1]), ['widxd'], [('ring5', slot)])
            s.dma('sp', [(xg[b_ % 2][:], xsb[b_])], writes=[('xg', b_ % 2)], semkey=('xg', b_ % 2))

        if nblk_run > 0:
            blk_loads(0)
        ecnt = 0
        ocnt5 = 0
        for b_ in range(nblk_run):
            if b_ + 1 < nblk_run:
                blk_loads(b_ + 1)
            bi = b_ % 2
            slots = [(b_ * 3 + j_) % NR5 for j_ in range(3)]
            wg = ring5[slots[0]][:].rearrange("p (k h m) -> p k h m", k=16, h=4)
            wu = ring5[slots[1]][:].rearrange("p (k h m) -> p k h m", k=16, h=4)
            wd = ring5[slots[2]][:].rearrange("p (k n) -> p k n", k=4)
            xgv = xg[bi][:].rearrange("p i (k q) -> p i k q", k=16)
            for kk in range(8):
                b = psn([6, 7])
                pst = ps[b].bitcast(BF16)

                def trx(pe, kk=kk, pst=pst, xgv=xgv):
                    last = None
                    for k2 in range(2):
                        for i4 in range(4):
                            last = pe.transpose(pst[:, k2 * 512 + i4 * 128:k2 * 512 + (i4 + 1) * 128], xgv[:, i4, kk * 2 + k2, :], ident[:])
                    return last
                s.op('pe', trx, reads=[('xg', bi), 'ident'], writes=[('ps', b)])
                eng = 'act' if ecnt % 2 == 0 else 'dve'
                ecnt += 1
                if eng == 'act':
                    s.op('act', lambda a, pst=pst, kk=kk, bi=bi: a.copy(xT[bi][:, kk * 2:kk * 2 + 2, :], pst.rearrange("p (k t) -> p k t", k=2)),
                         reads=[('ps', b)], writes=[('xT', bi, kk)])
                else:
                    s.op('dve', lambda v, pst=pst, kk=kk, bi=bi: v.tensor_copy(xT[bi][:, kk * 2:kk * 2 + 2, :], pst.rearrange("p (k t) -> p k t", k=2)),
                         reads=[('ps', b)], writes=[('xT', bi, kk)])
            xkeys = [('xT', bi, kk) for kk in range(8)]
            for hc in range(4):
                bg = psn([0, 1])
                bu = psn([2, 3])
                s.op('pe', lambda pe, bg=bg, hc=hc, wg=wg, bi=bi: mm_group(pe, ps[bg][:], [(wg[:, k, hc, :], xT[bi][:, k, :]) for k in range(16)]),
                     reads=[('ring5', slots[0])] + xkeys, writes=[('ps', bg)])
                s.op('pe', lambda pe, bu=bu, hc=hc, wu=wu, bi=bi: mm_group(pe, ps[bu][:], [(wu[:, k, hc, :], xT[bi][:, k, :]) for k in range(16)]),
                     reads=[('ring5', slots[1])] + xkeys, writes=[('ps', bu)])
                fi = hc % 2
                s.op('act', lambda a, bg=bg, fi=fi: a.activation(out=sgf[fi][:], in_=ps[bg][:], func=AF.Silu), reads=[('ps', bg)], writes=[('sgf', fi)])
                s.op('dve', lambda v, bu=bu, fi=fi, hc=hc, bi=bi: v.tensor_mul(hid5[bi][:, hc, :], sgf[fi][:], ps[bu][:]),
                     reads=[('ps', bu), ('sgf', fi)], writes=[('hid5', bi, hc)])
            hkeys = [('hid5', bi, hc) for hc in range(4)]
            for i4 in range(4):
                oi = 0
                for cb in range(4):
                    b = psn([4, 5])
                    s.op('pe', lambda pe, b=b, i4=i4, cb=cb, wd=wd, bi=bi: mm_group(
                        pe, ps[b][:], [(hid5[bi][:, hc, i4 * 128:(i4 + 1) * 128], wd[:, hc, cb * 512:(cb + 1) * 512]) for hc in range(4)]),
                        reads=[('ring5', slots[2])] + hkeys, writes=[('ps', b)])
                    if cb % 2 == 0:
                        s.op('act', lambda a, b=b, oi=oi, cb=cb: a.copy(ob[oi][:, cb * 512:(cb + 1) * 512], ps[b][:]), reads=[('ps', b)], writes=[('ob', oi)])
                    else:
                        s.op('dve', lambda v, b=b, oi=oi, cb=cb: v.tensor_copy(ob[oi][:, cb * 512:(cb + 1) * 512], ps[b][:]), reads=[('ps', b)], writes=[('ob', oi)])
                s.dma('sp', [(ysb[hf][b_, i4], ob[oi][:, hf * 1024:(hf + 1) * 1024]) for hf in range(2)], reads=[('ob', oi)], writes=['ysort'], semkey=('ob', oi))
        s.barrier()
        bst.close()
        sbp = lambda name, shape, dt: pes.enter_context(nc.sbuf_tensor(name + '_p6', list(shape), dt))
        bc = [sbp("bc%d" % i, [128, D], F32) for i in range(2)]
        hb = sbp("hb", [128, D], BF16)
        accb = [sbp("accb%d" % i, [128, D], F32) for i in range(2)]
        yg = [sbp("yg%d" % i, [128, D], F32) for i in range(3)]
        x1b = [sbp("x1b%d" % i, [128, D], F32) for i in range(2)]
        load_bc(0, 0, 5, 'G2')
        load_bc(1, 1, 5, 'G2')
        ygc = 0
        outs = [(yp, 0, 0)] * 4 + [(ys, 512, 1)] * 16
        for i in tbs:
            ai = i % 2
            ydst, yoff, cnd = outs[i]
            s.dma('sp', [(accb[ai][:], shd[i * 128:(i + 1) * 128, :])], writes=[('accb', ai)], semkey=('accb', ai))
            s.dma('sp', [(x1b[ai][:], x1d[i * 128:(i + 1) * 128, :])], writes=[('x1b', ai)], semkey=('x1b', ai))
            for k in range(8):
                gi_ = ygc % 3
                ygc += 1
                for hf in range(2):
                    ind_dma(yg[gi_][:, hf * 1024:(hf + 1) * 1024], None, ysorth[hf][:, :], bass.IndirectOffsetOnAxis(ap=sloti[:, i * 8 + k:i * 8 + k + 1], axis=0),
                            NSLOT - 1, ['sloti'], [('yg', gi_, hf)], ('yg', gi_, hf))
                s.op('dve', lambda v, ai=ai, gi_=gi_, i=i, k=k: v.scalar_tensor_tensor(out=accb[ai][:], in0=yg[gi_][:], scalar=wk[:, i * 8 + k:i * 8 + k + 1],
                                                                                   in1=accb[ai][:], op0=ALU.mult, op1=ALU.add),
                     reads=[('yg', gi_, 0), ('yg', gi_, 1)], writes=[('accb', ai)])
            s.op('act', lambda a, ai=ai: a.activation(out=hb[:], in_=accb[ai][:], func=AF.Square, accum_out=st1[:, 5:6]),
                 reads=[('accb', ai)], writes=['hb', ('ss', 5)])
            rstd_from_ss(5)
            s.op('dve', lambda v, ai=ai, cnd=cnd: v.scalar_tensor_tensor(out=accb[ai][:], in0=accb[ai][:], scalar=st1[:, 5:6], in1=bc[cnd][:],
                                                                         op0=ALU.mult, op1=ALU.mult),
                 reads=[('ss', 5), ('bc', cnd)], writes=[('accb', ai)])
            s.op('dve', lambda v, ai=ai: v.tensor_add(accb[ai][:], accb[ai][:], x1b[ai][:]), reads=[('x1b', ai)], writes=[('accb', ai)])
            r = i * 128 - yoff
            s.dma('sp', [(ydst[r:r + 128, :], accb[ai][:])], reads=[('accb', ai)], semkey=('yst', ai))
        s.barrier()
        pes.close()
        pes.close()
    return nc


def _prep_inputs(inp):
    f = lambda a: np.ascontiguousarray(np.asarray(a, dtype=np.float32))
    x_prompt = f(inp['x_prompt'])
    x_sample = f(inp['x_sample'])
    cache_k = f(inp['cache_k'])
    cache_v = f(inp['cache_v'])
    c = f(inp['c'])
    c_ctx = f(inp['c_ctx'])
    shared = {
        'w_ada': f(inp['w_ada'][0]), 'b_ada': f(inp['b_ada']), 'g_pre_mix': f(inp['g_pre_mix']), 'g_post_mix': f(inp['g_post_mix']),
        'w_in': f(inp['w_in'][0]), 'lq1': f(inp['lambda_q1']), 'lk1': f(inp['lambda_k1']), 'lq2': f(inp['lambda_q2']),
        'lk2': f(inp['lambda_k2']), 'g_subln': f(inp['g_subln']), 'conv_w': f(inp['conv_w'][0]), 'w_ao': f(inp['w_attn_out'][0]),
        'w_co': f(inp['w_conv_out'][0]), 'w_o': f(inp['w_o'][0]), 'g_pre_ffn': f(inp['g_pre_ffn']), 'g_post_ffn': f(inp['g_post_ffn']),
        'w_router': f(inp['w_router'][0]), 'router_bias': f(inp['router_bias']), 'w_eg': f(inp['w_exp_gate'][0]),
        'w_eu': f(inp['w_exp_up'][0]), 'w_ed': f(inp['w_exp_down'][0]), 'w_sg': f(inp['w_sh_gate'][0]), 'w_su': f(inp['w_sh_up'][0]),
        'w_sd': f(inp['w_sh_down'][0]),
    }
    maps = []
    for core in range(8):
        b = core // 2
        h = core % 2
        own = x_sample[b, h * 2048:(h + 1) * 2048]
        oth = x_sample[b, (1 - h) * 2048:(2 - h) * 2048]
        xh = np.zeros((2, D), np.float32)
        hmask = np.array([[0.0, 1.0, 1.0, 0.0]], np.float32)
        if h == 1:
            xh[0] = x_sample[b, 2047]
            hmask[0, 0] = 1.0
        else:
            xh[1] = x_sample[b, 2048]
            hmask[0, 3] = 1.0
        pidx_ = np.concatenate([np.arange(h * 2048, (h + 1) * 2048), np.arange((1 - h) * 2048, (2 - h) * 2048)])
        posv = np.ascontiguousarray(np.stack([pidx_ // 64, pidx_ % 64], axis=0).astype(np.float32))
        m = dict(shared)
        m.update({
            'xp': np.ascontiguousarray(x_prompt[2 * core:2 * core + 2].reshape(512, D)),
            'xs': np.ascontiguousarray(np.concatenate([own, oth], axis=0)),
            'xh': xh, 'pos': posv, 'hmask': hmask,
            'csel': np.ascontiguousarray(np.stack([c_ctx, c[b]], axis=0)),
            'ck': np.ascontiguousarray(cache_k[b, 0].reshape(PAST, 2048)),
            'cv': np.ascontiguousarray(cache_v[b, 0].reshape(PAST, 2048)),
        })
        maps.append(m)
    return maps


def kernel(**inputs):
    nc = build()
    maps = _prep_inputs(inputs)
    res = run_bass_kernel_spmd(nc, maps, core_ids=list(range(8)))
    r = res.results
    y_prompt = np.stack([r[c]['yp'].reshape(2, 256, D) for c in range(8)], axis=0).reshape(16, 256, D)
    y_sample = np.stack([r[c]['ys'] for c in range(8)], axis=0).reshape(4, 4096, D)
    nkk = np.stack([r[c]['nk'].reshape(2, 256, 2, NH, HD) for c in range(8)], axis=0).reshape(16, 1, 256, 2, NH, HD)
    nvv = np.stack([r[c]['nv'].reshape(2, 256, NH, VD) for c in range(8)], axis=0).reshape(16, 1, 256, NH, VD)
    return (y_prompt.astype(np.float32), y_sample.astype(np.float32), nkk.astype(np.float32), nvv.astype(np.float32))
```

```python
import math
import numpy as np
from contextlib import ExitStack
import concourse.bass as bass
import concourse.mybir as mybir
from concourse.bass_utils import run_bass_kernel_spmd

F32 = mybir.dt.float32
BF16 = mybir.dt.bfloat16
AF = mybir.ActivationFunctionType
ALU = mybir.AluOpType

D = 2048
NPROJ = 13312
NH = 8
HD = 128
VD = 256
DCONV = 1024
NE = 64
DEXP = 512
EPS = 1e-6
LAM_INIT = 0.8 - 0.6 * math.exp(-0.3 * 0)
ROUTED_SCALE = 2.5
ROPE_THETA = 10000.0
NOWN = 2560
PAST = 512


class Sched:
    def __init__(self, nc, es):
        self.nc = nc
        self.eng = {'pe': nc.tensor, 'act': nc.scalar, 'dve': nc.vector, 'pool': nc.gpsimd, 'sp': nc.sync}
        self.es = es
        self.esem = {e: es.enter_context(nc.semaphore("sem_" + e)) for e in self.eng}
        self.eseq = {e: 0 for e in self.eng}
        self.waited = {e: {} for e in self.eng}
        self.lastw = {}
        self.readers = {}
        self.dsem = {}
        self.bar = es.enter_context(nc.semaphore("sem_bar"))
        self.barc = 0
        self.nsem = 6

    def _deps(self, e, reads, writes):
        deps = {}

        def add(tok):
            if tok is None:
                return
            s, v = tok
            k = id(s)
            if k not in deps or deps[k][1] < v:
                deps[k] = (s, v)
        for r in reads:
            add(self.lastw.get(r))
            if isinstance(r, tuple) and r[0] == 'ps':
                for tok in self.readers.get(r, {}).values():
                    if tok[0] is not self.esem.get(e):
                        add(tok)
        for w in writes:
            add(self.lastw.get(w))
            for tok in self.readers.get(w, {}).values():
                add(tok)
        for k, (s, v) in deps.items():
            if e == 'pe' and s is self.esem['pe']:
                continue
            if self.waited[e].get(k, 0) >= v:
                continue
            self.eng[e].wait_ge(s, v)
            self.waited[e][k] = v

    def _record(self, tok, reads, writes):
        for r in reads:
            self.readers.setdefault(r, {})[id(tok[0])] = tok
        for w in writes:
            self.lastw[w] = tok
            self.readers[w] = {}

    def op(self, e, fn, reads=(), writes=()):
        self._deps(e, reads, writes)
        ins = fn(self.eng[e])
        self.eseq[e] += 1
        ins.then_inc(self.esem[e], 1)
        self._record((self.esem[e], self.eseq[e]), reads, writes)
        return ins

    def dma(self, q, pairs, reads=(), writes=(), semkey=None, **kw):
        self._deps(q, reads, writes)
        if semkey not in self.dsem:
            self.dsem[semkey] = [self.es.enter_context(self.nc.semaphore("dsem%d" % self.nsem)), 0]
            self.nsem += 1
        ent = self.dsem[semkey]
        for (o, i) in pairs:
            self.eng[q].dma_start(out=o, in_=i, **kw).then_inc(ent[0], 16)
            ent[1] += 16
        self._record((ent[0], ent[1]), reads, writes)

    def barrier(self):
        sp = self.eng['sp']
        for e in ('pe', 'act', 'dve', 'pool'):
            if self.eseq[e] > 0:
                sp.wait_ge(self.esem[e], self.eseq[e])
        for k, ent in self.dsem.items():
            if ent[1] > 0:
                sp.wait_ge(ent[0], ent[1])
        self.barc += 1
        sp.nop().then_inc(self.bar, 1)
        for e in ('pe', 'act', 'dve', 'pool'):
            self.eng[e].wait_ge(self.bar, self.barc)
        self.lastw = {}
        self.readers = {}


def mm_group(pe, out, pairs):
    n = len(pairs)
    last = None
    for i, (l, r) in enumerate(pairs):
        last = pe.matmul(out, lhsT=l, rhs=r, start=(i == 0), stop=(i == n - 1))
    return last


def build(phases=99, dbg=False, only_tiles=None, cut=None, nexp_dbg=None):
    nc = bass.Bass("TRN2", target_bir_lowering=False)

    def din(name, shape, dt=F32):
        return nc.dram_tensor(name, list(shape), dt, kind="ExternalInput").ap()

    def dout(name, shape, dt=F32):
        return nc.dram_tensor(name, list(shape), dt, kind="ExternalOutput").ap()

    def dscr(name, shape, dt):
        return nc.dram_tensor(name, list(shape), dt, kind="ExternalOutput" if dbg else "Internal").ap()

    xp = din("xp", [512, D])
    xs = din("xs", [4096, D])
    xh = din("xh", [2, D])
    pos = din("pos", [2, 4096])
    hmask = din("hmask", [1, 4])
    csel = din("csel", [2, D])
    ck = din("ck", [PAST, 2048])
    cv = din("cv", [PAST, 2048])
    w_ada = din("w_ada", [D, 6 * D])
    b_ada = din("b_ada", [1, 6 * D])
    g_pre_mix = din("g_pre_mix", [1, D])
    g_post_mix = din("g_post_mix", [1, D])
    w_in = din("w_in", [D, NPROJ])
    lq1 = din("lq1", [1, HD])
    lk1 = din("lk1", [1, HD])
    lq2 = din("lq2", [1, HD])
    lk2 = din("lk2", [1, HD])
    g_subln = din("g_subln", [1, VD])
    conv_w = din("conv_w", [3, DCONV])
    w_ao = din("w_ao", [2048, D])
    w_co = din("w_co", [DCONV, D])
    w_o = din("w_o", [D, D])
    g_pre_ffn = din("g_pre_ffn", [1, D])
    g_post_ffn = din("g_post_ffn", [1, D])
    w_router = din("w_router", [D, NE])
    router_bias = din("router_bias", [1, NE])
    if phases >= 4:
        w_eg = din("w_eg", [NE, D, DEXP])
        w_eu = din("w_eu", [NE, D, DEXP])
        w_ed = din("w_ed", [NE, DEXP, D])
    w_sg = din("w_sg", [D, DEXP])
    w_su = din("w_su", [D, DEXP])
    w_sd = din("w_sd", [DEXP, D])
    yp = dout("yp", [512, D])
    ys = dout("ys", [2048, D])
    nk = dout("nk", [512, 2048])
    nv = dout("nv", [512, 2048])
    modrows = dscr("modrows", [12, D], F32)
    qT = dscr("qT", [2048, NOWN], BF16)
    kTp = dscr("kTp", [2048, 512], BF16)
    kTs = dscr("kTs", [2048, 4096], BF16)
    vp = dscr("vp", [512, 2048], BF16)
    vs = dscr("vs", [4096, 2048], BF16)
    sgaT = dscr("sgaT", [2048, NOWN], BF16)
    mcT = dscr("mcT", [2048, NOWN], BF16)
    attnT = dscr("attnT", [2048, NOWN], BF16)
    x1d = dscr("x1d", [NOWN, D], F32)
    NBLK = (NOWN * 8) // 512 + NE
    NSLOT = NBLK * 512
    h2d = dscr("h2d", [NOWN, D], BF16)
    shd = dscr("shd", [NOWN, D], F32)
    xsort = dscr("xsort", [NSLOT, D], BF16)
    ysorth = [dscr("ysort%d" % i, [NSLOT, D // 2], F32) for i in range(2)]
    ropec = dscr("ropec", [128, 4096], F32)
    ropes = dscr("ropes", [128, 4096], F32)

    with ExitStack() as es:
        s = Sched(nc, es)

        def sb(name, shape, dt):
            return es.enter_context(nc.sbuf_tensor(name, list(shape), dt))

        ps = [es.enter_context(nc.psum_tensor("ps%d" % i, [128, 512], F32)) for i in range(8)]
        psc = {}

        def psn(pool):
            k = tuple(pool)
            c = psc.get(k, 0)
            psc[k] = c + 1
            return pool[c % len(pool)]

        ident_f = sb("ident_f", [128, 128], F32)
        ident = sb("ident", [128, 128], BF16)
        pmat = sb("pmat", [128, 128], BF16)
        ones_b = sb("ones_b", [128, 128], BF16)
        st1 = sb("st1", [128, 8], F32)
        negpi = sb("negpi", [128, 1], F32)
        epsb = sb("epsb", [128, 1], F32)
        lam_t = sb("lam_t", [128, 2], F32)
        gsub = sb("gsub", [128, 2], F32)
        cw = sb("cw", [128, 3, 8], F32)
        hm = sb("hm", [128, 4], F32)
        NTB = NOWN // 128
        stc = [0]
        stfc = [0]

        class WS:
            def __init__(self, ring):
                self.ring = ring
                self.NR = len(ring)
                self.items = []
                self.issued = 0
                self.consumed = 0
                self.base = 0

            def add(self, src, k):
                self.items.append((src, k))

            def _view(self, slot, src, k):
                n = src.shape[-1]
                return self.ring[slot][:, 0:k * n].rearrange("p (k n) -> p k n", k=k)

            def _issue(self, i):
                src, k = self.items[i]
                slot = (self.base + i) % self.NR
                s.dma('pool', [(self._view(slot, src, k), src)], writes=[('ring', slot)], semkey=('ring', slot))

            def get(self):
                lim = min(len(self.items), self.consumed + self.NR)
                while self.issued < lim:
                    self._issue(self.issued)
                    self.issued += 1
                slot = (self.base + self.consumed) % self.NR
                src, k = self.items[self.consumed]
                self.consumed += 1
                return slot, self._view(slot, src, k)

            def get_group(self, n):
                lim = min(len(self.items), self.consumed + self.NR)
                while self.issued < lim:
                    self._issue(self.issued)
                    self.issued += 1
                out = []
                for _ in range(n):
                    slot = (self.base + self.consumed) % self.NR
                    src, k = self.items[self.consumed]
                    assert self.consumed < self.issued
                    self.consumed += 1
                    out.append((slot, self._view(slot, src, k)))
                return out

            def finish(self):
                assert self.consumed == len(self.items), (self.consumed, len(self.items))
                self.base = (self.base + len(self.items)) % self.NR
                self.items = []
                self.issued = 0
                self.consumed = 0

        def wsrc(w2d, c0, n):
            return w2d[:, c0:c0 + n].rearrange("(k p) n -> p k n", p=128)

        s.op('pool', lambda g: g.memset(ident_f[:], 0.0), writes=['ident_f'])
        s.op('pool', lambda g: g.affine_select(out=ident_f[:], in_=ident_f[:], pattern=[[-1, 128]],
                                               compare_op=ALU.not_equal, fill=1.0, base=0, channel_multiplier=1),
             reads=['ident_f'], writes=['ident_f'])
        s.op('dve', lambda v: v.tensor_copy(ident[:], ident_f[:]), reads=['ident_f'], writes=['ident'])
        for (a, b) in ((0, 32), (32, 0), (64, 96), (96, 64)):
            s.op('dve', lambda v, a=a, b=b: v.tensor_copy(pmat[:, a:a + 32], ident_f[:, b:b + 32]),
                 reads=['ident_f'], writes=['pmat'])
        s.op('dve', lambda v: v.memset(ones_b[:], 1.0), writes=['ones_b'])
        s.op('dve', lambda v: v.memset(negpi[:], -math.pi), writes=['negpi'])
        s.op('dve', lambda v: v.memset(epsb[:], EPS), writes=['epsb'])

        pes = ExitStack()
        sbp = lambda name, shape, dt: pes.enter_context(nc.sbuf_tensor(name + '_p1', list(shape), dt))
        ring = [sbp("ring%d" % i, [128, 8192], BF16) for i in range(4)]
        ws = WS(ring)
        big = sbp("big1", [128, 12288], F32)
        tmpf = sbp("tmpf1", [128, 2 * HD], F32)
        scT = sbp("scT", [128, 16, 2], F32)
        scTb = sbp("scTb", [128, 16, 2], BF16)
        btl = [sbp("bt%d" % i, [2, 512], F32) for i in range(2)]
        tg = [sbp("tg%d" % i, [2, D], F32) for i in range(2)]
        tr_ = [sbp("tr%d" % i, [2, D], F32) for i in range(2)]
        s.dma('sp', [(scT[:, :, cc_], csel[cc_:cc_ + 1, :].rearrange("o (k p) -> p (o k)", p=128)) for cc_ in range(2)],
              writes=['scT'], semkey='scT', allow_slow_non_contiguous=True)
        s.op('act', lambda a: a.activation(out=scTb[:], in_=scT[:], func=AF.Silu), reads=['scT'], writes=['scTb'])
        modv = big[0:2, 0:6 * D]
        for n in range(24):
            ws.add(wsrc(w_ada, n * 512, 512), 16)
        for n in range(24):
            slot, wv = ws.get()
            b = psn([0, 1])
            bi = n % 2
            s.dma('sp', [(btl[bi][:], b_ada[0:1, n * 512:(n + 1) * 512].partition_broadcast(2))], writes=[('bt', bi)], semkey=('bt', bi))
            s.op('pe', lambda pe, wv=wv, b=b: mm_group(pe, ps[b][0:2, :], [(scTb[:, k, :], wv[:, k, :]) for k in range(16)]),
                 reads=[('ring', slot), 'scTb'], writes=[('ps', b)])
            s.op('dve', lambda v, n=n, b=b, bi=bi: v.tensor_add(modv[:, n * 512:(n + 1) * 512], ps[b][0:2, :], btl[bi][:]),
                 reads=[('ps', b), ('bt', bi)], writes=['modv'])
        ws.finish()
        mv = lambda i: modv[:, i * D:(i + 1) * D]
        mr3 = modrows.rearrange("(c i) d -> c i d", c=2)
        plan = [(0, 1, g_pre_mix, True), (1, 0, None, False), (2, 2, g_post_mix, False),
                (3, 4, g_pre_ffn, True), (4, 3, None, False), (5, 5, g_post_ffn, False)]
        for idx, (row, chunk, gv, plus1) in enumerate(plan):
            ti = idx % 2
            if gv is not None:
                s.dma('sp', [(tg[ti][:], gv.partition_broadcast(2))], writes=[('tg', ti)], semkey=('tg', ti))
                if plus1:
                    s.op('dve', lambda v, ti=ti, chunk=chunk: v.scalar_tensor_tensor(out=tr_[ti][:], in0=mv(chunk), scalar=1.0, in1=tg[ti][:],
                                                                                    op0=ALU.add, op1=ALU.mult),
                         reads=['modv', ('tg', ti)], writes=[('tr', ti)])
                else:
                    s.op('dve', lambda v, ti=ti, chunk=chunk: v.tensor_mul(tr_[ti][:], mv(chunk), tg[ti][:]),
                         reads=['modv', ('tg', ti)], writes=[('tr', ti)])
            else:
                s.op('dve', lambda v, ti=ti, chunk=chunk: v.tensor_copy(tr_[ti][:], mv(chunk)), reads=['modv'], writes=[('tr', ti)])
            s.dma('sp', [(mr3[:, row, :], tr_[ti][:])], reads=[('tr', ti)], semkey=('trs', ti))

        lt = sbp("lt", [128, 4, HD], F32)
        for i, l in enumerate((lq1, lk1, lq2, lk2)):
            s.dma('sp', [(lt[:, i, :], l.partition_broadcast(128))], writes=[('lt', i)], semkey=('lt', i))
        for j in range(2):
            s.op('dve', lambda v, j=j: v.tensor_tensor(out=tmpf[:, j * HD:(j + 1) * HD], in0=lt[:, 2 * j, :], in1=lt[:, 2 * j + 1, :], op=ALU.mult),
                 reads=[('lt', 2 * j), ('lt', 2 * j + 1)], writes=[('tmpf', j)])
            s.op('dve', lambda v, j=j: v.reduce_sum(out=st1[:, j:j + 1], in_=tmpf[:, j * HD:(j + 1) * HD], axis=mybir.AxisListType.X),
                 reads=[('tmpf', j)], writes=[('st1', j)])
        s.op('act', lambda a: a.activation(out=st1[:, 2:4], in_=st1[:, 0:2], func=AF.Exp), reads=[('st1', 0), ('st1', 1)], writes=['st1e'])
        s.op('dve', lambda v: v.tensor_sub(lam_t[:, 0:1], st1[:, 3:4], st1[:, 2:3]), reads=['st1e'], writes=['lam_t'])
        s.op('dve', lambda v: v.tensor_scalar_add(lam_t[:, 0:1], lam_t[:, 0:1], -LAM_INIT), reads=['lam_t'], writes=['lam_t'])
        s.dma('sp', [(gsub[:], g_subln.rearrange("o (h p) -> p (o h)", p=128))], writes=['gsub'], semkey='gsub',
              allow_slow_non_contiguous=True)
        s.op('dve', lambda v: v.tensor_scalar_mul(gsub[:], gsub[:], 1.0 - LAM_INIT), reads=['gsub'], writes=['gsub'])
        s.dma('sp', [(cw[:, i_, :], conv_w[i_:i_ + 1, :].rearrange("o (c p) -> p (o c)", p=128)) for i_ in range(3)],
              writes=['cw'], semkey='cw', allow_slow_non_contiguous=True)

        I32 = mybir.dt.int32
        ang = big[:, 0:4096]
        rt1 = big[:, 4096:8192]
        rtf = big[:, 8192:12288]
        rti = rtf.bitcast(I32)
        pidx = sbp("pidx", [128, 4], F32)
        io_i = sbp("io_i", [128, 128], I32)
        io_f = sbp("io_f", [128, 128], F32)
        s.op('pool', lambda g: g.iota(io_i[:], pattern=[[0, 4], [1, 32]], base=0, channel_multiplier=0), writes=['io_i'])
        s.op('dve', lambda v: v.tensor_copy(io_f[:], io_i[:]), reads=['io_i'], writes=['io_f'])
        s.op('dve', lambda v: v.tensor_mul(io_f[:], io_f[:], ident_f[:]), reads=['io_f', 'ident_f'], writes=['io_f'])
        s.op('dve', lambda v: v.reduce_sum(out=pidx[:, 1:2], in_=io_f[:], axis=mybir.AxisListType.X), reads=['io_f'], writes=['pidx1'])
        s.op('act', lambda a: a.activation(out=pidx[:, 2:3], in_=pidx[:, 1:2], func=AF.Exp, scale=-math.log(ROPE_THETA) / 32.0),
             reads=['pidx1'], writes=['freq'])
        s.op('pool', lambda g: g.iota(io_i[:], pattern=[[0, 2], [1, 2], [0, 32]], base=0, channel_multiplier=0), reads=['io_i'], writes=['io_i'])
        s.op('dve', lambda v: v.tensor_copy(io_f[:], io_i[:]), reads=['io_i', 'io_f'], writes=['io_f'])
        s.op('dve', lambda v: v.tensor_mul(io_f[:], io_f[:], ident_f[:]), reads=['io_f', 'ident_f'], writes=['io_f'])
        s.op('dve', lambda v: v.reduce_sum(out=pidx[:, 3:4], in_=io_f[:], axis=mybir.AxisListType.X), reads=['io_f'], writes=['sgn'])
        s.op('dve', lambda v: v.tensor_scalar(out=pidx[:, 3:4], in0=pidx[:, 3:4], scalar1=2.0, scalar2=-1.0, op0=ALU.mult, op1=ALU.add),
             reads=['sgn'], writes=['sgn'])
        s.dma('sp', [(ang[0:64, :], pos[0:1, :].partition_broadcast(64)), (ang[64:128, :], pos[1:2, :].partition_broadcast(64))],
              reads=['modv'], writes=['ang', 'modv'], semkey='posb')
        s.op('dve', lambda v: v.tensor_scalar_mul(ang, ang, pidx[:, 2:3]), reads=['ang', 'freq'], writes=['ang'])

        def sin_of(shift, dst_dram, signed, key):
            s.op('dve', lambda v: v.tensor_scalar(out=rtf, in0=ang, scalar1=shift, scalar2=1.0 / (2 * math.pi), op0=ALU.add, op1=ALU.mult),
                 reads=['ang'], writes=['rtf'])
            s.op('dve', lambda v: v.tensor_copy(rt1.bitcast(I32), rtf), reads=['rtf'], writes=['rt1'])
            s.op('dve', lambda v: v.tensor_copy(rtf, rt1.bitcast(I32)), reads=['rt1'], writes=['rtf'])
            s.op('dve', lambda v: v.scalar_tensor_tensor(out=rt1, in0=rtf, scalar=-2 * math.pi, in1=ang, op0=ALU.mult, op1=ALU.add),
                 reads=['rtf', 'ang'], writes=['rt1'])
            s.op('dve', lambda v: v.tensor_scalar(out=rt1, in0=rt1, scalar1=-3.1415925 - shift, scalar2=3.1415925 - shift, op0=ALU.max, op1=ALU.min),
                 reads=['rt1'], writes=['rt1'])
            s.op('act', lambda a: a.activation(out=rt1, in_=rt1, func=AF.Sin, bias=sh_t[:, key:key + 1]), reads=['rt1', 'sh_t'], writes=['rt1'])
            if signed:
                s.op('dve', lambda v: v.tensor_scalar_mul(rt1, rt1, pidx[:, 3:4]), reads=['rt1', 'sgn'], writes=['rt1'])
            s.dma('sp', [(dst_dram, rt1)], reads=['rt1'], writes=[('rope', key)], semkey=('rope', key))

        sh_t = sbp("sh_t", [128, 2], F32)
        s.op('dve', lambda v: v.memset(sh_t[:, 0:1], 0.0), writes=['sh_t'])
        s.op('dve', lambda v: v.memset(sh_t[:, 1:2], math.pi / 2), reads=['sh_t'], writes=['sh_t'])
        sin_of(0.0, ropes, True, 0)
        sin_of(math.pi / 2, ropec, False, 1)
        s.dma('sp', [(hm[:], hmask.partition_broadcast(128))], writes=['hm'], semkey='hm')
        s.barrier()
        pes.close()
        if phases <= 1:
            return nc

        def load_bc(i, cond, row, key):
            r = cond * 6 + row
            s.dma('sp', [(bc[i][:], modrows[r:r + 1, :].partition_broadcast(128))], writes=[('bc', i)], semkey=('bc', i))

        def rstd_from_ss(col, dim=D):
            s.op('act', lambda a: a.activation(out=st1[:, col:col + 1], in_=st1[:, col:col + 1], func=AF.Sqrt, bias=epsb[:, 0:1], scale=1.0 / dim),
                 reads=[('ss', col), 'epsb'], writes=[('ss', col)])
            s.op('dve', lambda v: v.reciprocal(st1[:, col:col + 1], st1[:, col:col + 1]), reads=[('ss', col)], writes=[('ss', col)])

        def norm_mod_transpose(src_ap, src_key, npart, dst_fn, a_bc, b_bc, hb_store=None):
            P = npart
            s.op('act', lambda a: a.activation(out=hb[0:P, :], in_=src_ap, func=AF.Square, accum_out=st1[0:P, 4:5]),
                 reads=[src_key], writes=['hb', ('ss', 4)])
            rstd_from_ss(4)
            s.op('dve', lambda v: v.scalar_tensor_tensor(out=src_ap, in0=src_ap, scalar=st1[0:P, 4:5], in1=bc[a_bc][0:P, :],
                                                          op0=ALU.mult, op1=ALU.mult),
                 reads=[('ss', 4), ('bc', a_bc)], writes=[src_key])
            s.op('dve', lambda v: v.tensor_add(hb[0:P, :], src_ap, bc[b_bc][0:P, :]), reads=[src_key, ('bc', b_bc)], writes=['hb'])
            if hb_store is not None:
                s.op('dve', lambda v: v.tensor_copy(hbp[0:P, :].rearrange("p (k q) -> p k q", k=16), hb[0:P, :].rearrange("p (q k) -> p k q", k=16)),
                     reads=['hb'], writes=['hbp'])
                s.dma('sp', [(hb_store, hbp[0:P, :])], reads=['hbp'], semkey='hbst')
            transpose16(hb, 'hb', P, dst_fn)

        def transpose16(src, src_key, P, dst_fn):
            for half in range(2):
                b = psn([6, 7])
                pst = ps[b].bitcast(BF16)

                def tr(pe, half=half, pst=pst):
                    last = None
                    for j in range(8):
                        k = half * 8 + j
                        last = pe.transpose(pst[:, j * 128:j * 128 + P], src[0:P, k * 128:(k + 1) * 128], ident[0:P, 0:P])
                    return last
                s.op('pe', tr, reads=[src_key, 'ident'], writes=[('ps', b)])
                dst, dkeys = dst_fn(half)
                s.op('act', lambda a, pst=pst, dst=dst: a.copy(dst, pst.rearrange("p (j t) -> p j t", j=8)[:, :, 0:P]),
                     reads=[('ps', b)], writes=dkeys)

        tiles2 = [
            dict(name='P', T=512, x=xp, x0=0, cond=0, rope=None, full=True, own0=0, kdst=(kTp, 0), vdst=(vp, 0),
                 segs=[(0, 256), (256, 512)], halo=None, outkv=True),
            dict(name='S0', T=1024, x=xs, x0=0, cond=1, rope=0, full=True, own0=512, kdst=(kTs, 0), vdst=(vs, 0),
                 segs=[(0, 1024)], halo=((xh, 0), (xs, 1024), 0), outkv=False),
            dict(name='S1', T=1024, x=xs, x0=1024, cond=1, rope=1024, full=True, own0=1536, kdst=(kTs, 1024), vdst=(vs, 1024),
                 segs=[(0, 1024)], halo=((xs, 1023), (xh, 1), 2), outkv=False),
            dict(name='O0', T=1024, x=xs, x0=2048, cond=1, rope=2048, full=False, own0=None, kdst=(kTs, 2048), vdst=(vs, 2048),
                 segs=None, halo=None, outkv=False),
            dict(name='O1', T=1024, x=xs, x0=3072, cond=1, rope=3072, full=False, own0=None, kdst=(kTs, 3072), vdst=(vs, 3072),
                 segs=None, halo=None, outkv=False),
        ]
        pes = ExitStack()
        sbp = lambda name, shape, dt: pes.enter_context(nc.sbuf_tensor(name + '_p2', list(shape), dt))
        ring = [sbp("ring%d" % i, [128, 8192], BF16) for i in range(4)]
        ws = WS(ring)
        hT = sbp("hT", [128, 16, 1024], BF16)
        hTh = sbp("hTh", [128, 16, 2], BF16)
        bc = [sbp("bc%d" % i, [128, D], F32) for i in range(2)]
        xt = [sbp("xt%d" % i, [128, D], F32) for i in range(2)]
        hb = sbp("hb", [128, D], BF16)
        stage = [sbp("stage%d" % i, [128, 512], BF16) for i in range(4)]
        stagef = [sbp("stagef%d" % i, [128, 512], F32) for i in range(3)]
        ropeC = sbp("ropeC", [128, 1024], F32)
        ropeS = sbp("ropeS", [128, 1024], F32)
        ccu = sbp("ccu", [128, 4, 1026], F32)
        yv = sbp("yv", [128, 1024], F32)
        convT = sbp("convT", [128, 8, 1024], BF16)
        hbv = sbp("sgcb", [128, 4, 1024], BF16)
        xh2 = sbp("xh2", [2, D], F32)

        def stg():
            i = stc[0] % len(stage)
            stc[0] += 1
            return i

        def stgf():
            i = stfc[0] % len(stagef)
            stfc[0] += 1
            return i
        xcnt = [0]

        for tl in tiles2:
            if only_tiles is not None and tl['name'] not in only_tiles:
                continue
            T = tl['T']
            nb = T // 128
            nm = T // 512
            cond = tl['cond']
            full = tl['full']
            load_bc(0, cond, 0, 'A1')
            load_bc(1, cond, 1, 'B1')
            if tl['rope'] is not None:
                r0 = tl['rope']
                s.dma('sp', [(ropeC[:, 0:T], ropec[:, r0:r0 + T])], writes=['ropeC'], semkey='ropeC')
                s.dma('sp', [(ropeS[:, 0:T], ropes[:, r0:r0 + T])], writes=['ropeS'], semkey='ropeS')
            for i in range(nb):
                xi = xcnt[0] % 2
                xcnt[0] += 1
                r = tl['x0'] + i * 128
                s.dma('sp', [(xt[xi][:], tl['x'][r:r + 128, :])], writes=[('xt', xi)], semkey=('xt', xi))
                norm_mod_transpose(xt[xi][:], ('xt', xi), 128,
                                   lambda half, i=i: (hT[:, half * 8:(half + 1) * 8, i * 128:(i + 1) * 128], [('hT', i, half)]),
                                   0, 1)
            hT_keys = [('hT', i, h) for i in range(nb) for h in range(2)]
            if tl['halo'] is not None:
                (lsrc, lrow), (rsrc, rrow), hmc = tl['halo']
                s.dma('sp', [(xh2[0:1, :], lsrc[lrow:lrow + 1, :]), (xh2[1:2, :], rsrc[rrow:rrow + 1, :])], writes=['xh2'], semkey='xh2')
                norm_mod_transpose(xh2[:], 'xh2', 2,
                                   lambda half: (hTh[:, half * 8:(half + 1) * 8, :], [('hTh', half)]), 0, 1)
            hTh_keys = [('hTh', 0), ('hTh', 1)]

            if full:
                order = [('q', c) for c in range(4)] + [('k', c) for c in range(4, 8)] + [('v', c) for c in range(8, 12)]
                for j in range(2):
                    order += [('cc', 14 + j), ('cx', 16 + j), ('cb', 12 + j)]
                order += [('ga', c) for c in range(18, 22)]
                for c in range(22, 26):
                    order += [('gc', c), ('wco', c - 22)]
            else:
                order = [('k', c) for c in range(4, 8)] + [('v', c) for c in range(8, 12)]
            if cut is not None:
                order = order[:cut]
            for kind, c in order:
                if kind == 'wco':
                    ws.add(wsrc(w_co, c * 512, 512), 8)
                else:
                    ws.add(wsrc(w_in, c * 512, 512), 16)
            sgc_stage = None
            for kind, c in order:
                slot, wv = ws.get()
                wkey = ('ring', slot)
                if kind in ('q', 'k'):
                    for sc in range(4):
                        prow = ((c % 4) * 4 + sc) * 128
                        for m in range(nm):
                            b = psn([0, 1, 2, 3])
                            s.op('pe', lambda pe, b=b, sc=sc, m=m, wv=wv: mm_group(
                                pe, ps[b][:], [(wv[:, k, sc * 128:(sc + 1) * 128], hT[:, k, m * 512:(m + 1) * 512]) for k in range(16)]),
                                reads=[wkey] + hT_keys, writes=[('ps', b)])
                            si = stg()
                            if tl['rope'] is None:
                                s.op('act', lambda a, b=b, si=si: a.copy(stage[si][:], ps[b][:]), reads=[('ps', b)], writes=[('stage', si)])
                            else:
                                sx = stg()
                                s.op('act', lambda a, b=b, sx=sx: a.copy(stage[sx][:], ps[b][:]), reads=[('ps', b)], writes=[('stage', sx)])
                                b2 = psn([4, 5])
                                s.op('pe', lambda pe, b2=b2, sx=sx: pe.matmul(ps[b2][:], lhsT=pmat[:], rhs=stage[sx][:], start=True, stop=True),
                                     reads=[('stage', sx), 'pmat'], writes=[('ps', b2)])
                                f1 = stgf()
                                f2 = stgf()
                                s.op('dve', lambda v, b=b, f1=f1, m=m: v.tensor_mul(stagef[f1][:], ps[b][:], ropeC[:, m * 512:(m + 1) * 512]),
                                     reads=[('ps', b), 'ropeC'], writes=[('stagef', f1)])
                                s.op('dve', lambda v, b2=b2, f2=f2, m=m: v.tensor_mul(stagef[f2][:], ps[b2][:], ropeS[:, m * 512:(m + 1) * 512]),
                                     reads=[('ps', b2), 'ropeS'], writes=[('stagef', f2)])
                                s.op('dve', lambda v, f1=f1, f2=f2, si=si: v.tensor_add(stage[si][:], stagef[f1][:], stagef[f2][:]),
                                     reads=[('stagef', f1), ('stagef', f2)], writes=[('stage', si)])
                            if kind == 'q':
                                c0 = tl['own0'] + m * 512
                                s.dma('sp', [(qT[prow:prow + 128, c0:c0 + 512], stage[si][:])], reads=[('stage', si)], semkey=('stq', si))
                            else:
                                kd, k0 = tl['kdst']
                                c0 = k0 + m * 512
                                s.dma('sp', [(kd[prow:prow + 128, c0:c0 + 512], stage[si][:])], reads=[('stage', si)], semkey=('stq', si))
                    if kind == 'k' and tl['outkv']:
                        for i in range(nb):
                            b = psn([0, 1, 2, 3])
                            s.op('pe', lambda pe, b=b, i=i, wv=wv: mm_group(
                                pe, ps[b][:], [(hT[:, k, i * 128:(i + 1) * 128], wv[:, k, :]) for k in range(16)]),
                                reads=[wkey] + hT_keys, writes=[('ps', b)])
                            f1 = stgf()
                            s.op('act', lambda a, b=b, f1=f1: a.copy(stagef[f1][:], ps[b][:]), reads=[('ps', b)], writes=[('stagef', f1)])
                            cc0 = (c - 4) * 512
                            s.dma('sp', [(nk[i * 128:(i + 1) * 128, cc0:cc0 + 512], stagef[f1][:])], reads=[('stagef', f1)], semkey=('stf', f1))
                elif kind == 'v':
                    vd, v0 = tl['vdst']
                    cc0 = (c - 8) * 512
                    for i in range(nb):
                        b = psn([0, 1, 2, 3])
                        s.op('pe', lambda pe, b=b, i=i, wv=wv: mm_group(
                            pe, ps[b][:], [(hT[:, k, i * 128:(i + 1) * 128], wv[:, k, :]) for k in range(16)]),
                            reads=[wkey] + hT_keys, writes=[('ps', b)])
                        si = stg()
                        r = v0 + i * 128
                        if tl['outkv']:
                            f1 = stgf()
                            s.op('act', lambda a, b=b, f1=f1: a.copy(stagef[f1][:], ps[b][:]), reads=[('ps', b)], writes=[('stagef', f1)])
                            s.dma('sp', [(nv[i * 128:(i + 1) * 128, cc0:cc0 + 512], stagef[f1][:])], reads=[('stagef', f1)], semkey=('stf', f1))
                            s.op('dve', lambda v, si=si, f1=f1: v.tensor_copy(stage[si][:], stagef[f1][:]), reads=[('stagef', f1)], writes=[('stage', si)])
                        else:
                            s.op('act', lambda a, b=b, si=si: a.copy(stage[si][:], ps[b][:]), reads=[('ps', b)], writes=[('stage', si)])
                        s.dma('sp', [(vd[r:r + 128, cc0:cc0 + 512], stage[si][:])], reads=[('stage', si)], semkey=('stq', si))
                elif kind in ('cc', 'cx'):
                    for sc in range(4):
                        for m in range(nm):
                            b = psn([0, 1, 2, 3])
                            s.op('pe', lambda pe, b=b, sc=sc, m=m, wv=wv: mm_group(
                                pe, ps[b][:], [(wv[:, k, sc * 128:(sc + 1) * 128], hT[:, k, m * 512:(m + 1) * 512]) for k in range(16)]),
                                reads=[wkey] + hT_keys, writes=[('ps', b)])
                            dst = ccu[:, sc, 1 + m * 512:1 + (m + 1) * 512]
                            if kind == 'cc':
                                s.op('act', lambda a, b=b, dst=dst: a.copy(dst, ps[b][:]), reads=[('ps', b)], writes=[('ccu', sc, m)])
                            else:
                                s.op('dve', lambda v, b=b, dst=dst: v.tensor_mul(dst, dst, ps[b][:]), reads=[('ps', b), ('ccu', sc, m)],
                                     writes=[('ccu', sc, m)])
                        for hc, col in ((0, 0), (1, T + 1)):
                            dsth = ccu[:, sc, col:col + 1]
                            if tl['halo'] is None:
                                if kind == 'cc':
                                    s.op('dve', lambda v, dsth=dsth: v.memset(dsth, 0.0), writes=[('ccuh', sc, hc)])
                                continue
                            b = psn([0, 1, 2, 3])
                            s.op('pe', lambda pe, b=b, sc=sc, hc=hc, wv=wv: mm_group(
                                pe, ps[b][:, 0:1], [(wv[:, k, sc * 128:(sc + 1) * 128], hTh[:, k, hc:hc + 1]) for k in range(16)]),
                                reads=[wkey] + hTh_keys, writes=[('ps', b)])
                            if kind == 'cc':
                                s.op('act', lambda a, b=b, dsth=dsth: a.copy(dsth, ps[b][:, 0:1]), reads=[('ps', b)], writes=[('ccuh', sc, hc)])
                            else:
                                mcol = tl['halo'][2] + hc
                                s.op('dve', lambda v, b=b, dsth=dsth, mcol=mcol: v.scalar_tensor_tensor(
                                    out=dsth, in0=ps[b][:, 0:1], scalar=hm[:, mcol:mcol + 1], in1=dsth, op0=ALU.mult, op1=ALU.mult),
                                    reads=[('ps', b), ('ccuh', sc, hc), 'hm'], writes=[('ccuh', sc, hc)])
                elif kind == 'cb':
                    j = c - 12
                    for sc in range(4):
                        ch = j * 4 + sc
                        ukeys = [('ccu', sc, m) for m in range(nm)] + [('ccuh', sc, 0), ('ccuh', sc, 1)]
                        u = ccu[:, sc, :]
                        first = True
                        for (a0, b0) in tl['segs']:
                            has_halo = tl['halo'] is not None
                            s.op('dve', lambda v, a0=a0, b0=b0, ch=ch, u=u: v.tensor_scalar(
                                out=yv[:, a0:b0], in0=u[:, 1 + a0:1 + b0], scalar1=cw[:, 1, ch:ch + 1], scalar2=None, op0=ALU.mult),
                                reads=ukeys + ['cw'], writes=['yv'])
                            la = a0 if has_halo else a0 + 1
                            s.op('dve', lambda v, la=la, b0=b0, ch=ch, u=u: v.scalar_tensor_tensor(
                                out=yv[:, la:b0], in0=u[:, la:b0], scalar=cw[:, 0, ch:ch + 1], in1=yv[:, la:b0], op0=ALU.mult, op1=ALU.add),
                                reads=ukeys + ['cw', 'yv'], writes=['yv'])
                            rb = b0 if has_halo else b0 - 1
                            s.op('dve', lambda v, a0=a0, rb=rb, ch=ch, u=u: v.scalar_tensor_tensor(
                                out=yv[:, a0:rb], in0=u[:, a0 + 2:rb + 2], scalar=cw[:, 2, ch:ch + 1], in1=yv[:, a0:rb], op0=ALU.mult, op1=ALU.add),
                                reads=ukeys + ['cw', 'yv'], writes=['yv'])
                        for m in range(nm):
                            b = psn([0, 1, 2, 3])
                            s.op('pe', lambda pe, b=b, sc=sc, m=m, wv=wv: mm_group(
                                pe, ps[b][:], [(wv[:, k, sc * 128:(sc + 1) * 128], hT[:, k, m * 512:(m + 1) * 512]) for k in range(16)]),
                                reads=[wkey] + hT_keys, writes=[('ps', b)])
                            s.op('dve', lambda v, b=b, ch=ch, m=m: v.tensor_mul(convT[:, ch, m * 512:(m + 1) * 512], ps[b][:], yv[:, m * 512:(m + 1) * 512]),
                                 reads=[('ps', b), 'yv'], writes=[('convT', ch, m)])
                elif kind in ('ga', 'gc'):
                    if kind == 'gc':
                        sgc_stage = {}
                    for sc in range(4):
                        drow = ((c - (18 if kind == 'ga' else 22)) * 4 + sc) * 128
                        for m in range(nm):
                            b = psn([0, 1, 2, 3])
                            s.op('pe', lambda pe, b=b, sc=sc, m=m, wv=wv: mm_group(
                                pe, ps[b][:], [(wv[:, k, sc * 128:(sc + 1) * 128], hT[:, k, m * 512:(m + 1) * 512]) for k in range(16)]),
                                reads=[wkey] + hT_keys, writes=[('ps', b)])
                            if kind == 'ga':
                                si = stg()
                                s.op('act', lambda a, b=b, si=si: a.activation(out=stage[si][:], in_=ps[b][:], func=AF.Sigmoid),
                                     reads=[('ps', b)], writes=[('stage', si)])
                                c0 = tl['own0'] + m * 512
                                s.dma('sp', [(sgaT[drow:drow + 128, c0:c0 + 512], stage[si][:])], reads=[('stage', si)], semkey=('stq', si))
                            else:
                                dst = hbv[:, sc, m * 512:(m + 1) * 512]
                                s.op('act', lambda a, b=b, dst=dst: a.activation(out=dst, in_=ps[b][:], func=AF.Sigmoid),
                                     reads=[('ps', b)], writes=[('sgc', sc, m)])
                elif kind == 'wco':
                    cvkeys = [('convT', ch, m) for ch in range(8) for m in range(nm)]
                    for sc in range(4):
                        drow = (c * 4 + sc) * 128
                        for m in range(nm):
                            b = psn([0, 1, 2, 3])
                            s.op('pe', lambda pe, b=b, sc=sc, m=m, wv=wv: mm_group(
                                pe, ps[b][:], [(wv[:, k, sc * 128:(sc + 1) * 128], convT[:, k, m * 512:(m + 1) * 512]) for k in range(8)]),
                                reads=[wkey] + cvkeys, writes=[('ps', b)])
                            si = stg()
                            s.op('dve', lambda v, b=b, si=si, sc=sc, m=m: v.tensor_mul(stage[si][:], ps[b][:], hbv[:, sc, m * 512:(m + 1) * 512]),
                                 reads=[('ps', b), ('sgc', sc, m)], writes=[('stage', si)])
                            c0 = tl['own0'] + m * 512
                            s.dma('sp', [(mcT[drow:drow + 128, c0:c0 + 512], stage[si][:])], reads=[('stage', si)], semkey=('stq', si))
            ws.finish()
        s.barrier()
        pes.close()
        if phases <= 2:
            return nc

        pes = ExitStack()
        sbp = lambda name, shape, dt: pes.enter_context(nc.sbuf_tensor(name + '_p3', list(shape), dt))
        NKMAX = 4096 + PAST
        kTh = [sbp("kTh%d" % i, [128, 2, NKMAX], BF16) for i in range(2)]
        vh = [sbp("vh%d" % i, [128, 32, VD], BF16) for i in range(2)]
        qh = [sbp("qh%d" % i, [128, 2, 2048], BF16) for i in range(2)]
        ckb = sbp("ckb", [128, 4, 2048], BF16)
        cvb = sbp("cvb", [128, 4, 2048], BF16)
        pT = [sbp("pT%d" % i, [128, 512], BF16) for i in range(4)]
        onrm = sbp("onrm", [128, 2, 2, 512], F32)
        rl = sbp("rl", [128, 512], F32)
        av = sbp("av", [128, 2, 512], F32)
        sq = sbp("sq", [128, 2, 512], BF16)
        rs3 = sbp("rs3", [128, 512], F32)
        ost = [sbp("ost%d" % i, [128, 512], BF16) for i in range(2)]
        zt = sbp("zt", [128, 4, D], BF16)
        s.op('dve', lambda v: v.memset(zt[:], 0.0), writes=['zt'])
        xsv = xsort.rearrange("(b i p) d -> b p i d", p=128, i=4)
        zq = list(range(NBLK))

        def zero_some(n):
            for _ in range(n):
                if zq:
                    s.dma('sp', [(xsv[zq.pop(0)], zt[:])], reads=['zt'], semkey='ztst')
        s.dma('pool', [(ckb[:], ck.rearrange("(b p) n -> p b n", p=128))], writes=['ckb'], semkey='ckb')
        s.dma('pool', [(cvb[:], cv.rearrange("(b p) n -> p b n", p=128))], writes=['cvb'], semkey='cvb')
        SCALE = HD ** -0.5
        seqs = [dict(q0=0, nq=256, QT=256, ksrc=kTp, k0=0, vsrc=vp, nkb=2, cache=False),
                dict(q0=256, nq=256, QT=256, ksrc=kTp, k0=256, vsrc=vp, nkb=2, cache=False),
                dict(q0=512, nq=2048, QT=512, ksrc=kTs, k0=0, vsrc=vs, nkb=32, cache=True)]
        jobs = [(sq_, h) for sq_ in seqs for h in range(NH)]
        if only_tiles is not None:
            jobs = [jb for jb in jobs if (('P' in only_tiles and not jb[0]['cache']) or ('S0' in only_tiles and jb[0]['cache']))]
            if cut is not None:
                jobs = jobs[:cut]

        def attn_load(ji):
            sd, h = jobs[ji]
            bi = ji % 2
            nk_ = sd['nkb'] * 128
            s.dma('sp', [(kTh[bi][:, j, 0:nk_], sd['ksrc'][(j * NH + h) * 128:(j * NH + h + 1) * 128, sd['k0']:sd['k0'] + nk_]) for j in range(2)],
                  writes=[('kTh', bi)], semkey=('kTh', bi))
            s.dma('sp', [(vh[bi][:, 0:sd['nkb'], :], sd['vsrc'][sd['k0']:sd['k0'] + nk_, h * VD:(h + 1) * VD].rearrange("(kb p) e -> p kb e", p=128))],
                  writes=[('vh', bi)], semkey=('vh', bi))
            s.dma('sp', [(qh[bi][:, j, 0:sd['nq']], qT[(j * NH + h) * 128:(j * NH + h + 1) * 128, sd['q0']:sd['q0'] + sd['nq']]) for j in range(2)],
                  writes=[('qh', bi)], semkey=('qh', bi))

        pcnt = [0]
        ocnt = [0]
        if jobs:
            attn_load(0)
        for ji, (sd, h) in enumerate(jobs):
            bi = ji % 2
            if ji + 1 < len(jobs):
                attn_load(ji + 1)
            zero_some(5)
            nkb = sd['nkb'] + (4 if sd['cache'] else 0)
            QT = sd['QT']
            if sd['cache']:
                b = psn([6, 7])
                pst = ps[b].bitcast(BF16)

                def trc(pe, pst=pst, h=h):
                    last = None
                    for j in range(2):
                        for blk in range(4):
                            c0 = (j * NH + h) * 128
                            last = pe.transpose(pst[:, (j * 4 + blk) * 128:(j * 4 + blk + 1) * 128], ckb[:, blk, c0:c0 + 128], ident[:])
                    return last
                s.op('pe', trc, reads=['ckb', 'ident'], writes=[('ps', b)])
                s.op('act', lambda a, pst=pst, bi=bi: a.copy(kTh[bi][:, :, 4096:4096 + PAST], pst.rearrange("p (j t) -> p j t", j=2)),
                     reads=[('ps', b), ('kTh', bi)], writes=[('kThc', bi)])
            kkeys = [('kTh', bi), ('kThc', bi)]

            def vblk(kb, half, bi=bi, sd=sd, h=h):
                if kb < sd['nkb']:
                    return vh[bi][:, kb, half * 128:(half + 1) * 128]
                return cvb[:, kb - sd['nkb'], h * VD + half * 128:h * VD + (half + 1) * 128]
            for qt in range(sd['nq'] // QT):
                qsl = slice(qt * QT, (qt + 1) * QT)
                for j in range(2):
                    accb = [0, 1, 2] if j == 0 else [3, 4, 5]

                    def emit_s(kb, j=j, qsl=qsl, bi=bi):
                        sbk = psn([6, 7])
                        s.op('pe', lambda pe: pe.matmul(ps[sbk][:, 0:QT], lhsT=kTh[bi][:, j, kb * 128:(kb + 1) * 128], rhs=qh[bi][:, j, qsl],
                                                        start=True, stop=True),
                             reads=kkeys + [('qh', bi)], writes=[('ps', sbk)])
                        pi = pcnt[0] % 4
                        pcnt[0] += 1
                        s.op('act', lambda a: a.activation(out=pT[pi][:, 0:QT], in_=ps[sbk][:, 0:QT], func=AF.Exp, scale=SCALE),
                             reads=[('ps', sbk)], writes=[('pT', pi)])
                        return pi
                    pis = {0: emit_s(0)}
                    for kb in range(nkb):
                        if kb + 1 < nkb:
                            pis[kb + 1] = emit_s(kb + 1)
                        pi = pis.pop(kb)

                        def pv(pe, kb=kb, pi=pi):
                            st_, sp_ = (kb == 0), (kb == nkb - 1)
                            pe.matmul(ps[accb[0]][:, 0:QT], lhsT=vblk(kb, 0), rhs=pT[pi][:, 0:QT], start=st_, stop=sp_)
                            pe.matmul(ps[accb[1]][:, 0:QT], lhsT=vblk(kb, 1), rhs=pT[pi][:, 0:QT], start=st_, stop=sp_)
                            return pe.matmul(ps[accb[2]][:, 0:QT], lhsT=ones_b[:], rhs=pT[pi][:, 0:QT], start=st_, stop=sp_)
                        s.op('pe', pv, reads=[('pT', pi), ('vh', bi), 'cvb', 'ones_b'], writes=[('ps', accb[0]), ('ps', accb[1]), ('ps', accb[2])])
                    s.op('dve', lambda v: v.reciprocal(rl[:, 0:QT], ps[accb[2]][:, 0:QT]), reads=[('ps', accb[2])], writes=['rl'])
                    for half in range(2):
                        s.op('dve', lambda v, half=half, j=j: v.tensor_mul(onrm[:, j, half, 0:QT], ps[accb[half]][:, 0:QT], rl[:, 0:QT]),
                             reads=[('ps', accb[half]), 'rl'], writes=[('onrm', j, half)])
                for half in range(2):
                    s.op('dve', lambda v, half=half: v.scalar_tensor_tensor(out=av[:, half, 0:QT], in0=onrm[:, 1, half, 0:QT], scalar=lam_t[:, 0:1],
                                                                            in1=onrm[:, 0, half, 0:QT], op0=ALU.mult, op1=ALU.add),
                         reads=[('onrm', 1, half), ('onrm', 0, half), 'lam_t'], writes=[('av', half)])
                    s.op('dve', lambda v, half=half: v.tensor_mul(sq[:, half, 0:QT], av[:, half, 0:QT], av[:, half, 0:QT]),
                         reads=[('av', half)], writes=[('sq', half)])
                sbk = psn([6, 7])
                s.op('pe', lambda pe, sbk=sbk: mm_group(pe, ps[sbk][:, 0:QT], [(ones_b[:], sq[:, hf, 0:QT]) for hf in range(2)]),
                     reads=[('sq', 0), ('sq', 1), 'ones_b'], writes=[('ps', sbk)])
                s.op('act', lambda a, sbk=sbk: a.activation(out=rs3[:, 0:QT], in_=ps[sbk][:, 0:QT], func=AF.Sqrt, bias=epsb[:, 0:1], scale=1.0 / VD),
                     reads=[('ps', sbk), 'epsb'], writes=['rs3'])
                s.op('dve', lambda v: v.reciprocal(rs3[:, 0:QT], rs3[:, 0:QT]), reads=['rs3'], writes=['rs3'])
                for half in range(2):
                    oi = ocnt[0] % 2
                    ocnt[0] += 1
                    s.op('dve', lambda v, half=half, oi=oi: v.scalar_tensor_tensor(out=ost[oi][:, 0:QT], in0=av[:, half, 0:QT], scalar=gsub[:, half:half + 1],
                                                                                   in1=rs3[:, 0:QT], op0=ALU.mult, op1=ALU.mult),
                         reads=[('av', half), 'rs3', 'gsub'], writes=[('ost', oi)])
                    r0 = h * VD + half * 128
                    c0 = sd['q0'] + qt * QT
                    s.dma('sp', [(attnT[r0:r0 + 128, c0:c0 + QT], ost[oi][:, 0:QT])], reads=[('ost', oi)], semkey=('ost', oi))
        zero_some(NBLK)
        s.barrier()
        pes.close()
        if phases <= 3:
            return nc

        combA = sb("combA", [128, NTB, NE], F32)
        selA = sb("selA", [128, NTB, NE], BF16)
        s.op('dve', lambda v: v.memset(combA[:], 0.0), writes=['combA0'])
        s.op('dve', lambda v: v.memset(selA[:], 0.0), writes=['selA0'])
        pes = ExitStack()
        sbp = lambda name, shape, dt: pes.enter_context(nc.sbuf_tensor(name + '_p4', list(shape), dt))
        ring = [sbp("ring%d" % i, [128, 8192], BF16) for i in range(4)]
        ws = WS(ring)
        big = sbp("big", [128, 8, D], F32)
        big_bf = big[:].rearrange("p a d -> p (a d)").bitcast(BF16)
        hT = sbp("hT", [128, 16, 1024], BF16)
        bc = [sbp("bc%d" % i, [128, D], F32) for i in range(2)]
        hb = sbp("hb", [128, D], BF16)
        stagef = [sbp("stagef%d" % i, [128, 512], F32) for i in range(1)]
        wr = sbp("wr", [128, 16, NE], BF16)
        rbias = sbp("rbias", [128, NE], F32)
        rt = sbp("rt", [128, 6, NE], F32)
        m8 = sbp("m8", [128, 8, 8], F32)
        g8 = sbp("g8", [128, 4, 8], F32)
        s.dma('pool', [(wr[:], w_router.rearrange("(k p) n -> p k n", p=128))], writes=['wr'], semkey='wr')
        s.dma('sp', [(rbias[:], router_bias.partition_broadcast(128))], writes=['rbias'], semkey='rbias')
        tiles4 = [dict(name='P', T=512, x=xp, x0=0, cond=0, own0=0, y=yp, y0=0),
                  dict(name='S0', T=1024, x=xs, x0=0, cond=1, own0=512, y=ys, y0=0),
                  dict(name='S1', T=1024, x=xs, x0=1024, cond=1, own0=1536, y=ys, y0=1024)]
        for tl in tiles4:
            if only_tiles is not None and tl['name'] not in only_tiles:
                continue
            T = tl['T']
            nb = T // 128
            nm = T // 512
            own0 = tl['own0']
            cond = tl['cond']
            ls = ExitStack()
            sbl = lambda name, shape, dt: ls.enter_context(nc.sbuf_tensor(name + '_4a' + tl['name'], list(shape), dt))
            gbuf = [sbl("gbuf%d" % i, [128, 1024], BF16) for i in range(2)]
            mbuf = [sbl("mbuf%d" % i, [128, 1024], BF16) for i in range(1)]
            aT = big_bf[:, 0:16 * T].rearrange("p (k t) -> p k t", k=16)
            s.dma('sp', [(aT, attnT[:, own0:own0 + T].rearrange("(k p) t -> p k t", p=128))], writes=['aT'], semkey='aT')
            for c in range(4):
                ws.add(wsrc(w_ao, c * 512, 512), 16)
            for c in range(4):
                ws.add(wsrc(w_o, c * 512, 512), 16)
            gcnt = 0
            for c in range(4):
                slot, wv = ws.get()
                for sc in range(4):
                    drow = (c * 4 + sc) * 128
                    gi = gcnt % 2
                    gcnt += 1
                    s.dma('sp', [(gbuf[gi][:, 0:T], sgaT[drow:drow + 128, own0:own0 + T])], writes=[('gbuf', gi)], semkey=('gbuf', gi))
                    s.dma('sp', [(mbuf[0][:, 0:T], mcT[drow:drow + 128, own0:own0 + T])], writes=[('mbuf', 0)], semkey=('mbuf', 0))
                    for m in range(nm):
                        b = psn([0, 1, 2, 3])
                        s.op('pe', lambda pe, b=b, sc=sc, m=m, wv=wv: mm_group(
                            pe, ps[b][:], [(wv[:, k, sc * 128:(sc + 1) * 128], aT[:, k, m * 512:(m + 1) * 512]) for k in range(16)]),
                            reads=[('ring', slot), 'aT'], writes=[('ps', b)])
                        f1 = stgf()
                        s.op('dve', lambda v, b=b, f1=f1, gi=gi, m=m: v.tensor_mul(stagef[f1][:], ps[b][:], gbuf[gi][:, m * 512:(m + 1) * 512]),
                             reads=[('ps', b), ('gbuf', gi)], writes=[('stagef', f1)])
                        s.op('dve', lambda v, f1=f1, gi=gi, m=m, c=c, sc=sc: v.tensor_add(hT[:, c * 4 + sc, m * 512:(m + 1) * 512], stagef[f1][:],
                                                                                      mbuf[0][:, m * 512:(m + 1) * 512]),
                             reads=[('stagef', f1), ('mbuf', 0)], writes=[('hT', c * 4 + sc, m)])
            s.barrier()
            ls.close()
            ls = ExitStack()
            sbl = lambda name, shape, dt: ls.enter_context(nc.sbuf_tensor(name + '_4c' + tl['name'], list(shape), dt))
            xt4 = [sbl("xt%d" % i, [128, D], F32) for i in range(1)]
            hbp = sbl("hbp", [128, D], BF16)
            for c in range(4):
                slot, wv = ws.get()
                for i in range(nb):
                    b = psn([0, 1, 2, 3])
                    s.op('pe', lambda pe, b=b, i=i, wv=wv: mm_group(
                        pe, ps[b][:], [(hT[:, k, i * 128:(i + 1) * 128], wv[:, k, :]) for k in range(16)]),
                        reads=[('ring', slot)], writes=[('ps', b)])
                    s.op('act', lambda a, b=b, i=i, c=c: a.copy(big[:, i, c * 512:(c + 1) * 512], ps[b][:]), reads=[('ps', b)], writes=[('o1', i)])
            ws.finish()
            s.barrier()
            load_bc(0, cond, 2, 'G1')
            for i in range(nb):
                r = tl['x0'] + i * 128
                s.dma('sp', [(xt4[0][:], tl['x'][r:r + 128, :])], writes=[('xt', 0)], semkey=('xt4', 0))
                s.op('act', lambda a, i=i: a.activation(out=hb[:], in_=big[:, i, :], func=AF.Square, accum_out=st1[:, 5:6]),
                     reads=[('o1', i)], writes=['hb', ('ss', 5)])
                rstd_from_ss(5)
                s.op('dve', lambda v, i=i: v.scalar_tensor_tensor(out=big[:, i, :], in0=big[:, i, :], scalar=st1[:, 5:6], in1=bc[0][:],
                                                                  op0=ALU.mult, op1=ALU.mult),
                     reads=[('ss', 5), ('bc', 0)], writes=[('o1', i)])
                s.op('dve', lambda v, i=i: v.tensor_add(big[:, i, :], big[:, i, :], xt4[0][:]), reads=[('xt', 0)], writes=[('o1', i)])
                s.dma('sp', [(x1d[own0 + i * 128:own0 + (i + 1) * 128, :], big[:, i, :])], reads=[('o1', i)], writes=[('x1d', i)], semkey=('x1s', i))
            load_bc(1, cond, 3, 'A2')
            load_bc(0, cond, 4, 'B2')
            for i in range(nb):
                norm_mod_transpose(big[:, i, :], ('o1', i), 128,
                                   lambda half, i=i: (hT[:, half * 8:(half + 1) * 8, i * 128:(i + 1) * 128], [('h2T', i, half)]), 1, 0,
                                   hb_store=h2d[own0 + i * 128:own0 + (i + 1) * 128, :])
                b = psn([4, 5])
                s.op('pe', lambda pe, b=b, i=i: mm_group(pe, ps[b][:, 0:NE], [(hT[:, k, i * 128:(i + 1) * 128], wr[:, k, :]) for k in range(16)]),
                     reads=['wr', ('h2T', i, 0), ('h2T', i, 1)], writes=[('ps', b)])
                sg_, bi_, mk_, sel_, w_, pen_ = [rt[:, q_, :] for q_ in range(6)]
                s.op('act', lambda a, b=b: a.activation(out=sg_, in_=ps[b][:, 0:NE], func=AF.Sigmoid), reads=[('ps', b)], writes=['rt_sg'])
                s.op('dve', lambda v: v.tensor_add(bi_, sg_, rbias[:]), reads=['rt_sg', 'rbias'], writes=['rt_bi'])
                for g in range(8):
                    s.op('dve', lambda v, g=g: v.max(out=m8[:, g, :], in_=bi_[:, g * 8:(g + 1) * 8]), reads=['rt_bi'], writes=[('m8', g)])
                s.op('dve', lambda v: v.tensor_add(g8[:, 0, :], m8[:, :, 0], m8[:, :, 1]), reads=[('m8', g) for g in range(8)], writes=['g8_0'])
                s.op('dve', lambda v: v.max(out=g8[:, 1, :], in_=g8[:, 0, :]), reads=['g8_0'], writes=['g8_1'])
                s.op('dve', lambda v: v.tensor_scalar(out=g8[:, 2, :], in0=g8[:, 0, :], scalar1=g8[:, 1, 3:4], scalar2=None, op0=ALU.is_ge),
                     reads=['g8_0', 'g8_1'], writes=['g8_2'])
                s.op('dve', lambda v: v.tensor_scalar(out=g8[:, 3, :], in0=g8[:, 2, :], scalar1=-1.0, scalar2=1e30, op0=ALU.add, op1=ALU.mult),
                     reads=['g8_2'], writes=['g8_3'])
                for g in range(8):
                    s.op('dve', lambda v, g=g: v.tensor_scalar(out=mk_[:, g * 8:(g + 1) * 8], in0=bi_[:, g * 8:(g + 1) * 8], scalar1=g8[:, 2, g:g + 1],
                                                               scalar2=g8[:, 3, g:g + 1], op0=ALU.mult, op1=ALU.add),
                         reads=['rt_bi', 'g8_2', 'g8_3'], writes=[('rt_mk', g)])
                s.op('dve', lambda v: v.max(out=g8[:, 1, :], in_=mk_), reads=[('rt_mk', g) for g in range(8)] + ['g8_2'], writes=['g8_1'])
                s.op('dve', lambda v: v.tensor_scalar(out=sel_, in0=mk_, scalar1=g8[:, 1, 7:8], scalar2=None, op0=ALU.is_ge),
                     reads=[('rt_mk', g) for g in range(8)] + ['g8_1'], writes=['rt_sel'])
                s.op('dve', lambda v: v.tensor_mul(w_, sg_, sel_), reads=['rt_sg', 'rt_sel'], writes=['rt_w'])
                s.op('dve', lambda v: v.reduce_sum(out=st1[:, 6:7], in_=w_, axis=mybir.AxisListType.X), reads=['rt_w'], writes=[('ss', 6)])
                s.op('dve', lambda v: v.reciprocal(st1[:, 6:7], st1[:, 6:7]), reads=[('ss', 6)], writes=[('ss', 6)])
                gi_ = own0 // 128 + i
                s.op('dve', lambda v, gi_=gi_: v.tensor_scalar(out=combA[:, gi_, :], in0=w_, scalar1=st1[:, 6:7], scalar2=ROUTED_SCALE, op0=ALU.mult, op1=ALU.mult),
                     reads=['rt_w', ('ss', 6)], writes=[('combA', gi_)])
                s.op('dve', lambda v, gi_=gi_: v.tensor_copy(selA[:, gi_, :], sel_), reads=['rt_sel'], writes=[('selA', gi_)])
            s.barrier()
            ls.close()
            ls = ExitStack()
            sbl = lambda name, shape, dt: ls.enter_context(nc.sbuf_tensor(name + '_4d' + tl['name'], list(shape), dt))
            hid = sbl("hid", [128, 4, 1024], BF16)
            ws.add(wsrc(w_sg, 0, DEXP), 16)
            ws.add(wsrc(w_su, 0, DEXP), 16)
            ws.add(w_sd.rearrange("(k p) n -> p k n", p=128), 4)
            (gslot, wg), (uslot, wu), (dslot, wd) = ws.get_group(3)
            for hc in range(4):
                for m in range(nm):
                    bg = psn([0, 1])
                    bu = psn([2, 3])
                    s.op('pe', lambda pe, bg=bg, hc=hc, m=m, wg=wg: mm_group(
                        pe, ps[bg][:], [(wg[:, k, hc * 128:(hc + 1) * 128], hT[:, k, m * 512:(m + 1) * 512]) for k in range(16)]),
                        reads=[('ring', gslot)], writes=[('ps', bg)])
                    s.op('pe', lambda pe, bu=bu, hc=hc, m=m, wu=wu: mm_group(
                        pe, ps[bu][:], [(wu[:, k, hc * 128:(hc + 1) * 128], hT[:, k, m * 512:(m + 1) * 512]) for k in range(16)]),
                        reads=[('ring', uslot)], writes=[('ps', bu)])
                    f1 = stgf()
                    s.op('act', lambda a, bg=bg, f1=f1: a.activation(out=stagef[f1][:], in_=ps[bg][:], func=AF.Silu),
                         reads=[('ps', bg)], writes=[('stagef', f1)])
                    s.op('dve', lambda v, bu=bu, f1=f1, hc=hc, m=m: v.tensor_mul(hid[:, hc, m * 512:(m + 1) * 512], stagef[f1][:], ps[bu][:]),
                         reads=[('ps', bu), ('stagef', f1)], writes=[('hid', hc, m)])
            hkeys = [('hid', hc, m) for hc in range(4) for m in range(nm)]
            for i in range(nb):
                for cb in range(4):
                    b = psn([4, 5, 6, 7])
                    s.op('pe', lambda pe, b=b, i=i, cb=cb, wd=wd: mm_group(
                        pe, ps[b][:], [(hid[:, hc, i * 128:(i + 1) * 128], wd[:, hc, cb * 512:(cb + 1) * 512]) for hc in range(4)]),
                        reads=[('ring', dslot)] + hkeys, writes=[('ps', b)])
                    s.op('act', lambda a, b=b, i=i, cb=cb: a.copy(big[:, i, cb * 512:(cb + 1) * 512], ps[b][:]), reads=[('ps', b)], writes=[('acc', i)])
                s.dma('sp', [(shd[own0 + i * 128:own0 + (i + 1) * 128, :], big[:, i, :])], reads=[('acc', i)], semkey=('x1s', i))
            ws.finish()
            s.barrier()
            ls.close()
        pes.close()
        if phases <= 4:
            return nc

        I32 = mybir.dt.int32
        pes = ExitStack()
        sbp = lambda name, shape, dt: pes.enter_context(nc.sbuf_tensor(name + '_p5', list(shape), dt))
        selb = selA
        sloti = sbp("sloti", [128, NTB * 8], I32)
        wk = sbp("wk", [128, NTB * 8], F32)
        widx = sbp("widx", [128, NBLK], I32)
        widxd = sbp("widxd", [128, NBLK * 4], I32)
        yidx = sbp("yidx", [128, NBLK * 4], I32)
        tst = ExitStack()
        sbt = lambda name, shape, dt: tst.enter_context(nc.sbuf_tensor(name + '_p5t', list(shape), dt))
        ltri = sbt("ltri", [128, 128], BF16)
        ltf = sbt("ltf", [128, 128], F32)
        rankA = sbt("rankA", [128, NTB, NE], F32)
        cnt = sbt("cnt", [128, 6, NE], F32)
        cnti = sbt("cnti", [128, NE], I32)
        valt = sbt("valt", [128, NE], F32)
        oht = sbt("oht", [128, NE], F32)
        t8 = sbt("t8", [128, 8], F32)
        slotf = sbt("slotf", [128, NTB * 8], F32)
        ebf = sbt("ebf", [128, NBLK], F32)
        pcol_i = sbt("pcol_i", [128, 1], I32)
        pcol = sbt("pcol", [128, 1], F32)
        ebp = sbt("ebp", [128, NBLK], F32)
        wdf = sbt("wdf", [128, NBLK, 4], F32)
        pk4 = sbt("pk4", [128, 4], F32)
        bio_i = sbt("bio_i", [128, NBLK], I32)
        bio = sbt("bio", [128, NBLK], F32)
        oob = sbt("oob", [128, NBLK], F32)
        wyf = sbt("wyf", [128, NBLK, 4], F32)
        s.op('pool', lambda g: g.memset(ltf[:], 1.0), writes=['ltf'])
        s.op('pool', lambda g: g.affine_select(out=ltf[:], in_=ltf[:], pattern=[[1, 128]], compare_op=ALU.is_gt, fill=0.0, base=0,
                                               channel_multiplier=-1), reads=['ltf'], writes=['ltf'])
        s.op('dve', lambda v: v.tensor_copy(ltri[:], ltf[:]), reads=['ltf'], writes=['ltri'])
        s.op('pool', lambda g: g.iota(pcol_i[:], pattern=[[0, 1]], base=0, channel_multiplier=1), writes=['pcol_i'])
        s.op('dve', lambda v: v.tensor_copy(pcol[:], pcol_i[:]), reads=['pcol_i'], writes=['pcol'])
        for i in range(NTB):
            b = psn([0, 1, 2, 3])
            s.op('pe', lambda pe, b=b, i=i: mm_group(pe, ps[b][:, 0:NE], [(ltri[:], selb[:, i, :])] + [(ones_b[:], selb[:, i2, :]) for i2 in range(i)]),
                 reads=['selb', 'ltri', 'ones_b'], writes=[('ps', b)])
            s.op('act', lambda a, b=b, i=i: a.copy(rankA[:, i, :], ps[b][:, 0:NE]), reads=[('ps', b)], writes=[('rank', i)])
        b = psn([0, 1, 2, 3])
        s.op('pe', lambda pe, b=b: mm_group(pe, ps[b][:, 0:NE], [(ones_b[:], selb[:, i2, :]) for i2 in range(NTB)]),
             reads=['selb', 'ones_b'], writes=[('ps', b)])
        c_cnt, c_nb, c_a, c_b, c_bs, c_sb = [cnt[:, q_, :] for q_ in range(6)]
        s.op('act', lambda a, b=b: a.copy(c_cnt, ps[b][:, 0:NE]), reads=[('ps', b)], writes=['cnt'])
        s.op('dve', lambda v: v.tensor_scalar(out=c_nb, in0=c_cnt, scalar1=511.0, scalar2=1.0 / 512.0, op0=ALU.add, op1=ALU.mult), reads=['cnt'], writes=['nb'])
        s.op('dve', lambda v: v.tensor_scalar_add(c_nb, c_nb, -0.5 + 2.0 ** -11), reads=['nb'], writes=['nb'])
        s.op('dve', lambda v: v.tensor_copy(cnti[:], c_nb), reads=['nb'], writes=['cnti'])
        s.op('dve', lambda v: v.tensor_copy(c_nb, cnti[:]), reads=['cnti'], writes=['nb'])
        s.op('dve', lambda v: v.tensor_copy(c_a, c_nb), reads=['nb'], writes=['sa'])
        cur, nxt, kc, kn = c_a, c_b, 'sa', 'sb'
        for st_ in (1, 2, 4, 8, 16, 32):
            s.op('dve', lambda v, cur=cur, nxt=nxt, st_=st_: v.tensor_copy(nxt[:, 0:st_], cur[:, 0:st_]), reads=[kc], writes=[kn])
            s.op('dve', lambda v, cur=cur, nxt=nxt, st_=st_: v.tensor_add(nxt[:, st_:NE], cur[:, st_:NE], cur[:, 0:NE - st_]), reads=[kc, kn], writes=[kn])
            cur, nxt, kc, kn = nxt, cur, kn, kc
        s.op('dve', lambda v, cur=cur: v.tensor_sub(c_bs, cur, c_nb), reads=[kc, 'nb'], writes=['bs'])
        s.op('dve', lambda v: v.tensor_scalar(out=c_sb, in0=c_bs, scalar1=512.0, scalar2=1.0, op0=ALU.mult, op1=ALU.add), reads=['bs'], writes=['sbase'])
        for i in range(NTB):
            s.op('dve', lambda v, i=i: v.tensor_add(valt[:], rankA[:, i, :], c_sb), reads=[('rank', i), 'sbase'], writes=['valt'])
            s.op('dve', lambda v, i=i: v.tensor_mul(valt[:], valt[:], selA[:, i, :]), reads=['valt'], writes=['valt'])
            s.op('dve', lambda v: v.max(out=t8[:], in_=valt[:]), reads=['valt'], writes=['t8'])
            s.op('dve', lambda v, i=i: v.tensor_scalar_add(slotf[:, i * 8:(i + 1) * 8], t8[:], -1.0), reads=['t8'], writes=[('slotf', i)])
            for k in range(8):
                s.op('dve', lambda v, i=i, k=k: v.scalar_tensor_tensor(out=oht[:], in0=valt[:], scalar=t8[:, k:k + 1], in1=combA[:, i, :],
                                                                        op0=ALU.is_equal, op1=ALU.mult), reads=['valt', 't8'], writes=['oht'])
                s.op('dve', lambda v, i=i, k=k: v.reduce_sum(out=wk[:, i * 8 + k:i * 8 + k + 1], in_=oht[:], axis=mybir.AxisListType.X),
                     reads=['oht'], writes=[('wk', i)])
        s.op('dve', lambda v: v.tensor_copy(sloti[:], slotf[:]), reads=[('slotf', i) for i in range(NTB)], writes=['sloti'])
        for b_ in range(NBLK):
            s.op('dve', lambda v, b_=b_: v.tensor_scalar(out=oht[:], in0=c_bs, scalar1=float(b_), scalar2=None, op0=ALU.is_le), reads=['bs'], writes=['oht'])
            s.op('dve', lambda v, b_=b_: v.reduce_sum(out=ebf[:, b_:b_ + 1], in_=oht[:], axis=mybir.AxisListType.X), reads=['oht'], writes=[('ebf', b_)])
        s.op('dve', lambda v: v.tensor_scalar(out=ebf[:], in0=ebf[:], scalar1=-1.0, scalar2=128.0, op0=ALU.add, op1=ALU.mult),
             reads=[('ebf', b_) for b_ in range(NBLK)], writes=['ebf2'])
        OOBV = float(1 << 20)
        s.op('pool', lambda g: g.iota(bio_i[:], pattern=[[1, NBLK]], base=0, channel_multiplier=0), writes=['bio_i'])
        s.op('dve', lambda v: v.tensor_copy(bio[:], bio_i[:]), reads=['bio_i'], writes=['bio'])
        s.op('dve', lambda v, cur=cur: v.tensor_scalar(out=oob[:], in0=bio[:], scalar1=cur[:, NE - 1:NE], scalar2=OOBV, op0=ALU.is_ge, op1=ALU.mult),
             reads=['bio', kc], writes=['oob'])
        s.op('dve', lambda v: v.tensor_add(ebf[:], ebf[:], oob[:]), reads=['ebf2', 'oob'], writes=['ebf2'])
        s.op('dve', lambda v: v.tensor_scalar(out=ebp[:], in0=ebf[:], scalar1=pcol[:, 0:1], scalar2=None, op0=ALU.add), reads=['ebf2', 'pcol'], writes=['ebp'])
        s.op('dve', lambda v: v.tensor_copy(widx[:], ebp[:]), reads=['ebp'], writes=['widx'])
        for k in range(4):
            s.op('dve', lambda v, k=k: v.tensor_scalar_add(pk4[:, k:k + 1], pcol[:, 0:1], float(k * 128)), reads=['pcol'], writes=[('pk4', k)])
            s.op('dve', lambda v, k=k: v.tensor_scalar(out=wdf[:, :, k], in0=ebf[:], scalar1=4.0, scalar2=pk4[:, k:k + 1], op0=ALU.mult, op1=ALU.add),
                 reads=['ebf2', ('pk4', k)], writes=[('wdf', k)])
        s.op('dve', lambda v: v.tensor_copy(widxd[:], wdf[:].rearrange("p b k -> p (b k)")), reads=[('wdf', k) for k in range(4)], writes=['widxd'])
        s.op('dve', lambda v: v.scalar_tensor_tensor(out=bio[:], in0=bio[:], scalar=512.0, in1=oob[:], op0=ALU.mult, op1=ALU.add), reads=['bio', 'oob'], writes=['bio'])
        for k in range(4):
            s.op('dve', lambda v, k=k: v.tensor_scalar(out=wyf[:, :, k], in0=bio[:], scalar1=pk4[:, k:k + 1], scalar2=None, op0=ALU.add),
                 reads=['bio', ('pk4', k)], writes=[('wyf', k)])
        s.op('dve', lambda v: v.tensor_copy(yidx[:], wyf[:].rearrange("p b k -> p (b k)")), reads=[('wyf', k) for k in range(4)], writes=['yidx'])
        bnd_w = nc.gpsimd.alloc_register("bnd_w")
        nc.gpsimd.reg_mov(bnd_w, NE * 128 - 1)
        bnd_d = nc.gpsimd.alloc_register("bnd_d")
        nc.gpsimd.reg_mov(bnd_d, NE * DEXP - 1)
        bnd_y = nc.gpsimd.alloc_register("bnd_y")
        nc.gpsimd.reg_mov(bnd_y, NSLOT - 1)
        hst = ExitStack()
        hrow = [hst.enter_context(nc.sbuf_tensor("hrow%d_p5" % i, [128, D], BF16)) for i in range(2)]

        def ind_dma(out, out_off, in_, in_off, bound, reads, writes, semkey):
            s._deps('pool', reads, writes)
            if semkey not in s.dsem:
                s.dsem[semkey] = [es.enter_context(nc.semaphore("dsem%d" % s.nsem)), 0]
                s.nsem += 1
            ent = s.dsem[semkey]
            if isinstance(bound, int):
                nc.gpsimd.indirect_dma_start(out=out, out_offset=out_off, in_=in_, in_offset=in_off).then_inc(ent[0], 16)
            else:
                nc.gpsimd.indirect_dma_start(out=out, out_offset=out_off, in_=in_, in_offset=in_off, bounds_check=bound, oob_is_err=False).then_inc(ent[0], 16)
            ent[1] += 16
            s._record((ent[0], ent[1]), reads, writes)

        tbs = list(range(NTB))
        if only_tiles is not None:
            tbs = ([0, 1, 2, 3] if 'P' in only_tiles else []) + (list(range(4, 12)) if 'S0' in only_tiles else []) + (list(range(12, 20)) if 'S1' in only_tiles else [])
        for i in tbs:
            hi = i % 2
            s.dma('sp', [(hrow[hi][:], h2d[i * 128:(i + 1) * 128, :])], writes=[('hrow', hi)], semkey=('hrow', hi))
            for k in range(8):
                ind_dma(xsort[:, :], bass.IndirectOffsetOnAxis(ap=sloti[:, i * 8 + k:i * 8 + k + 1], axis=0), hrow[hi][:, :], None, NSLOT - 1,
                        [('hrow', hi), 'sloti'], ['xsort'], ('hsc', hi))
        s.barrier()
        hst.close()
        tst.close()
        NR5 = 6
        bst = ExitStack()
        sbq = lambda name, shape, dt: bst.enter_context(nc.sbuf_tensor(name + '_p5c', list(shape), dt))
        ring5 = [sbq("ring%d" % i, [128, 8192], BF16) for i in range(NR5)]
        xg = [sbq("xg%d" % i, [128, 4, D], BF16) for i in range(2)]
        xT = [sbq("xT%d" % i, [128, 16, 512], BF16) for i in range(2)]
        hid5 = [sbq("hid%d" % i, [128, 4, 512], BF16) for i in range(2)]
        sgf = [sbq("sgf%d" % i, [128, 512], F32) for i in range(2)]
        ob = [sbq("ob%d" % i, [128, D], F32) for i in range(2)]
        wgr = w_eg.rearrange("e (p k) n -> (e p) (k n)", k=16)
        wur = w_eu.rearrange("e (p k) n -> (e p) (k n)", k=16)
        wdr = w_ed.rearrange("e h n -> (e h) n")
        nblk_run = NBLK if nexp_dbg is None else nexp_dbg
        xsb = xsort.rearrange("(b i p) d -> b p i d", p=128, i=4)
        ysb = [y_.rearrange("(b i p) d -> b i p d", p=128, i=4) for y_ in ysorth]

        def blk_loads(b_):
            for j_, src in enumerate((wgr, wur)):
                slot = (b_ * 3 + j_) % NR5
                ind_dma(ring5[slot][:, :], None, src, bass.IndirectOffsetOnAxis(ap=widx[:, b_:b_ + 1], axis=0), bnd_w,
                        ['widx'], [('ring5', slot)], ('ring5', slot))
            slot = (b_ * 3 + 2) % NR5
            s._deps('pool', ['widxd'], [('ring5', slot)])
            for k in range(4):
                ind_dma(ring5[slot][:, k * 2048:(k + 1) * 2048], None, wdr, bass.IndirectOffsetOnAxis(ap=widxd[:, b_ * 4 + k:b_ * 4 + k + 1], axis=0), bnd_d,
                        [], [], ('ring5', slot))
            s._record((s.dsem[('ring5', slot)][0], s.dsem[('ring5', slot)][1]), ['widxd'], [('ring5', slot)])
            s.dma('sp', [(xg[b_ % 2][:], xsb[b_])], writes=[('xg', b_ % 2)], semkey=('xg', b_ % 2))

        if nblk_run > 0:
            blk_loads(0)
        ecnt = 0
        ocnt5 = 0
        for b_ in range(nblk_run):
            if b_ + 1 < nblk_run:
                blk_loads(b_ + 1)
            bi = b_ % 2
            slots = [(b_ * 3 + j_) % NR5 for j_ in range(3)]
            wg = ring5[slots[0]][:].rearrange("p (k h m) -> p k h m", k=16, h=4)
            wu = ring5[slots[1]][:].rearrange("p (k h m) -> p k h m", k=16, h=4)
            wd = ring5[slots[2]][:].rearrange("p (k n) -> p k n", k=4)
            xgv = xg[bi][:].rearrange("p i (k q) -> p i k q", k=16)
            for kk in range(8):
                b = psn([6, 7])
                pst = ps[b].bitcast(BF16)

                def trx(pe, kk=kk, pst=pst, xgv=xgv):
                    last = None
                    for k2 in range(2):
                        for i4 in range(4):
                            last = pe.transpose(pst[:, k2 * 512 + i4 * 128:k2 * 512 + (i4 + 1) * 128], xgv[:, i4, kk * 2 + k2, :], ident[:])
                    return last
                s.op('pe', trx, reads=[('xg', bi), 'ident'], writes=[('ps', b)])
                eng = 'act' if ecnt % 2 == 0 else 'dve'
                ecnt += 1
                if eng == 'act':
                    s.op('act', lambda a, pst=pst, kk=kk, bi=bi: a.copy(xT[bi][:, kk * 2:kk * 2 + 2, :], pst.rearrange("p (k t) -> p k t", k=2)),
                         reads=[('ps', b)], writes=[('xT', bi, kk)])
                else:
                    s.op('dve', lambda v, pst=pst, kk=kk, bi=bi: v.tensor_copy(xT[bi][:, kk * 2:kk * 2 + 2, :], pst.rearrange("p (k t) -> p k t", k=2)),
                         reads=[('ps', b)], writes=[('xT', bi, kk)])
            xkeys = [('xT', bi, kk) for kk in range(8)]
            for hc in range(4):
                bg = psn([0, 1])
                bu = psn([2, 3])
                s.op('pe', lambda pe, bg=bg, hc=hc, wg=wg, bi=bi: mm_group(pe, ps[bg][:], [(wg[:, k, hc, :], xT[bi][:, k, :]) for k in range(16)]),
                     reads=[('ring5', slots[0])] + xkeys, writes=[('ps', bg)])
                s.op('pe', lambda pe, bu=bu, hc=hc, wu=wu, bi=bi: mm_group(pe, ps[bu][:], [(wu[:, k, hc, :], xT[bi][:, k, :]) for k in range(16)]),
                     reads=[('ring5', slots[1])] + xkeys, writes=[('ps', bu)])
                fi = hc % 2
                s.op('act', lambda a, bg=bg, fi=fi: a.activation(out=sgf[fi][:], in_=ps[bg][:], func=AF.Silu), reads=[('ps', bg)], writes=[('sgf', fi)])
                s.op('dve', lambda v, bu=bu, fi=fi, hc=hc, bi=bi: v.tensor_mul(hid5[bi][:, hc, :], sgf[fi][:], ps[bu][:]),
                     reads=[('ps', bu), ('sgf', fi)], writes=[('hid5', bi, hc)])
            hkeys = [('hid5', bi, hc) for hc in range(4)]
            for i4 in range(4):
                oi = ocnt5 % 2
                ocnt5 += 1
                for cb in range(4):
                    b = psn([4, 5])
                    s.op('pe', lambda pe, b=b, i4=i4, cb=cb, wd=wd, bi=bi: mm_group(
                        pe, ps[b][:], [(hid5[bi][:, hc, i4 * 128:(i4 + 1) * 128], wd[:, hc, cb * 512:(cb + 1) * 512]) for hc in range(4)]),
                        reads=[('ring5', slots[2])] + hkeys, writes=[('ps', b)])
                    if cb % 2 == 0:
                        s.op('act', lambda a, b=b, oi=oi, cb=cb: a.copy(ob[oi][:, cb * 512:(cb + 1) * 512], ps[b][:]), reads=[('ps', b)], writes=[('ob', oi)])
                    else:
                        s.op('dve', lambda v, b=b, oi=oi, cb=cb: v.tensor_copy(ob[oi][:, cb * 512:(cb + 1) * 512], ps[b][:]), reads=[('ps', b)], writes=[('ob', oi)])
                s.dma('sp', [(ysb[hf][b_, i4], ob[oi][:, hf * 1024:(hf + 1) * 1024]) for hf in range(2)], reads=[('ob', oi)], writes=['ysort'], semkey=('ob', oi))
        s.barrier()
        bst.close()
        sbp = lambda name, shape, dt: pes.enter_context(nc.sbuf_tensor(name + '_p6', list(shape), dt))
        bc = [sbp("bc%d" % i, [128, D], F32) for i in range(2)]
        hb = sbp("hb", [128, D], BF16)
        accb = [sbp("accb%d" % i, [128, D], F32) for i in range(2)]
        yg = [sbp("yg%d" % i, [128, D], F32) for i in range(3)]
        x1b = [sbp("x1b%d" % i, [128, D], F32) for i in range(2)]
        load_bc(0, 0, 5, 'G2')
        load_bc(1, 1, 5, 'G2')
        ygc = 0
        outs = [(yp, 0, 0)] * 4 + [(ys, 512, 1)] * 16
        for i in tbs:
            ai = i % 2
            ydst, yoff, cnd = outs[i]
            s.dma('sp', [(accb[ai][:], shd[i * 128:(i + 1) * 128, :])], writes=[('accb', ai)], semkey=('accb', ai))
            s.dma('sp', [(x1b[ai][:], x1d[i * 128:(i + 1) * 128, :])], writes=[('x1b', ai)], semkey=('x1b', ai))
            for k in range(8):
                gi_ = ygc % 3
                ygc += 1
                for hf in range(2):
                    ind_dma(yg[gi_][:, hf * 1024:(hf + 1) * 1024], None, ysorth[hf][:, :], bass.IndirectOffsetOnAxis(ap=sloti[:, i * 8 + k:i * 8 + k + 1], axis=0),
                            NSLOT - 1, ['sloti'], [('yg', gi_, hf)], ('yg', gi_, hf))
                s.op('dve', lambda v, ai=ai, gi_=gi_, i=i, k=k: v.scalar_tensor_tensor(out=accb[ai][:], in0=yg[gi_][:], scalar=wk[:, i * 8 + k:i * 8 + k + 1],
                                                                                   in1=accb[ai][:], op0=ALU.mult, op1=ALU.add),
                     reads=[('yg', gi_, 0), ('yg', gi_, 1)], writes=[('accb', ai)])
            s.op('act', lambda a, ai=ai: a.activation(out=hb[:], in_=accb[ai][:], func=AF.Square, accum_out=st1[:, 5:6]),
                 reads=[('accb', ai)], writes=['hb', ('ss', 5)])
            rstd_from_ss(5)
            s.op('dve', lambda v, ai=ai, cnd=cnd: v.scalar_tensor_tensor(out=accb[ai][:], in0=accb[ai][:], scalar=st1[:, 5:6], in1=bc[cnd][:],
                                                                         op0=ALU.mult, op1=ALU.mult),
                 reads=[('ss', 5), ('bc', cnd)], writes=[('accb', ai)])
            s.op('dve', lambda v, ai=ai: v.tensor_add(accb[ai][:], accb[ai][:], x1b[ai][:]), reads=[('x1b', ai)], writes=[('accb', ai)])
            r = i * 128 - yoff
            s.dma('sp', [(ydst[r:r + 128, :], accb[ai][:])], reads=[('accb', ai)], semkey=('yst', ai))
        s.barrier()
        pes.close()
        pes.close()
    return nc


def _prep_inputs(inp):
    f = lambda a: np.ascontiguousarray(np.asarray(a, dtype=np.float32))
    x_prompt = f(inp['x_prompt'])
    x_sample = f(inp['x_sample'])
    cache_k = f(inp['cache_k'])
    cache_v = f(inp['cache_v'])
    c = f(inp['c'])
    c_ctx = f(inp['c_ctx'])
    shared = {
        'w_ada': f(inp['w_ada'][0]), 'b_ada': f(inp['b_ada']), 'g_pre_mix': f(inp['g_pre_mix']), 'g_post_mix': f(inp['g_post_mix']),
        'w_in': f(inp['w_in'][0]), 'lq1': f(inp['lambda_q1']), 'lk1': f(inp['lambda_k1']), 'lq2': f(inp['lambda_q2']),
        'lk2': f(inp['lambda_k2']), 'g_subln': f(inp['g_subln']), 'conv_w': f(inp['conv_w'][0]), 'w_ao': f(inp['w_attn_out'][0]),
        'w_co': f(inp['w_conv_out'][0]), 'w_o': f(inp['w_o'][0]), 'g_pre_ffn': f(inp['g_pre_ffn']), 'g_post_ffn': f(inp['g_post_ffn']),
        'w_router': f(inp['w_router'][0]), 'router_bias': f(inp['router_bias']), 'w_eg': f(inp['w_exp_gate'][0]),
        'w_eu': f(inp['w_exp_up'][0]), 'w_ed': f(inp['w_exp_down'][0]), 'w_sg': f(inp['w_sh_gate'][0]), 'w_su': f(inp['w_sh_up'][0]),
        'w_sd': f(inp['w_sh_down'][0]),
    }
    maps = []
    for core in range(8):
        b = core // 2
        h = core % 2
        own = x_sample[b, h * 2048:(h + 1) * 2048]
        oth = x_sample[b, (1 - h) * 2048:(2 - h) * 2048]
        xh = np.zeros((2, D), np.float32)
        hmask = np.array([[0.0, 1.0, 1.0, 0.0]], np.float32)
        if h == 1:
            xh[0] = x_sample[b, 2047]
            hmask[0, 0] = 1.0
        else:
            xh[1] = x_sample[b, 2048]
            hmask[0, 3] = 1.0
        pidx_ = np.concatenate([np.arange(h * 2048, (h + 1) * 2048), np.arange((1 - h) * 2048, (2 - h) * 2048)])
        posv = np.ascontiguousarray(np.stack([pidx_ // 64, pidx_ % 64], axis=0).astype(np.float32))
        m = dict(shared)
        m.update({
            'xp': np.ascontiguousarray(x_prompt[2 * core:2 * core + 2].reshape(512, D)),
            'xs': np.ascontiguousarray(np.concatenate([own, oth], axis=0)),
            'xh': xh, 'pos': posv, 'hmask': hmask,
            'csel': np.ascontiguousarray(np.stack([c_ctx, c[b]], axis=0)),
            'ck': np.ascontiguousarray(cache_k[b, 0].reshape(PAST, 2048)),
            'cv': np.ascontiguousarray(cache_v[b, 0].reshape(PAST, 2048)),
        })
        maps.append(m)
    return maps


def kernel(**inputs):
    nc = build()
    maps = _prep_inputs(inputs)
    res = run_bass_kernel_spmd(nc, maps, core_ids=list(range(8)))
    r = res.results
    y_prompt = np.stack([r[c]['yp'].reshape(2, 256, D) for c in range(8)], axis=0).reshape(16, 256, D)
    y_sample = np.stack([r[c]['ys'] for c in range(8)], axis=0).reshape(4, 4096, D)
    nkk = np.stack([r[c]['nk'].reshape(2, 256, 2, NH, HD) for c in range(8)], axis=0).reshape(16, 1, 256, 2, NH, HD)
    nvv = np.stack([r[c]['nv'].reshape(2, 256, NH, VD) for c in range(8)], axis=0).reshape(16, 1, 256, NH, VD)
    return (y_prompt.astype(np.float32), y_sample.astype(np.float32), nkk.astype(np.float32), nvv.astype(np.float32))
```
